# Optimizing a Trainium2 kernel written in Bass

```python
import math
import jax, jax.numpy as jnp
from jax import lax
import numpy as np

D_MODEL = 4096
BATCH = 4
SEQ = 4096
DEPTH = 1

GRID_W = 64
CTX_LEN = 256
W_HYENA = D_MODEL // 2
W_LRU = D_MODEL - W_HYENA
HY_SHORT = 3
HY_SHORT_LEFT = 1
FILT_EMB = 33
FILT_BANDS = (FILT_EMB - 1) // 2
FILT_HID = 64
DECAY_TARGET = 1e-2
FAST_DECAY_PCT = 0.3
SLOW_DECAY_PCT = 1.5
MIN_DECAY = math.log(DECAY_TARGET) / SLOW_DECAY_PCT
MAX_DECAY = math.log(DECAY_TARGET) / FAST_DECAY_PCT
LRU_HEADS = 16
LRU_HEAD_DIM = W_LRU // LRU_HEADS
LRU_CONV = 4
LRU_CONV_LEFT = 2
LRU_C = 8.0
N_EXPERTS = 16
EXPERT_FF = D_MODEL // 2
CAPACITY_FACTOR = 2
N_MOD = 6
EPS = 1e-6
D_IN = 3 * W_HYENA + 2 * W_LRU
IN_LRU_GATE = 3 * W_HYENA
IN_LRU_X = 3 * W_HYENA + W_LRU

kernel_name = "hymba_hyena_rglru_ecmoe_dit"


def rmsnorm(x, g):
    x32 = x.astype(jnp.float32)
    y = x32 * lax.rsqrt(jnp.mean(x32 * x32, axis=-1, keepdims=True) + EPS)
    return (y * g.astype(jnp.float32)).astype(x.dtype)


def modulate(h, shift, scale):
    return h * (1 + scale) + shift


def dwconv(x, w, b, left):
    width = w.shape[0]
    n = x.shape[1]
    xp = jnp.pad(x, ((0, 0), (left, width - 1 - left), (0, 0)))
    y = b
    for k in range(width):
        y = y + w[k] * xp[:, k:k + n]
    return y


def grid_dwconv(x, w, b, left, rows):
    bsz, n, ch = x.shape
    y = dwconv(x.reshape(bsz * rows, GRID_W, ch), w, b, left)
    return y.reshape(bsz, n, ch)


def hyena_filter(n, w1, b1, w2, b2, w3, b3, wout, freq):
    f32 = jnp.float32
    pos = jnp.arange(n, dtype=f32)
    t = jnp.linspace(0.0, 1.0, n, dtype=f32)[:, None]
    bands = jnp.linspace(1e-4, FILT_BANDS - 1, FILT_BANDS, dtype=f32)
    ang = (2.0 * math.pi * pos / n)[:, None] * bands[None, :]
    z = jnp.concatenate([t, jnp.cos(ang), -jnp.sin(ang)], axis=-1)
    fr = freq.astype(f32)
    h = jnp.sin(fr * (z @ w1.astype(f32) + b1.astype(f32)))
    h = jnp.sin(fr * (h @ w2.astype(f32) + b2.astype(f32)))
    h = jnp.sin(fr * (h @ w3.astype(f32) + b3.astype(f32)))
    h = (h @ wout.astype(f32)).reshape(n, 2, W_HYENA)
    deltas = jnp.abs(jnp.linspace(MIN_DECAY, MAX_DECAY, W_HYENA, dtype=f32))
    decay = jnp.exp(-t * deltas[None, :])
    h = h * decay[:, None, :]
    h = h / jnp.sum(jnp.abs(h), axis=(0, 1), keepdims=True)
    return jnp.concatenate([h[:, 0], jnp.zeros((1, W_HYENA), f32), h[:0:-1, 1]], axis=0)


def fftconv(u, kern, bias):
    n = u.shape[1]
    u32 = u.astype(jnp.float32)
    uf = jnp.fft.rfft(u32, n=2 * n, axis=1)
    kf = jnp.fft.rfft(kern, n=2 * n, axis=0)
    y = jnp.fft.irfft(uf * kf[None], n=2 * n, axis=1)[:, :n]
    return y + u32 * bias.astype(jnp.float32)


def hyena_mix(u, kern, bias):
    x0 = u[..., :W_HYENA]
    x1 = u[..., W_HYENA:2 * W_HYENA]
    v = u[..., 2 * W_HYENA:]
    z = fftconv(x1 * v, kern, bias)
    return (x0.astype(jnp.float32) * z).astype(u.dtype)


def block_diag(x, w, b):
    bsz, n, _ = x.shape
    y = jnp.einsum('bnhi,hij->bnhj', x.reshape(bsz, n, LRU_HEADS, LRU_HEAD_DIM), w)
    return y.reshape(bsz, n, W_LRU) + b


def rglru_coeffs(xc, wa, ba, wx, bx, lam):
    r = jax.nn.sigmoid(block_diag(xc, wa, ba)).astype(jnp.float32)
    i = jax.nn.sigmoid(block_diag(xc, wx, bx))
    log_a = -LRU_C * r * jax.nn.softplus(-lam.astype(jnp.float32))
    a = jnp.exp(log_a)
    b = jnp.sqrt(-jnp.expm1(2.0 * log_a)) * (i * xc).astype(jnp.float32)
    return a, b


def linear_scan(a, b, h0, reverse):
    def step(h, ab):
        h = ab[0] * h + ab[1]
        return h, h
    h_last, hs = lax.scan(step, h0, (jnp.swapaxes(a, 0, 1), jnp.swapaxes(b, 0, 1)), reverse=reverse)
    return jnp.swapaxes(hs, 0, 1), h_last


def rglru_bidir(xc, wa, ba, wx, bx, lam, h0_f, h0_b):
    a_f, b_f = rglru_coeffs(xc, wa[0], ba[0], wx[0], bx[0], lam[0])
    hs_f, hT_f = linear_scan(a_f, b_f, h0_f, False)
    a_b, b_b = rglru_coeffs(xc, wa[1], ba[1], wx[1], bx[1], lam[1])
    hs_b, hT_b = linear_scan(a_b, b_b, h0_b, True)
    return hs_f, hs_b, hT_f, hT_b


def expert_choice_ffn(h, w_router, w_g, w_u, w_d):
    bsz, n, d = h.shape
    cap = CAPACITY_FACTOR * n // N_EXPERTS
    aff = jax.nn.softmax(jnp.einsum('bnd,de->bne', h, w_router).astype(jnp.float32), axis=-1)
    gates, idx = lax.top_k(jnp.swapaxes(aff, 1, 2), cap)
    xs = jax.vmap(lambda hb, ib: hb[ib])(h, idx)
    ga = jnp.einsum('becd,edf->becf', xs, w_g)
    up = jnp.einsum('becd,edf->becf', xs, w_u)
    y = jnp.einsum('becf,efd->becd', jax.nn.silu(ga) * up, w_d) * gates[..., None].astype(h.dtype)
    return jax.vmap(lambda ib, yb: jnp.zeros((n, d), h.dtype).at[ib.reshape(-1)].add(yb.reshape(-1, d)))(idx, y)


def setup_inputs(seed: int = 0) -> dict:
    key = jax.random.key(seed)
    ks = jax.random.split(key, 40)
    f32 = jnp.float32

    def nrm(k, shape, scale):
        return jax.random.normal(k, shape, f32) * scale

    L = DEPTH
    u_lam = jax.random.uniform(ks[26], (L, 2, W_LRU), f32, minval=0.9, maxval=0.999)
    a0 = u_lam ** (1.0 / LRU_C)
    lru_lambda = jnp.log(a0) - jnp.log1p(-a0)
    return {
        'x': nrm(ks[0], (BATCH, SEQ, D_MODEL), 1.0),
        'c': nrm(ks[1], (BATCH, D_MODEL), 1.0),
        'ctx': nrm(ks[2], (BATCH, CTX_LEN, D_MODEL), 1.0),
        'c_ctx': nrm(ks[3], (D_MODEL,), 1.0),
        'w_mod': nrm(ks[4], (L, D_MODEL, N_MOD * D_MODEL), 0.5 * D_MODEL ** -0.5),
        'b_mod': nrm(ks[5], (L, N_MOD * D_MODEL), 0.02),
        'g_mix': 1.0 + nrm(ks[6], (L, D_MODEL), 0.02),
        'g_ffn': 1.0 + nrm(ks[7], (L, D_MODEL), 0.02),
        'w_in': nrm(ks[8], (L, D_MODEL, D_IN), D_MODEL ** -0.5),
        'b_in': nrm(ks[9], (L, D_IN), 0.02),
        'hy_conv_w': nrm(ks[10], (L, HY_SHORT, 3 * W_HYENA), HY_SHORT ** -0.5),
        'hy_conv_b': nrm(ks[11], (L, 3 * W_HYENA), 0.02),
        'hy_f_w1': nrm(ks[12], (L, FILT_EMB, FILT_HID), FILT_EMB ** -0.5),
        'hy_f_b1': nrm(ks[13], (L, FILT_HID), 0.02),
        'hy_f_w2': nrm(ks[14], (L, FILT_HID, FILT_HID), FILT_HID ** -0.5),
        'hy_f_b2': nrm(ks[15], (L, FILT_HID), 0.02),
        'hy_f_w3': nrm(ks[16], (L, FILT_HID, FILT_HID), FILT_HID ** -0.5),
        'hy_f_b3': nrm(ks[17], (L, FILT_HID), 0.02),
        'hy_f_wout': nrm(ks[18], (L, FILT_HID, 2 * W_HYENA), FILT_HID ** -0.5),
        'hy_f_freq': 1.0 + nrm(ks[19], (L, FILT_HID), 0.1),
        'hy_bias': nrm(ks[20], (L, W_HYENA), 0.5),
        'lru_conv_w': nrm(ks[21], (L, LRU_CONV, W_LRU), 0.5),
        'lru_conv_b': nrm(ks[22], (L, W_LRU), 0.02),
        'lru_wa': nrm(ks[23], (L, 2, LRU_HEADS, LRU_HEAD_DIM, LRU_HEAD_DIM), LRU_HEAD_DIM ** -0.5),
        'lru_ba': nrm(ks[24], (L, 2, W_LRU), 0.02),
        'lru_wx': nrm(ks[25], (L, 2, LRU_HEADS, LRU_HEAD_DIM, LRU_HEAD_DIM), LRU_HEAD_DIM ** -0.5),
        'lru_bx': nrm(ks[27], (L, 2, W_LRU), 0.02),
        'lru_lambda': lru_lambda,
        'w_out': nrm(ks[28], (L, D_MODEL, D_MODEL), D_MODEL ** -0.5),
        'b_out': nrm(ks[29], (L, D_MODEL), 0.02),
        'w_router': nrm(ks[30], (L, D_MODEL, N_EXPERTS), D_MODEL ** -0.5),
        'w_exp_gate': nrm(ks[31], (L, N_EXPERTS, D_MODEL, EXPERT_FF), D_MODEL ** -0.5),
        'w_exp_up': nrm(ks[32], (L, N_EXPERTS, D_MODEL, EXPERT_FF), D_MODEL ** -0.5),
        'w_exp_down': nrm(ks[33], (L, N_EXPERTS, EXPERT_FF, D_MODEL), EXPERT_FF ** -0.5),
        'g_final': 1.0 + nrm(ks[34], (D_MODEL,), 0.02),
    }


def reference(x, c, ctx, c_ctx, w_mod, b_mod, g_mix, g_ffn, w_in, b_in, hy_conv_w, hy_conv_b,
              hy_f_w1, hy_f_b1, hy_f_w2, hy_f_b2, hy_f_w3, hy_f_b3, hy_f_wout, hy_f_freq, hy_bias,
              lru_conv_w, lru_conv_b, lru_wa, lru_ba, lru_wx, lru_bx, lru_lambda, w_out, b_out,
              w_router, w_exp_gate, w_exp_up, w_exp_down, g_final):
    bsz, n_lat, _ = x.shape
    n_ctx = ctx.shape[1]
    rows = n_lat // GRID_W
    silu_c = jax.nn.silu(c)
    silu_cc = jax.nn.silu(c_ctx)
    for l in range(DEPTH):
        ctx_needed = l < DEPTH - 1
        mod_x = (silu_c @ w_mod[l] + b_mod[l])[:, None, :]
        sh1, sc1, gt1, sh2, sc2, gt2 = jnp.split(mod_x, N_MOD, axis=-1)
        mod_c = silu_cc @ w_mod[l] + b_mod[l]
        csh1, csc1, cgt1, csh2, csc2, cgt2 = jnp.split(mod_c, N_MOD, axis=-1)
        filt = (hy_f_w1[l], hy_f_b1[l], hy_f_w2[l], hy_f_b2[l], hy_f_w3[l], hy_f_b3[l], hy_f_wout[l], hy_f_freq[l])
        lru_p = (lru_wa[l], lru_ba[l], lru_wx[l], lru_bx[l], lru_lambda[l])

        hx = modulate(rmsnorm(x, g_mix[l]), sh1, sc1)
        hc = modulate(rmsnorm(ctx, g_mix[l]), csh1, csc1)
        px = hx @ w_in[l] + b_in[l]
        if ctx_needed:
            pc = hc @ w_in[l] + b_in[l]
            pc_lx = pc[..., IN_LRU_X:]
        else:
            pc_lx = hc @ w_in[l][:, IN_LRU_X:] + b_in[l][IN_LRU_X:]

        xc_c = dwconv(pc_lx, lru_conv_w[l], lru_conv_b[l], LRU_CONV_LEFT)
        h_zero = jnp.zeros((bsz, W_LRU), jnp.float32)
        hcf, hcb, hT_f, hT_b = rglru_bidir(xc_c, *lru_p, h_zero, h_zero)

        u_x = grid_dwconv(px[..., :IN_LRU_GATE], hy_conv_w[l], hy_conv_b[l], HY_SHORT_LEFT, rows)
        y_hy = hyena_mix(u_x, hyena_filter(n_lat, *filt), hy_bias[l])
        xc_x = grid_dwconv(px[..., IN_LRU_X:], lru_conv_w[l], lru_conv_b[l], LRU_CONV_LEFT, rows)
        hxf, hxb, _, _ = rglru_bidir(xc_x, *lru_p, hT_f, hT_b)
        y_lru = jax.nn.gelu(px[..., IN_LRU_GATE:IN_LRU_X]) * (hxf + hxb).astype(px.dtype)
        y_x = jnp.concatenate([y_hy, y_lru], axis=-1) @ w_out[l] + b_out[l]
        x = x + gt1 * y_x

        if ctx_needed:
            u_c = dwconv(pc[..., :IN_LRU_GATE], hy_conv_w[l], hy_conv_b[l], HY_SHORT_LEFT)
            yc_hy = hyena_mix(u_c, hyena_filter(n_ctx, *filt), hy_bias[l])
            yc_lru = jax.nn.gelu(pc[..., IN_LRU_GATE:IN_LRU_X]) * (hcf + hcb).astype(pc.dtype)
            y_c = jnp.concatenate([yc_hy, yc_lru], axis=-1) @ w_out[l] + b_out[l]
            ctx = ctx + cgt1 * y_c
            hc2 = modulate(rmsnorm(ctx, g_ffn[l]), csh2, csc2)
            ctx = ctx + cgt2 * expert_choice_ffn(hc2, w_router[l], w_exp_gate[l], w_exp_up[l], w_exp_down[l])

        hx2 = modulate(rmsnorm(x, g_ffn[l]), sh2, sc2)
        x = x + gt2 * expert_choice_ffn(hx2, w_router[l], w_exp_gate[l], w_exp_up[l], w_exp_down[l])
    return rmsnorm(x, g_final)
```

```python
import os, math
import numpy as np
import ml_dtypes
from contextlib import ExitStack
import concourse.bass as bass
import concourse.mybir as mybir
from concourse.bass_utils import run_bass_kernel_spmd

F32 = mybir.dt.float32; BF16 = mybir.dt.bfloat16; U32 = mybir.dt.uint32; I32 = mybir.dt.int32
AF = mybir.ActivationFunctionType; ALU = mybir.AluOpType; AX = mybir.AxisListType
NPBF = ml_dtypes.bfloat16

D = 4096; T = 4096; NB = 4; KC = 32; NCTX = 256
DIN = 10240; NE = 16; FF = 2048; CAP = 512
EPS = 1e-6
MIN_DECAY = math.log(1e-2) / 1.5; MAX_DECAY = math.log(1e-2) / 0.3
MAGIC = 12582912.0
TWO_PI = 2.0 * math.pi
STAGE = int(os.environ.get("KSTAGE", "99"))
NCORES = int(os.environ.get('KCORES', '4'))

ENGS = {'pe': 'tensor', 'dve': 'vector', 'act': 'scalar', 'pool': 'gpsimd', 'sp': 'sync'}


class Reg:
    __slots__ = ('name', 'w', 'r')

    def __init__(self, name):
        self.name = name; self.w = None; self.r = {}


class Em:
    def __init__(self, nc, es):
        self.nc = nc; self.es = es
        self.q = {e: [] for e in ENGS}
        self.cnt = {e: 0 for e in ENGS}
        self.nsem = 0
        self.sem = {e: self._newsem("e_" + e) for e in ENGS}
        self.mine = {e: {id(self.sem[e])} for e in ENGS}
        self.seen = {e: {} for e in ENGS}
        self.streams = {}
        self.allsems = {}

    def _newsem(self, name):
        self.nsem += 1
        return self.es.enter_context(self.nc.semaphore(f"{name}_{self.nsem}"))

    def _wait(self, eng, tok):
        sem, val = tok
        if eng == 'pe' and id(sem) in self.mine['pe']:
            return
        if self.seen[eng].get(id(sem), 0) >= val:
            return
        self.seen[eng][id(sem)] = val
        self.q[eng].append(('w', sem, val))

    def _deps(self, eng, reads, writes):
        for r in reads:
            if r.w is not None:
                self._wait(eng, r.w)
        for w in writes:
            if w.w is not None:
                self._wait(eng, w.w)
            for t in w.r.values():
                self._wait(eng, t)

    def _mark(self, tok, reads, writes):
        k = id(tok[0])
        for r in reads:
            if k not in r.r or r.r[k][1] < tok[1]:
                r.r[k] = tok
        for w in writes:
            w.w = tok; w.r = {}
        self.allsems[k] = tok

    def op(self, eng, fn, reads=(), writes=()):
        self._deps(eng, reads, writes)
        if self.cnt[eng] >= 30000:
            self.sem[eng] = self._newsem("e_" + eng); self.mine[eng].add(id(self.sem[eng])); self.cnt[eng] = 0
        self.cnt[eng] += 1
        tok = (self.sem[eng], self.cnt[eng])
        self.q[eng].append(('o', fn, tok[0]))
        self._mark(tok, reads, writes)

    def dma(self, eng, fn, reads=(), writes=(), stream='d', depth=2):
        self._deps(eng, reads, writes)
        st = self.streams.setdefault(stream, {'n': 0, 'slots': []})
        i = st['n'] % depth; st['n'] += 1
        if i >= len(st['slots']):
            st['slots'].append([self._newsem("d_" + stream), 0])
        slot = st['slots'][i]
        if slot[1] > 0:
            self._wait(eng, (slot[0], slot[1]))
        if slot[1] >= 60000:
            raise RuntimeError("dma sem overflow " + stream)
        slot[1] += 16
        tok = (slot[0], slot[1])
        self.q[eng].append(('d', fn, tok[0]))
        self._mark(tok, reads, writes)

    def barrier(self):
        toks = list(self.allsems.values())
        for e in ENGS:
            for t in toks:
                self._wait(e, t)

    def run(self, block):
        def mk(name):
            items = self.q[name]

            def f(e):
                for it in items:
                    if it[0] == 'w':
                        e.wait_ge(it[1], it[2])
                    elif it[0] == 'o':
                        it[1](e).then_inc(it[2], 1)
                    else:
                        it[1](e).then_inc(it[2], 16)
            return f
        block.tensor(mk('pe')); block.vector(mk('dve')); block.scalar(mk('act'))
        block.gpsimd(mk('pool')); block.sync(mk('sp'))


def col_layout(v):
    v = np.asarray(v, np.float32).reshape(-1, 128)
    return np.ascontiguousarray(v.T)


class ColPack:
    def __init__(self):
        self.blocks = []; self.off = {}; self.n = 0

    def add(self, name, arr):
        arr = np.asarray(arr, np.float32)
        assert arr.shape[0] == 128
        arr = arr.reshape(128, -1)
        self.off[name] = self.n; self.blocks.append(arr); self.n += arr.shape[1]

    def build(self):
        return np.ascontiguousarray(np.concatenate(self.blocks, axis=1))


def pad128(a):
    out = np.zeros((128,) + a.shape[1:], a.dtype); out[:a.shape[0]] = a; return out


def dft_tables():
    f = np.arange(128)
    r = np.arange(64)
    th = 2 * np.pi * np.outer(r, f) / 128.0
    T1d = np.concatenate([np.cos(th), np.sin(th)], 1)
    T2ad = T1d.copy()
    T2bd = np.concatenate([-np.sin(th), np.cos(th)], 1)
    dk = np.arange(127) - 63
    thk = 2 * np.pi * np.outer(dk, f) / 128.0
    T1k = np.concatenate([np.cos(thk), np.sin(thk)], 1)
    T2ak = T1k.copy()
    T2bk = np.concatenate([-np.sin(thk), np.cos(thk)], 1)
    thi = 2 * np.pi * np.outer(f, r) / 128.0
    I2a = np.concatenate([np.cos(thi), np.sin(thi)], 1)
    I2b = np.concatenate([np.sin(thi), -np.cos(thi)], 1)
    I1a = np.cos(thi) / 16384.0
    I1b = -np.sin(thi) / 16384.0
    blocks = [pad128(T1d), pad128(T2ad), pad128(T2bd), pad128(T1k), pad128(T2ak), pad128(T2bk), I2a, I2b, I1a, I1b,
              np.eye(128), np.ones((128, 128))]
    names = ['T1d', 'T2ad', 'T2bd', 'T1k', 'T2ak', 'T2bk', 'I2a', 'I2b', 'I1a', 'I1b', 'identb', 'onesb']
    off = {}; n = 0
    for nm, b in zip(names, blocks):
        off[nm] = n; n += b.shape[1]
    tab = np.concatenate(blocks, 1).astype(np.float32).astype(NPBF)
    return tab, off


def filter_consts():
    n = T
    pos = np.arange(n, dtype=np.float32)
    t = np.linspace(0.0, 1.0, n, dtype=np.float32)
    bands = np.linspace(1e-4, 15, 16, dtype=np.float32)
    ang = (np.float32(2.0 * math.pi) * pos / np.float32(n))[:, None] * bands[None, :]
    z = np.concatenate([t[:, None], np.cos(ang), -np.sin(ang)], axis=-1).astype(np.float32)
    zT = pad128(np.ascontiguousarray(z.T))
    trow = np.ascontiguousarray(np.broadcast_to(t[None, :], (128, n))).astype(np.float32)
    delta = np.abs(np.linspace(MIN_DECAY, MAX_DECAY, 2048, dtype=np.float32))
    return zT, trow, delta


def prep_inputs(inp):
    g = lambda k: np.asarray(inp[k])
    sh = {}
    w_mod = g('w_mod')[0]
    sh['wmod'] = np.ascontiguousarray(w_mod.reshape(32, 128, 192, 128).transpose(2, 1, 0, 3))
    w_in = g('w_in')[0]
    sh['win'] = np.ascontiguousarray(w_in.reshape(32, 128, 80, 128).transpose(2, 1, 0, 3))
    w_out = g('w_out')[0]
    sh['wout'] = np.ascontiguousarray(w_out.reshape(32, 128, 8, 512).transpose(2, 1, 0, 3))
    sh['wg'] = np.ascontiguousarray(g('w_exp_gate')[0].reshape(16, 32, 128, 16, 128).transpose(0, 3, 2, 1, 4))
    sh['wu'] = np.ascontiguousarray(g('w_exp_up')[0].reshape(16, 32, 128, 16, 128).transpose(0, 3, 2, 1, 4))
    sh['wd'] = np.ascontiguousarray(g('w_exp_down')[0].reshape(16, 16, 128, 8, 512).transpose(0, 3, 2, 1, 4))
    sh['wr'] = np.ascontiguousarray(g('w_router')[0].reshape(32, 128, 16).transpose(1, 0, 2))
    wa = g('lru_wa')[0]; wx = g('lru_wx')[0]
    lw = np.stack([wa[0], wa[1], wx[0], wx[1]], 0)
    sh['lruw'] = np.ascontiguousarray(lw.transpose(1, 2, 0, 3))
    cp = ColPack()
    cp.add('b_mod', col_layout(g('b_mod')[0]))
    cp.add('g_mix', col_layout(g('g_mix')[0]))
    cp.add('g_ffn', col_layout(g('g_ffn')[0]))
    cp.add('b_in', col_layout(g('b_in')[0]))
    cp.add('hy_conv_b', col_layout(g('hy_conv_b')[0]))
    hcw = g('hy_conv_w')[0]
    cp.add('hy_conv_w', np.stack([col_layout(hcw[k]) for k in range(3)], -1))
    cp.add('hy_bias', col_layout(g('hy_bias')[0]))
    lcw = g('lru_conv_w')[0]
    cp.add('lru_conv_w', np.stack([col_layout(lcw[k]) for k in range(4)], -1))
    cp.add('lru_conv_b', col_layout(g('lru_conv_b')[0]))
    for nm in ['lru_ba', 'lru_bx', 'lru_lambda']:
        a = g(nm)[0]
        cp.add(nm, np.stack([col_layout(a[0]), col_layout(a[1])], 1))
    zT, trow, delta = filter_consts()
    cp.add('delta', col_layout(delta))
    for nm in ['hy_f_freq', 'hy_f_b1', 'hy_f_b2', 'hy_f_b3']:
        cp.add(nm, pad128(g(nm)[0].reshape(64, 1)))
    sh['pcols'] = cp.build()
    fw = np.zeros((128, 3, 64), np.float32)
    fw[:33, 0] = g('hy_f_w1')[0]; fw[:64, 1] = g('hy_f_w2')[0]; fw[:64, 2] = g('hy_f_w3')[0]
    sh['fw'] = fw
    sh['fwout'] = pad128(g('hy_f_wout')[0])
    sh['zT'] = zT; sh['trow'] = trow
    sh['rows3'] = np.ascontiguousarray(np.stack([
        np.broadcast_to(g('g_ffn')[0][None, :], (128, D)),
        np.broadcast_to(g('g_final')[None, :], (128, D)),
        np.broadcast_to(g('b_out')[0][None, :], (128, D))], 0)).astype(np.float32)
    tab, toff = dft_tables()
    sh['tabs'] = tab
    sh['cf32'] = np.ascontiguousarray(np.concatenate([np.eye(128), np.ones((128, 128))], 1)).astype(np.float32)
    x = g('x'); c = g('c'); ctx = g('ctx'); c_ctx = g('c_ctx')
    maps = []
    for b in range(NCORES):
        m = dict(sh)
        m['xT'] = np.ascontiguousarray(x[b].T)
        m['xtok'] = np.ascontiguousarray(x[b])
        m['ctxT'] = np.ascontiguousarray(ctx[b].T)
        m['ccol'] = np.ascontiguousarray(np.stack([col_layout(c[b]), col_layout(c_ctx)], -1))
        maps.append(m)
    return maps, cp.off, toff


def build(poff, toff, ntab, npc):
    nc = bass.Bass("TRN2", target_bir_lowering=False)
    dt_in = lambda name, shape, dt=F32: nc.dram_tensor(name, list(shape), dt, kind="ExternalInput").ap()
    xT = dt_in('xT', [D, T]); xtok = dt_in('xtok', [T, D]); ctxT = dt_in('ctxT', [D, NCTX])
    ccol_d = dt_in('ccol', [128, 32, 2])
    wmod = dt_in('wmod', [192, 128, 32, 128]); win = dt_in('win', [80, 128, 32, 128])
    wout = dt_in('wout', [8, 128, 32, 512])
    wg = dt_in('wg', [16, 16, 128, 32, 128]); wu = dt_in('wu', [16, 16, 128, 32, 128])
    wd = dt_in('wd', [16, 8, 128, 16, 512]); wr_d = dt_in('wr', [128, 32, 16])
    lruw_d = dt_in('lruw', [16, 128, 4, 128])
    pcols_d = dt_in('pcols', [128, npc]); fw_d = dt_in('fw', [128, 3, 64]); fwout_d = dt_in('fwout', [128, 4096])
    zT_d = dt_in('zT', [128, T]); trow_d = dt_in('trow', [128, T]); rows3_d = dt_in('rows3', [3, 128, D])
    tabs_d = dt_in('tabs', [128, ntab], BF16); cf32_d = dt_in('cf32', [128, 256])
    out_d = nc.dram_tensor('out', [T, D], F32, kind="ExternalOutput").ap()
    scr = lambda name, shape, dt: nc.dram_tensor(name, list(shape), dt, kind=("Internal" if STAGE >= 99 else "ExternalOutput")).ap()
    hxs = scr('hxs', [16, 128, 32 * 256], BF16)
    kfs = scr('kfs', [16, 128, 128 * 256], BF16)
    ysc = scr('ysc', [32, 128, T], BF16)
    acc = scr('acc', [8, T, 512], F32)
    hx2s = scr('hx2s', [T, D], BF16)
    dbg = {}
    if STAGE < 99:
        dbg['d_mod'] = nc.dram_tensor('d_mod', [128, 384], F32, kind="ExternalOutput").ap()
        dbg['d_a'] = nc.dram_tensor('d_a', [128, T], F32, kind="ExternalOutput").ap()
        dbg['d_b'] = nc.dram_tensor('d_b', [128, T], F32, kind="ExternalOutput").ap()
        dbg['d_c'] = nc.dram_tensor('d_c', [128, T], F32, kind="ExternalOutput").ap()
        dbg['d_d'] = nc.dram_tensor('d_d', [128, T], F32, kind="ExternalOutput").ap()
        dbg['d_kk'] = nc.dram_tensor('d_kk', [128, 8192], F32, kind="ExternalOutput").ap()
        dbg['d_dc'] = nc.dram_tensor('d_dc', [128, 512], F32, kind="ExternalOutput").ap()

    es = ExitStack()
    with es:
        E = es.enter_context
        em = Em(nc, es)
        AW = 50176
        arena = E(nc.sbuf_tensor("s_arena", [128, AW], F32))
        pc = E(nc.sbuf_tensor("s_pc", [128, npc], F32))
        dc = E(nc.sbuf_tensor("s_dc", [128, 512], F32))
        modc = E(nc.sbuf_tensor("s_modc", [128, 384], F32))
        tabs = E(nc.sbuf_tensor("s_tabs", [128, ntab], BF16))
        cf32 = E(nc.sbuf_tensor("s_cf32", [128, 256], F32))
        PSB = [E(nc.psum_tensor(f"ps{i}", [128, 512], F32)) for i in range(8)]
        PSR = [Reg(f"ps{i}") for i in range(8)]
        R_pc = Reg('pc'); R_dc = Reg('dc'); R_modc = Reg('modc'); R_tabs = Reg('tabs'); R_cf = Reg('cf32')
        identf = cf32[:, 0:128]; onesf = cf32[:, 128:256]
        TB = lambda nm, rows, c0, c1: tabs[0:rows, toff[nm] + c0: toff[nm] + c1]
        identb = TB('identb', 128, 0, 128); onesb = TB('onesb', 128, 0, 128)
        modc3 = modc[:, :].rearrange("p (j two) -> p j two", two=2)
        PC = lambda nm, i, n=1: pc[:, poff[nm] + i: poff[nm] + i + n]

        def carve(off, shape, dt=F32, parts=128):
            n = 1
            for s in shape:
                n *= s
            words = n if dt == F32 or dt == U32 or dt == I32 else (n + 1) // 2
            assert off + words <= AW, (off, words, AW)
            ap = arena[0:parts, off:off + words]
            if dt != F32:
                ap = ap.bitcast(dt)
            if len(shape) == 2:
                ap = ap.rearrange("p (a b) -> p a b", a=shape[0])
            elif len(shape) == 3:
                ap = ap.rearrange("p (a b c) -> p a b c", a=shape[0], b=shape[1])
            return ap

        em.dma('sp', lambda e: e.dma_start(out=pc[:, :], in_=pcols_d), writes=[R_pc], stream='su', depth=4)
        em.dma('sp', lambda e: e.dma_start(out=tabs[:, :], in_=tabs_d), writes=[R_tabs], stream='su', depth=4)
        em.dma('sp', lambda e: e.dma_start(out=cf32[:, :], in_=cf32_d), writes=[R_cf], stream='su', depth=4)

        scol = carve(0, [32, 2]); R_scol = Reg('scol')
        em.dma('sp', lambda e: e.dma_start(out=scol, in_=ccol_d), writes=[R_scol], stream='su', depth=4)
        em.op('act', lambda e: e.activation(out=scol, in_=scol, func=AF.Silu), reads=[R_scol], writes=[R_scol])
        wst = [carve(256 + i * 4096, [32, 128]) for i in range(3)]; R_wst = [Reg(f'wst{i}') for i in range(3)]
        for jc in range(192):
            b = jc % 3
            em.dma('sp' if jc % 2 == 0 else 'act', lambda e, b=b, jc=jc: e.dma_start(out=wst[b], in_=wmod[jc]),
                   writes=[R_wst[b]], stream='wm', depth=3)
            ps = PSB[jc % 2]
            for kc in range(KC):
                em.op('pe', lambda e, b=b, kc=kc, ps=ps: e.matmul(ps[:, 0:2], lhsT=wst[b][:, kc, :], rhs=scol[:, kc, :],
                                                                   start=(kc == 0), stop=(kc == KC - 1)),
                      reads=[R_wst[b], R_scol], writes=[PSR[jc % 2]])
            em.op('dve', lambda e, jc=jc, ps=ps: e.tensor_scalar(out=modc3[:, jc, :], in0=ps[:, 0:2], scalar1=PC('b_mod', jc),
                                                                  scalar2=None, op0=ALU.add),
                  reads=[PSR[jc % 2], R_pc], writes=[R_modc])
        MODX = lambda j0, kc: modc3[:, j0 + kc, 0:1]
        MODC = lambda j0, kc: modc3[:, j0 + kc, 1:2]
        em.op('dve', lambda e: e.scalar_tensor_tensor(out=dc[:, 0:32], in0=modc3[:, 32:64, 0], scalar=1.0, in1=PC('g_mix', 0, 32),
                                                      op0=ALU.add, op1=ALU.mult), reads=[R_modc, R_pc], writes=[R_dc])
        em.op('dve', lambda e: e.scalar_tensor_tensor(out=dc[:, 32:64], in0=modc3[:, 32:64, 1], scalar=1.0, in1=PC('g_mix', 0, 32),
                                                      op0=ALU.add, op1=ALU.mult), reads=[R_modc, R_pc], writes=[R_dc])
        em.op('dve', lambda e: e.scalar_tensor_tensor(out=dc[:, 64:96], in0=modc3[:, 128:160, 0], scalar=1.0, in1=PC('g_ffn', 0, 32),
                                                      op0=ALU.add, op1=ALU.mult), reads=[R_modc, R_pc], writes=[R_dc])
        em.op('act', lambda e: e.activation(out=dc[:, 96:128], in_=PC('lru_lambda', 0, 32), func=AF.Exp, scale=-1.0),
              reads=[R_pc], writes=[R_dc])
        em.op('act', lambda e: e.activation(out=dc[:, 96:128], in_=dc[:, 96:128], func=AF.Ln, bias=1.0),
              reads=[R_dc], writes=[R_dc])
        em.op('dve', lambda e: e.tensor_scalar(out=dc[:, 128:160], in0=dc[:, 96:128], scalar1=-8.0, scalar2=None, op0=ALU.mult),
              reads=[R_dc], writes=[R_dc])
        em.op('dve', lambda e: e.tensor_scalar(out=dc[:, 160:192], in0=dc[:, 96:128], scalar1=-16.0, scalar2=None, op0=ALU.mult),
              reads=[R_dc], writes=[R_dc])
        em.op('dve', lambda e: e.tensor_scalar(out=dc[:, 208:224], in0=PC('delta', 0, 16), scalar1=-1.0, scalar2=None, op0=ALU.mult),
              reads=[R_pc], writes=[R_dc])
        for i, nm in enumerate(['hy_f_b1', 'hy_f_b2', 'hy_f_b3']):
            em.op('dve', lambda e, i=i, nm=nm: e.tensor_tensor(out=dc[:, 224 + i:225 + i], in0=PC(nm, 0), in1=PC('hy_f_freq', 0),
                                                               op=ALU.mult), reads=[R_pc], writes=[R_dc])
        if STAGE < 99:
            em.dma('sp', lambda e: e.dma_start(out=dbg['d_mod'], in_=modc[:, :]), reads=[R_modc], stream='dbg', depth=2)
        em.barrier()
        if STAGE >= 2:
            phase2_onwards(nc, em, locals())
        em.barrier()
        block = E(nc.Block())
        em.run(block)
    return nc


def phase2_onwards(nc, em, L):
    globals().update({})
    carve = L['carve']; PSB = L['PSB']; PSR = L['PSR']; pc = L['pc']; dc = L['dc']; modc3 = L['modc3']
    R_pc = L['R_pc']; R_dc = L['R_dc']; R_modc = L['R_modc']; R_tabs = L['R_tabs']; R_cf = L['R_cf']
    PC = L['PC']; TB = L['TB']; identf = L['identf']; onesf = L['onesf']; identb = L['identb']; onesb = L['onesb']
    MODX = L['MODX']; MODC = L['MODC']; dbg = L['dbg']; poff = L['poff']
    xT = L['xT']; ctxT = L['ctxT']; hxs = L['hxs']; kfs = L['kfs']; ysc = L['ysc']; win = L['win']
    lruw_d = L['lruw_d']; fw_d = L['fw_d']; fwout_d = L['fwout_d']; zT_d = L['zT_d']; trow_d = L['trow_d']
    KB = 256

    HC_OFF = 180 * KB
    hc = carve(HC_OFF, [32, 256], BF16); R_hc = Reg('hc')
    xb = [carve(i * 32 * KB, [32, 256]) for i in range(2)]; R_xb = [Reg(f'xb{i}') for i in range(2)]
    sq = carve(64 * KB, [32, 256], BF16); R_sq = Reg('sq')
    hxo = [carve((80 + 16 * i) * KB, [32, 256], BF16) for i in range(2)]; R_hxo = [Reg(f'hxo{i}') for i in range(2)]
    rs = carve(112 * KB, [256]); R_rs = Reg('rs')
    xTv = xT.rearrange("(kc p) t -> p kc t", p=128)
    ctxTv = ctxT.rearrange("(kc p) t -> p kc t", p=128)
    for tt in range(17):
        b = tt % 2
        isctx = (tt == 16)
        src = ctxTv if isctx else xTv[:, :, tt * 256:(tt + 1) * 256]
        em.dma('sp', lambda e, b=b, src=src: e.dma_start(out=xb[b], in_=src), writes=[R_xb[b]], stream='xl', depth=2)
        em.op('act', lambda e, b=b: e.activation(out=sq, in_=xb[b], func=AF.Square), reads=[R_xb[b]], writes=[R_sq])
        ps = PSB[tt % 2]; psr = PSR[tt % 2]
        for kc in range(KC):
            em.op('pe', lambda e, kc=kc, ps=ps: e.matmul(ps[:, 0:256], lhsT=onesb, rhs=sq[:, kc, :], start=(kc == 0), stop=(kc == KC - 1)),
                  reads=[R_sq, R_tabs], writes=[psr])
        em.op('act', lambda e, ps=ps: e.activation(out=rs, in_=ps[:, 0:256], func=AF.Sqrt, scale=1.0 / D, bias=EPS),
              reads=[psr], writes=[R_rs])
        em.op('dve', lambda e: e.reciprocal(out=rs, in_=rs), reads=[R_rs], writes=[R_rs])
        em.op('dve', lambda e, b=b: e.tensor_tensor(out=xb[b], in0=xb[b], in1=rs[:, None, :].broadcast_to([128, 32, 256]), op=ALU.mult),
              reads=[R_xb[b], R_rs], writes=[R_xb[b]])
        dst = hc if isctx else hxo[b]; R_dst = R_hc if isctx else R_hxo[b]
        for kc in range(KC):
            sc_ap = dc[:, 32 + kc:33 + kc] if isctx else dc[:, kc:kc + 1]
            bi_ap = MODC(0, kc) if isctx else MODX(0, kc)
            em.op('act', lambda e, b=b, kc=kc, dst=dst, sc_ap=sc_ap, bi_ap=bi_ap: e.activation(
                out=dst[:, kc, :], in_=xb[b][:, kc, :], func=AF.Identity, scale=sc_ap, bias=bi_ap),
                reads=[R_xb[b], R_dc, R_modc], writes=[R_dst])
        if not isctx:
            em.dma('pool', lambda e, b=b, tt=tt: e.dma_start(out=hxs[tt].rearrange("p (a b) -> p a b", a=32), in_=hxo[b]),
                   reads=[R_hxo[b]], stream='hxst', depth=2)
    em.barrier()
    if STAGE < 3:
        return
    if not os.environ.get('KSKIPF'):
        phase_filter(nc, em, L, locals())
    if STAGE < 4:
        return
    phase_groups(nc, em, L, locals())
    if STAGE < 7:
        return
    phase_out(nc, em, L, locals())
    if STAGE < 8:
        return
    phase_moe(nc, em, L, locals())


def copy_op(em, eng, out, in_, reads, writes):
    if eng == 'act':
        em.op('act', lambda e: e.activation(out=out, in_=in_, func=AF.Copy), reads=reads, writes=writes)
    else:
        em.op(eng, lambda e: e.tensor_copy(out=out, in_=in_), reads=reads, writes=writes)


def sin_act(em, pre, R_pre, tmp, R_tmp, n):
    em.op('dve', lambda e: e.tensor_scalar(out=tmp, in0=pre, scalar1=1.0 / TWO_PI, scalar2=MAGIC, op0=ALU.mult, op1=ALU.add),
          reads=[R_pre], writes=[R_tmp])
    em.op('dve', lambda e: e.tensor_scalar(out=tmp, in0=tmp, scalar1=-MAGIC, scalar2=-TWO_PI, op0=ALU.add, op1=ALU.mult),
          reads=[R_tmp], writes=[R_tmp])
    em.op('dve', lambda e: e.tensor_tensor(out=pre, in0=pre, in1=tmp, op=ALU.add), reads=[R_pre, R_tmp], writes=[R_pre])
    em.op('dve', lambda e: e.tensor_scalar(out=pre, in0=pre, scalar1=-3.1415925, scalar2=3.1415925, op0=ALU.max, op1=ALU.min),
          reads=[R_pre], writes=[R_pre])
    em.op('act', lambda e: e.activation(out=pre, in_=pre, func=AF.Sin), reads=[R_pre], writes=[R_pre])


def phase_filter(nc, em, L, L2):
    carve = L['carve']; PSB = L['PSB']; PSR = L['PSR']; pc = L['pc']; dc = L['dc']
    R_pc = L['R_pc']; R_dc = L['R_dc']; R_tabs = L['R_tabs']; R_cf = L['R_cf']
    PC = L['PC']; TB = L['TB']; identf = L['identf']; dbg = L['dbg']
    kfs = L['kfs']; fw_d = L['fw_d']; fwout_d = L['fwout_d']; zT_d = L['zT_d']; trow_d = L['trow_d']
    KB = 256
    zT = carve(0, [T]); R_zT = Reg('zT')
    trow = carve(16 * KB, [T]); R_trow = Reg('trow')
    hA = carve(32 * KB, [T]); R_hA = Reg('hA')
    hB = carve(48 * KB, [T]); R_hB = Reg('hB')
    fwout = carve(64 * KB, [4096]); R_fwout = Reg('fwout')
    fw = carve(80 * KB, [3, 64]); R_fw = Reg('fw')
    kk = carve(82 * KB, [8192]); R_kk = Reg('kk')
    dec = [carve((114 + 2 * i) * KB, [512]) for i in range(2)]; R_dec = [Reg(f'dec{i}') for i in range(2)]
    Krm = carve(118 * KB, [128, 128], BF16); R_Krm = Reg('Krm')
    Zk = carve(150 * KB, [32, 256], BF16); R_Zk = Reg('Zk')
    Kfo = [carve((0 + 8 * i) * KB, [16, 256], BF16) for i in range(2)]; R_Kfo = [Reg(f'Kfo{i}') for i in range(2)]
    nrm = dc[:, 240:241]
    em.dma('sp', lambda e: e.dma_start(out=zT, in_=zT_d), writes=[R_zT], stream='fl', depth=4)
    em.dma('sp', lambda e: e.dma_start(out=trow, in_=trow_d), writes=[R_trow], stream='fl', depth=4)
    em.dma('sp', lambda e: e.dma_start(out=fwout, in_=fwout_d), writes=[R_fwout], stream='fl', depth=4)
    em.dma('sp', lambda e: e.dma_start(out=fw, in_=fw_d), writes=[R_fw], stream='fl', depth=4)
    srcs = [(zT, R_zT, 33), (hA, R_hA, 64), (hB, R_hB, 64)]
    dsts = [(hA, R_hA), (hB, R_hB), (hA, R_hA)]
    for layer in range(3):
        src, R_src, K = srcs[layer]; dst, R_d = dsts[layer]
        for tl in range(8):
            ps = PSB[tl % 2]; psr = PSR[tl % 2]
            em.op('pe', lambda e, src=src, K=K, tl=tl, ps=ps, layer=layer: e.matmul(
                ps[0:64, :], lhsT=fw[0:K, layer, :], rhs=src[0:K, tl * 512:(tl + 1) * 512], start=True, stop=True),
                reads=[R_fw, R_src], writes=[psr])
            em.op('act', lambda e, dst=dst, tl=tl, ps=ps, layer=layer: e.activation(
                out=dst[0:64, tl * 512:(tl + 1) * 512], in_=ps[0:64, :], func=AF.Identity,
                scale=PC('hy_f_freq', 0)[0:64], bias=dc[0:64, 224 + layer:225 + layer]),
                reads=[psr, R_pc, R_dc], writes=[R_d])
        tmp = carve(118 * KB, [T]); R_tmp = R_Krm
        sin_act(em, dst[0:64, :], R_d, tmp[0:64, :], R_tmp, T)
    h3 = hA; R_h3 = R_hA
    em.barrier()
    for cc in range(16):
        for tl in range(8):
            db = tl % 2
            em.op('act', lambda e, db=db, tl=tl, cc=cc: e.activation(out=dec[db], in_=trow[:, tl * 512:(tl + 1) * 512], func=AF.Exp,
                                                                    scale=dc[:, 208 + cc:209 + cc]),
                  reads=[R_trow, R_dc], writes=[R_dec[db]])
            for dr in range(2):
                pi = (tl * 2 + dr) % 4; ps = PSB[pi]; psr = PSR[pi]
                c0 = dr * 2048 + cc * 128
                em.op('pe', lambda e, c0=c0, tl=tl, ps=ps: e.matmul(ps[:, :], lhsT=fwout[0:64, c0:c0 + 128],
                                                                    rhs=h3[0:64, tl * 512:(tl + 1) * 512], start=True, stop=True),
                      reads=[R_fwout, R_h3], writes=[psr])
                if dr == 0:
                    em.op('dve', lambda e, tl=tl, ps=ps, db=db: e.tensor_tensor(out=kk[:, 4096 + tl * 512: 4096 + (tl + 1) * 512],
                                                                               in0=ps[:, :], in1=dec[db], op=ALU.mult),
                          reads=[psr, R_dec[db]], writes=[R_kk])
                else:
                    if tl == 0:
                        em.op('dve', lambda e, ps=ps, db=db: e.tensor_tensor(out=kk[:, 0:1], in0=ps[:, 0:1], in1=dec[db][:, 0:1], op=ALU.mult),
                              reads=[psr, R_dec[db]], writes=[R_kk])
                        em.op('dve', lambda e, ps=ps, db=db: e.tensor_tensor(out=kk[:, 3585:4096][:, ::-1], in0=ps[:, 1:512],
                                                                           in1=dec[db][:, 1:512], op=ALU.mult),
                              reads=[psr, R_dec[db]], writes=[R_kk])
                    else:
                        lo = 4096 - tl * 512 - 511
                        em.op('dve', lambda e, ps=ps, db=db, lo=lo: e.tensor_tensor(out=kk[:, lo:lo + 512][:, ::-1], in0=ps[:, :],
                                                                                  in1=dec[db], op=ALU.mult),
                              reads=[psr, R_dec[db]], writes=[R_kk])
        em.op('dve', lambda e: e.tensor_reduce(out=nrm, in_=kk, axis=AX.X, op=ALU.add, apply_absolute_value=True),
              reads=[R_kk], writes=[R_dc])
        em.op('dve', lambda e, cc=cc: e.reciprocal(out=dc[:, 192 + cc:193 + cc], in_=nrm), reads=[R_dc], writes=[R_dc])
        if STAGE < 99 and cc == 0:
            em.dma('sp', lambda e: e.dma_start(out=dbg['d_kk'], in_=kk), reads=[R_kk], stream='dbg', depth=2)
        for e4 in range(32):
            ps = PSB[4 + e4 % 2]; psr = PSR[4 + e4 % 2]
            ne = 4 if e4 < 31 else 3
            for j in range(ne):
                ee = e4 * 4 + j
                em.op('pe', lambda e, ee=ee, j=j, ps=ps: e.transpose(out=ps[0:127, j * 128:(j + 1) * 128],
                                                                     in_=kk[:, 1 + ee: 1 + ee + 64 * 126 + 1: 64], identity=identf),
                      reads=[R_kk, R_cf], writes=[psr])
            copy_op(em, 'act' if e4 % 2 == 0 else 'dve', Krm[0:127, :, e4 * 4:e4 * 4 + ne],
                    ps[0:127, 0:ne * 128].rearrange("p (j c) -> p c j", j=ne), [psr], [R_Krm])
        for cb in range(4):
            for c2 in range(16):
                ps = PSB[c2 % 2]; psr = PSR[c2 % 2]
                for j in range(2):
                    c = cb * 32 + c2 * 2 + j
                    em.op('pe', lambda e, c=c, j=j, ps=ps: e.matmul(ps[0:127, j * 256:(j + 1) * 256], lhsT=Krm[0:127, c, 0:127],
                                                                    rhs=TB('T1k', 127, 0, 256), start=True, stop=True),
                          reads=[R_Krm, R_tabs], writes=[psr])
                em.op('act', lambda e, c2=c2, ps=ps: e.activation(out=Zk[0:127, c2 * 2:c2 * 2 + 2, :],
                                                                  in_=ps[0:127, :].rearrange("p (j f) -> p j f", j=2), func=AF.Copy),
                      reads=[psr], writes=[R_Zk])
            for c2 in range(16):
                ps = PSB[2 + c2 % 2]; psr = PSR[2 + c2 % 2]
                kb = c2 // 8
                for j in range(2):
                    cs = c2 * 2 + j
                    em.op('pe', lambda e, cs=cs, j=j, ps=ps: e.matmul(ps[:, j * 256:(j + 1) * 256], lhsT=Zk[0:127, cs, 0:128],
                                                                      rhs=TB('T2ak', 127, 0, 256), start=True, stop=False),
                          reads=[R_Zk, R_tabs], writes=[psr])
                    em.op('pe', lambda e, cs=cs, j=j, ps=ps: e.matmul(ps[:, j * 256:(j + 1) * 256], lhsT=Zk[0:127, cs, 128:256],
                                                                      rhs=TB('T2bk', 127, 0, 256), start=False, stop=True),
                          reads=[R_Zk, R_tabs], writes=[psr])
                em.op('dve', lambda e, c2=c2, kb=kb, ps=ps: e.tensor_copy(out=Kfo[kb][:, (c2 % 8) * 2:(c2 % 8) * 2 + 2, :],
                                                                          in_=ps[:, :].rearrange("p (j f) -> p j f", j=2)),
                      reads=[psr], writes=[R_Kfo[kb]])
                if c2 % 8 == 7:
                    c0 = cb * 32 + kb * 16
                    em.dma('sp', lambda e, kb=kb, cc=cc, c0=c0: e.dma_start(
                        out=kfs[cc][:, c0 * 256:(c0 + 16) * 256].rearrange("p (a b) -> p a b", a=16), in_=Kfo[kb]),
                        reads=[R_Kfo[kb]], stream='kfst', depth=2)
    em.barrier()


def phase_groups(nc, em, L, L2):
    carve = L['carve']; PSB = L['PSB']; PSR = L['PSR']; pc = L['pc']; dc = L['dc']
    R_pc = L['R_pc']; R_dc = L['R_dc']; R_modc = L['R_modc']; R_tabs = L['R_tabs']; R_cf = L['R_cf']
    PC = L['PC']; TB = L['TB']; identf = L['identf']; dbg = L['dbg']; poff = L['poff']
    hxs = L['hxs']; kfs = L['kfs']; ysc = L['ysc']; win = L['win']; lruw_d = L['lruw_d']
    hc = L2['hc']; R_hc = L2['R_hc']
    KB = 256
    NG = int(os.environ.get('KGROUPS', '16')); KSUB = int(os.environ.get('KSUB', '9'))
    U = carve(0, [T]); X0C = carve(16 * KB, [T]); XC = carve(32 * KB, [T]); GG = carve(48 * KB, [T])
    R_U = Reg('U'); R_X0C = Reg('X0C'); R_XC = Reg('XC'); R_GG = Reg('GG')
    wstg = [carve((64 + 16 * i) * KB, [32, 128]) for i in range(1)]; R_wstg = [Reg('wstg0')]
    wbf = carve(80 * KB, [5, 32, 128], BF16)
    wbf = wbf
    R_wbf = [Reg(f'wbf{i}') for i in range(5)]
    hxt = [carve((120 + 16 * i) * KB, [32, 256], BF16) for i in range(2)]; R_hxt = [Reg(f'hxt{i}') for i in range(2)]
    ptmp = [carve((152 + 5 * i) * KB, [5, 256]) for i in range(2)]; R_ptmp = [[Reg(f'pt{i}_{j}') for j in range(5)] for i in range(2)]
    lwst = carve(162 * KB, [4, 128]); R_lwst = Reg('lwst')
    lwb = carve(164 * KB, [4, 128], BF16); R_lwb = Reg('lwb')
    cst = 165 * KB
    ctmp = [carve(cst + i * 256, [256]) for i in range(10)]; R_ctmp = [Reg(f'ct{i}') for i in range(10)]
    cxb = carve(cst + 10 * 256, [256], BF16); R_cxb = Reg('cxb')
    ct2 = [carve(176 * KB + i * 256, [256]) for i in range(4)]; R_ct2 = [Reg(f'ct2_{i}') for i in range(4)]
    xcb = carve(64 * KB, [T], BF16); R_xcb = Reg('xcb')
    LA = carve(72 * KB, [T]); LB = carve(88 * KB, [T]); LT = carve(104 * KB, [T]); HF = carve(120 * KB, [T]); HB = carve(136 * KB, [T])
    R_LA = Reg('LA'); R_LB = Reg('LB'); R_LT = Reg('LT'); R_HF = Reg('HF'); R_HB = Reg('HB')
    ylru = carve(152 * KB, [T], BF16); R_ylru = Reg('ylru')
    Urm = carve(64 * KB, [128, 64], BF16, parts=64); R_Urm = Reg('Urm')
    Zs = [carve((80 + 8 * i) * KB, [16, 256], BF16, parts=64) for i in range(2)]; R_Zs = [Reg(f'Zs{i}') for i in range(2)]
    Kfs = [carve((96 + 8 * i) * KB, [16, 256], BF16) for i in range(2)]; R_Kfs = [Reg(f'Kfs{i}') for i in range(2)]
    Yab = [carve(112 * KB + i * 256, [2, 2, 128], BF16) for i in range(4)]; R_Yab = [Reg(f'Yab{i}') for i in range(4)]
    prod = [carve(116 * KB + i * 1024, [2, 512]) for i in range(2)]; R_prod = [Reg(f'prod{i}') for i in range(2)]
    GH = carve(124 * KB, [2 * 64, 128], BF16); R_GH = Reg('GH')
    GH4 = GH.rearrange("p (g r) c -> p g r c", g=2)
    yhy = carve(156 * KB, [T], BF16); R_yhy = Reg('yhy')
    t1b = [carve((80 + 8 * i) * KB, [512]) for i in range(2)]; R_t1b = R_Zs
    h0 = lambda d: dc[:, 230 + d:231 + d]

    def lru_coeffs(n, xc_ap, xcb_ap, g, d, A, R_A, Bq, R_B, TMP, R_TMP, R_xc, R_xcbr, tile):
        nt = n // tile
        for which, dstb, R_dst in ((0, A, R_A), (1, TMP, R_TMP)):
            for tl in range(nt):
                ps = PSB[tl % 2]; psr = PSR[tl % 2]
                sl = slice(tl * tile, (tl + 1) * tile)
                em.op('pe', lambda e, ps=ps, sl=sl, which=which: e.matmul(ps[:, 0:tile], lhsT=lwb[:, which * 2 + d, :], rhs=xcb_ap[:, sl],
                                                                        start=True, stop=True), reads=[R_lwb, R_xcbr], writes=[psr])
                bcol = PC('lru_ba' if which == 0 else 'lru_bx', d * 16 + g)
                em.op('act', lambda e, ps=ps, sl=sl, dstb=dstb, bcol=bcol: e.activation(out=dstb[:, sl], in_=ps[:, 0:tile], func=AF.Sigmoid, bias=bcol),
                      reads=[psr, R_pc], writes=[R_dst])
        em.op('dve', lambda e: e.tensor_tensor(out=Bq, in0=TMP, in1=xc_ap, op=ALU.mult), reads=[R_TMP, R_xc], writes=[R_B])
        em.op('act', lambda e: e.activation(out=TMP, in_=A, func=AF.Exp, scale=dc[:, 160 + d * 16 + g:161 + d * 16 + g]),
              reads=[R_A, R_dc], writes=[R_TMP])
        em.op('act', lambda e: e.activation(out=TMP, in_=TMP, func=AF.Sqrt, scale=-1.0, bias=1.0), reads=[R_TMP], writes=[R_TMP])
        em.op('dve', lambda e: e.tensor_tensor(out=Bq, in0=Bq, in1=TMP, op=ALU.mult), reads=[R_TMP, R_B], writes=[R_B])
        em.op('act', lambda e: e.activation(out=A, in_=A, func=AF.Exp, scale=dc[:, 128 + d * 16 + g:129 + d * 16 + g]),
              reads=[R_A, R_dc], writes=[R_A])

    for g in range(NG):
        jl = [g, 16 + g, 32 + g, 48 + g, 64 + g]
        for i, jc in enumerate(jl):
            em.dma('sp', lambda e, jc=jc: e.dma_start(out=wstg[0], in_=win[jc]), writes=[R_wstg[0]], stream='wi', depth=2)
            copy_op(em, 'act' if i % 2 == 0 else 'pool', wbf[:, i], wstg[0], [R_wstg[0]], [R_wbf[i]])
        em.dma('sp', lambda e, g=g: e.dma_start(out=lwst, in_=lruw_d[g]), writes=[R_lwst], stream='wi', depth=2)
        copy_op(em, 'dve', lwb, lwst, [R_lwst], [R_lwb])
        ps = PSB[7]; psr = PSR[7]
        for kc in range(KC):
            em.op('pe', lambda e, kc=kc, ps=ps: e.matmul(ps[:, 0:256], lhsT=wbf[:, 4, kc, :], rhs=hc[:, kc, :], start=(kc == 0), stop=(kc == KC - 1)),
                  reads=[R_wbf[4], R_hc], writes=[psr])
        pcl, xcc, cA, cB, cT, cH = ctmp[0], ctmp[1], ctmp[2], ctmp[3], ctmp[4], ctmp[5]
        em.op('act', lambda e, ps=ps, g=g: e.activation(out=pcl, in_=ps[:, 0:256], func=AF.Identity, bias=PC('b_in', 64 + g)),
              reads=[psr, R_pc], writes=[R_ctmp[0]])
        LW = lambda k, g=g: PC('lru_conv_w', g * 4 + k)
        em.op('dve', lambda e, LW=LW, g=g: e.tensor_scalar(out=xcc, in0=pcl, scalar1=LW(2), scalar2=PC('lru_conv_b', g), op0=ALU.mult, op1=ALU.add),
              reads=[R_ctmp[0], R_pc], writes=[R_ctmp[1]])
        for k, (so, si) in ((0, (slice(2, 256), slice(0, 254))), (1, (slice(1, 256), slice(0, 255))), (3, (slice(0, 255), slice(1, 256)))):
            em.op('dve', lambda e, LW=LW, k=k, so=so, si=si: e.scalar_tensor_tensor(out=xcc[:, so], in0=pcl[:, si], scalar=LW(k), in1=xcc[:, so],
                                                                           op0=ALU.mult, op1=ALU.add),
                  reads=[R_ctmp[0], R_ctmp[1], R_pc], writes=[R_ctmp[1]])
        copy_op(em, 'act', cxb, xcc, [R_ctmp[1]], [R_cxb])
        for d in range(2):
            lru_coeffs(256, xcc, cxb, g, d, cA, R_ctmp[2], cB, R_ctmp[3], cT, R_ctmp[4], R_ctmp[1], R_cxb, 256)
            if d == 0:
                em.op('dve', lambda e: e.tensor_tensor_scan(out=cH, data0=cA, data1=cB, initial=0.0, op0=ALU.mult, op1=ALU.add),
                      reads=[R_ctmp[2], R_ctmp[3]], writes=[R_ctmp[5]])
                em.op('dve', lambda e: e.tensor_copy(out=h0(0), in_=cH[:, 255:256]), reads=[R_ctmp[5]], writes=[R_dc])
            else:
                em.op('dve', lambda e: e.tensor_tensor_scan(out=cH[:, ::-1], data0=cA[:, ::-1], data1=cB[:, ::-1], initial=0.0,
                                                            op0=ALU.mult, op1=ALU.add), reads=[R_ctmp[2], R_ctmp[3]], writes=[R_ctmp[5]])
                em.op('dve', lambda e: e.tensor_copy(out=h0(1), in_=cH[:, 0:1]), reads=[R_ctmp[5]], writes=[R_dc])
        HW = lambda ch, k: PC('hy_conv_w', ch * 3 + k)
        for tt in range(16):
            hb = tt % 2; pb = tt % 2
            em.dma('sp', lambda e, hb=hb, tt=tt: e.dma_start(out=hxt[hb], in_=hxs[tt].rearrange("p (a b) -> p a b", a=32)),
                   writes=[R_hxt[hb]], stream='hxl', depth=2)
            tsl = slice(tt * 256, (tt + 1) * 256)
            for i in range(5):
                pi = (tt * 5 + i) % 4; ps = PSB[pi]; psr = PSR[pi]
                for kc in range(KC):
                    em.op('pe', lambda e, i=i, kc=kc, hb=hb, ps=ps: e.matmul(ps[:, 0:256], lhsT=wbf[:, i, kc, :], rhs=hxt[hb][:, kc, :],
                                                                           start=(kc == 0), stop=(kc == KC - 1)),
                          reads=[R_wbf[i], R_hxt[hb]], writes=[psr])
                em.op('act', lambda e, i=i, pb=pb, ps=ps, jc=jl[i]: e.activation(out=ptmp[pb][:, i, :], in_=ps[:, 0:256], func=AF.Identity,
                                                                                 bias=PC('b_in', jc)),
                      reads=[psr, R_pc], writes=[R_ptmp[pb][i]])
            v3 = lambda ap: ap.rearrange("p (r q) -> p r q", q=64)
            x1c, vcv = ct2[0], ct2[1]
            for i, (dst, R_dst) in enumerate(((X0C[:, tsl], R_X0C), (x1c, R_ct2[0]), (vcv, R_ct2[1]))):
                ch = i * 16 + g
                src = ptmp[pb][:, i, :]
                em.op('dve', lambda e, dst=dst, src=src, ch=ch: e.tensor_scalar(out=dst, in0=src, scalar1=HW(ch, 1), scalar2=PC('hy_conv_b', ch),
                                                                                 op0=ALU.mult, op1=ALU.add),
                      reads=[R_ptmp[pb][i], R_pc], writes=[R_dst])
                em.op('dve', lambda e, dst=dst, src=src, ch=ch: e.scalar_tensor_tensor(out=v3(dst)[:, :, 1:64], in0=v3(src)[:, :, 0:63], scalar=HW(ch, 0),
                                                                                        in1=v3(dst)[:, :, 1:64], op0=ALU.mult, op1=ALU.add),
                      reads=[R_ptmp[pb][i], R_pc, R_dst], writes=[R_dst])
                em.op('dve', lambda e, dst=dst, src=src, ch=ch: e.scalar_tensor_tensor(out=v3(dst)[:, :, 0:63], in0=v3(src)[:, :, 1:64], scalar=HW(ch, 2),
                                                                                        in1=v3(dst)[:, :, 0:63], op0=ALU.mult, op1=ALU.add),
                      reads=[R_ptmp[pb][i], R_pc, R_dst], writes=[R_dst])
            em.op('pool', lambda e, tsl=tsl: e.tensor_tensor(out=U[:, tsl], in0=x1c, in1=vcv, op=ALU.mult), reads=[R_ct2[0], R_ct2[1]], writes=[R_U])
            src = ptmp[pb][:, 4, :]; dst = XC[:, tsl]
            em.op('dve', lambda e, LW=LW, dst=dst, src=src, g=g: e.tensor_scalar(out=dst, in0=src, scalar1=LW(2), scalar2=PC('lru_conv_b', g),
                                                                           op0=ALU.mult, op1=ALU.add), reads=[R_ptmp[pb][4], R_pc], writes=[R_XC])
            for k, (so, si) in ((0, (slice(2, 64), slice(0, 62))), (1, (slice(1, 64), slice(0, 63))), (3, (slice(0, 63), slice(1, 64)))):
                em.op('dve', lambda e, LW=LW, dst=dst, src=src, k=k, so=so, si=si: e.scalar_tensor_tensor(
                    out=v3(dst)[:, :, so], in0=v3(src)[:, :, si], scalar=LW(k), in1=v3(dst)[:, :, so], op0=ALU.mult, op1=ALU.add),
                    reads=[R_ptmp[pb][4], R_pc, R_XC], writes=[R_XC])
            pg = ptmp[pb][:, 3, :]; gt = ct2[2]
            em.op('pool', lambda e, pg=pg: e.tensor_tensor(out=gt, in0=pg, in1=pg, op=ALU.mult), reads=[R_ptmp[pb][3]], writes=[R_ct2[2]])
            em.op('pool', lambda e: e.tensor_scalar(out=gt, in0=gt, scalar1=0.044715, scalar2=1.0, op0=ALU.mult, op1=ALU.add),
                  reads=[R_ct2[2]], writes=[R_ct2[2]])
            em.op('pool', lambda e, pg=pg: e.tensor_tensor(out=gt, in0=gt, in1=pg, op=ALU.mult), reads=[R_ct2[2], R_ptmp[pb][3]], writes=[R_ct2[2]])
            em.op('act', lambda e: e.activation(out=gt, in_=gt, func=AF.Sigmoid, scale=1.5957691216057308), reads=[R_ct2[2]], writes=[R_ct2[2]])
            em.op('pool', lambda e, pg=pg, tsl=tsl: e.tensor_tensor(out=GG[:, tsl], in0=gt, in1=pg, op=ALU.mult),
                  reads=[R_ct2[2], R_ptmp[pb][3]], writes=[R_GG])
        if STAGE < 5 and g == 0:
            for nm, ap, R_ in (('d_a', U, R_U), ('d_b', X0C, R_X0C), ('d_c', XC, R_XC), ('d_d', GG, R_GG)):
                em.dma('sp', lambda e, nm=nm, ap=ap: e.dma_start(out=dbg[nm], in_=ap), reads=[R_], stream='dbg', depth=2)
        if STAGE < 5:
            continue
        mix_regs = [R_xcb, R_LA, R_LB, R_LT, R_HF, R_HB, R_ylru, R_Urm, R_GH, R_yhy] + R_Zs + R_Kfs + R_Yab + R_prod + R_t1b
        inp_regs = R_wstg + R_wbf + R_hxt + [r for rr in R_ptmp for r in rr] + [R_lwst]
        em.barrier()
        copy_op(em, 'act', xcb, XC, [R_XC], [R_xcb])
        for d in range(2):
            H = HF if d == 0 else HB; R_H = R_HF if d == 0 else R_HB
            lru_coeffs(T, XC, xcb, g, d, LA, R_LA, LB, R_LB, LT, R_LT, R_XC, R_xcb, 512)
            if d == 0:
                em.op('dve', lambda e, H=H: e.tensor_tensor_scan(out=H, data0=LA, data1=LB, initial=h0(0), op0=ALU.mult, op1=ALU.add),
                      reads=[R_LA, R_LB, R_dc], writes=[R_H])
            else:
                em.op('dve', lambda e, H=H: e.tensor_tensor_scan(out=H[:, ::-1], data0=LA[:, ::-1], data1=LB[:, ::-1], initial=h0(1),
                                                                 op0=ALU.mult, op1=ALU.add), reads=[R_LA, R_LB, R_dc], writes=[R_H])
        if STAGE == 5 and g == 0:
            for nm, ap, R_ in (('d_a', HF, R_HF), ('d_b', HB, R_HB), ('d_c', LA, R_LA), ('d_d', LB, R_LB)):
                em.dma('sp', lambda e, nm=nm, ap=ap: e.dma_start(out=dbg[nm], in_=ap), reads=[R_], stream='dbg', depth=2)
            em.dma('sp', lambda e: e.dma_start(out=dbg['d_dc'], in_=dc[:, :]), reads=[R_dc], stream='dbg', depth=2)
        em.op('pool', lambda e: e.tensor_tensor(out=HF, in0=HF, in1=HB, op=ALU.add), reads=[R_HF, R_HB], writes=[R_HF])
        em.op('pool', lambda e: e.tensor_tensor(out=ylru, in0=HF, in1=GG, op=ALU.mult), reads=[R_HF, R_GG], writes=[R_ylru])
        em.dma('sp', lambda e, g=g: e.dma_start(out=ysc[16 + g], in_=ylru), reads=[R_ylru], stream='yst', depth=2)
        em.barrier()
        if STAGE < 6:
            continue
        for q4 in range(16):
            ps = PSB[q4 % 2]; psr = PSR[q4 % 2]
            for j in range(4):
                q = q4 * 4 + j
                em.op('pe', lambda e, q=q, j=j, ps=ps: e.transpose(out=ps[0:64, j * 128:(j + 1) * 128], in_=U[:, q:T:64], identity=identf),
                      reads=[R_U, R_cf], writes=[psr])
            copy_op(em, 'act' if q4 % 2 == 0 else 'dve', Urm[0:64, :, q4 * 4:q4 * 4 + 4],
                    ps[0:64, :].rearrange("p (j c) -> p c j", j=4), [psr], [R_Urm])
        pending = None

        def emit_i2(cb, c2, yb):
            ps2 = PSB[4 + (c2 // 2) % 2]; psr2 = PSR[4 + (c2 // 2) % 2]
            for j in range(2):
                col = ((c2 % 2) * 2 + j) * 128
                em.op('pe', lambda e, j=j, yb=yb, ps2=ps2, col=col: e.matmul(ps2[:, col:col + 128], lhsT=Yab[yb][:, j, 0, :], rhs=TB('I2a', 128, 0, 128),
                                                                             start=True, stop=False), reads=[R_Yab[yb], R_tabs], writes=[psr2])
                em.op('pe', lambda e, j=j, yb=yb, ps2=ps2, col=col: e.matmul(ps2[:, col:col + 128], lhsT=Yab[yb][:, j, 1, :], rhs=TB('I2b', 128, 0, 128),
                                                                             start=False, stop=True), reads=[R_Yab[yb], R_tabs], writes=[psr2])
            if c2 % 2 == 1:
                c0 = cb * 16 + (c2 - 1) * 2
                copy_op(em, 'act', GH4[:, :, :, c0:c0 + 4].rearrange("p g r c -> p c g r"),
                        ps2[:, :].rearrange("p (c g r) -> p c g r", c=4, g=2), [psr2], [R_GH])

        for cb in range(8):
            zb = cb % 2
            em.dma('sp', lambda e, zb=zb, cb=cb, g=g: e.dma_start(out=Kfs[zb], in_=kfs[g][:, cb * 4096:(cb + 1) * 4096].rearrange("p (a b) -> p a b", a=16)),
                   writes=[R_Kfs[zb]], stream='kfl', depth=2)
            for c2 in range(8):
                ps = PSB[2 + c2 % 2]; psr = PSR[2 + c2 % 2]
                for j in range(2):
                    c = cb * 16 + c2 * 2 + j
                    em.op('pe', lambda e, c=c, j=j, ps=ps: e.matmul(ps[0:64, j * 256:(j + 1) * 256], lhsT=Urm[0:64, c, :], rhs=TB('T1d', 64, 0, 256),
                                                                    start=True, stop=True), reads=[R_Urm, R_tabs], writes=[psr])
                copy_op(em, 'act', Zs[zb][0:64, c2 * 2:c2 * 2 + 2, :], ps[0:64, :].rearrange("p (j f) -> p j f", j=2), [psr], [R_Zs[zb]])
            for c2 in range(8):
                ps = PSB[c2 % 2]; psr = PSR[c2 % 2]
                yb = c2 % 4; pb2 = c2 % 2
                for j in range(2):
                    cs = c2 * 2 + j
                    em.op('pe', lambda e, cs=cs, j=j, ps=ps, zb=zb: e.matmul(ps[:, j * 256:(j + 1) * 256], lhsT=Zs[zb][0:64, cs, 0:128],
                                                                             rhs=TB('T2ad', 64, 0, 256), start=True, stop=False),
                          reads=[R_Zs[zb], R_tabs], writes=[psr])
                    em.op('pe', lambda e, cs=cs, j=j, ps=ps, zb=zb: e.matmul(ps[:, j * 256:(j + 1) * 256], lhsT=Zs[zb][0:64, cs, 128:256],
                                                                             rhs=TB('T2bd', 64, 0, 256), start=False, stop=True),
                          reads=[R_Zs[zb], R_tabs], writes=[psr])
                psv = ps[:, :].rearrange("p (c h f) -> p c h f", c=2, h=2)
                kv = Kfs[zb][:, c2 * 2:c2 * 2 + 2, :].rearrange("p c (h f) -> p c h f", h=2)
                pr0 = prod[pb2][:, 0, :].rearrange("p (c h f) -> p c h f", c=2, h=2)
                pr1 = prod[pb2][:, 1, :].rearrange("p (c h f) -> p c h f", c=2, h=2)
                em.op('dve', lambda e, psv=psv, kv=kv, pr0=pr0: e.tensor_tensor(out=pr0, in0=psv, in1=kv, op=ALU.mult),
                      reads=[psr, R_Kfs[zb]], writes=[R_prod[pb2]])
                em.op('dve', lambda e, psv=psv, kv=kv, pr1=pr1: e.tensor_tensor(out=pr1, in0=psv, in1=kv[:, :, ::-1, :], op=ALU.mult),
                      reads=[psr, R_Kfs[zb]], writes=[R_prod[pb2]])
                em.op('pool', lambda e, pr0=pr0, yb=yb: e.tensor_tensor(out=Yab[yb][:, :, 0, :], in0=pr0[:, :, 0, :], in1=pr0[:, :, 1, :], op=ALU.subtract),
                      reads=[R_prod[pb2]], writes=[R_Yab[yb]])
                em.op('pool', lambda e, pr1=pr1, yb=yb: e.tensor_tensor(out=Yab[yb][:, :, 1, :], in0=pr1[:, :, 0, :], in1=pr1[:, :, 1, :], op=ALU.add),
                      reads=[R_prod[pb2]], writes=[R_Yab[yb]])
                if pending is not None:
                    emit_i2(*pending)
                pending = (cb, c2, yb)
        emit_i2(*pending)
        if KSUB < 6:
            em.barrier(); continue
        for half in range(2):
            for r in range(32):
                rr = half * 32 + r
                ps = PSB[4 + r // 8]; psr = PSR[4 + r // 8]
                col = (r % 8) * 64
                em.op('pe', lambda e, rr=rr, ps=ps, col=col: e.matmul(ps[:, col:col + 64], lhsT=GH4[:, 0, rr, :], rhs=TB('I1a', 128, 0, 64),
                                                                      start=True, stop=False), reads=[R_GH, R_tabs], writes=[psr])
                em.op('pe', lambda e, rr=rr, ps=ps, col=col: e.matmul(ps[:, col:col + 64], lhsT=GH4[:, 1, rr, :], rhs=TB('I1b', 128, 0, 64),
                                                                      start=False, stop=True), reads=[R_GH, R_tabs], writes=[psr])
            for bk in range(4):
                ps = PSB[4 + bk]; psr = PSR[4 + bk]
                seg = slice(half * 2048 + bk * 512, half * 2048 + (bk + 1) * 512)
                tb = bk % 2
                em.op('act', lambda e, seg=seg, tb=tb, g=g: e.activation(out=t1b[tb], in_=U[:, seg], func=AF.Identity, scale=PC('hy_bias', g)),
                      reads=[R_U, R_pc], writes=[R_t1b[tb]])
                em.op('dve', lambda e, ps=ps, tb=tb, g=g: e.scalar_tensor_tensor(out=t1b[tb], in0=ps[:, :], scalar=dc[:, 192 + g:193 + g], in1=t1b[tb],
                                                                                 op0=ALU.mult, op1=ALU.add), reads=[psr, R_dc, R_t1b[tb]], writes=[R_t1b[tb]])
                em.op('pool', lambda e, seg=seg, tb=tb: e.tensor_tensor(out=yhy[:, seg], in0=t1b[tb], in1=X0C[:, seg], op=ALU.mult),
                      reads=[R_t1b[tb], R_X0C], writes=[R_yhy])
        em.dma('sp', lambda e, g=g: e.dma_start(out=ysc[g], in_=yhy), reads=[R_yhy], stream='yst', depth=2)
        em.barrier()


def build_row(em, L, dst, R_dst, colfn, ncols, tmpd, R_tmpd, reads):
    PSB = L['PSB']; PSR = L['PSR']; identf = L['identf']; onesf = L['onesf']; R_cf = L['R_cf']
    for k4 in range(ncols // 4):
        ps = PSB[6 + k4 % 2]; psr = PSR[6 + k4 % 2]
        for j in range(4):
            kc = k4 * 4 + j; tb = kc % 2
            em.op('dve', lambda e, kc=kc, tb=tb: e.tensor_scalar(out=tmpd[tb], in0=identf, scalar1=colfn(kc), scalar2=None, op0=ALU.mult),
                  reads=[R_cf] + reads, writes=[R_tmpd[tb]])
            em.op('pe', lambda e, j=j, tb=tb, ps=ps: e.matmul(ps[:, j * 128:(j + 1) * 128], lhsT=onesf, rhs=tmpd[tb], start=True, stop=True),
                  reads=[R_cf, R_tmpd[tb]], writes=[psr])
        copy_op(em, 'act', dst[:, k4 * 512:(k4 + 1) * 512], ps[:, :], [psr], [R_dst])


def phase_out(nc, em, L, L2):
    carve = L['carve']; PSB = L['PSB']; PSR = L['PSR']; dc = L['dc']
    R_dc = L['R_dc']; R_modc = L['R_modc']; MODX = L['MODX']
    ysc = L['ysc']; wout = L['wout']; acc = L['acc']; xtok = L['xtok']; rows3_d = L['rows3_d']
    KB = 256
    wstg = [carve(16 * i * KB, [8, 512]) for i in range(2)]; R_wstg = [Reg(f'cw{i}') for i in range(2)]
    wbf = carve(32 * KB, [32, 512], BF16); R_wbf = Reg('cwbf')
    yt = [carve((64 + 32 * i) * KB, [32, 512], BF16) for i in range(2)]; R_yt = [Reg(f'yt{i}') for i in range(2)]
    gt1r = carve(128 * KB, [D]); R_gt1r = Reg('gt1r')
    gbr = carve(144 * KB, [D]); R_gbr = Reg('gbr')
    xt = [carve((160 + 2 * i) * KB, [512]) for i in range(3)]; R_xt = [Reg(f'xt{i}') for i in range(3)]
    ot = [carve((166 + 2 * i) * KB, [512]) for i in range(3)]; R_ot = [Reg(f'ot{i}') for i in range(3)]
    tmpd = [carve(172 * KB + i * 128, [128]) for i in range(2)]; R_tmpd = [Reg(f'td{i}') for i in range(2)]
    brow = carve(176 * KB, [D]); R_brow = Reg('brow')
    em.dma('sp', lambda e: e.dma_start(out=brow, in_=rows3_d[2]), writes=[R_brow], stream='cl', depth=2)
    build_row(em, L, gt1r, R_gt1r, lambda kc: MODX(64, kc), 32, tmpd, R_tmpd, [R_modc])
    em.op('dve', lambda e: e.tensor_tensor(out=gbr, in0=gt1r, in1=brow, op=ALU.mult), reads=[R_gt1r, R_brow], writes=[R_gbr])
    yv = ysc.rearrange("k p t -> p k t")
    n = 0
    for ct in range(8):
        for q in range(4):
            sb = q % 2
            em.dma('sp', lambda e, sb=sb, ct=ct, q=q: e.dma_start(out=wstg[sb], in_=wout[ct][:, q * 8:(q + 1) * 8, :]), writes=[R_wstg[sb]],
                   stream='cw', depth=2)
            copy_op(em, 'act' if q % 2 == 0 else 'pool', wbf[:, q * 8:(q + 1) * 8, :], wstg[sb], [R_wstg[sb]], [R_wbf])
        csl = slice(ct * 512, (ct + 1) * 512)
        for t8 in range(8):
            yb = (ct * 8 + t8) % 2
            em.dma('sp', lambda e, yb=yb, t8=t8: e.dma_start(out=yt[yb], in_=yv[:, :, t8 * 512:(t8 + 1) * 512]), writes=[R_yt[yb]], stream='cy', depth=2)
            for sub in range(4):
                t0 = t8 * 512 + sub * 128
                pi = n % 4; ps = PSB[pi]; psr = PSR[pi]
                xb = n % 3; n += 1
                em.dma('act', lambda e, xb=xb, t0=t0, csl=csl: e.dma_start(out=xt[xb], in_=xtok[t0:t0 + 128, csl]), writes=[R_xt[xb]], stream='cx', depth=3)
                for kc in range(KC):
                    em.op('pe', lambda e, kc=kc, yb=yb, sub=sub, ps=ps: e.matmul(ps[:, :], lhsT=yt[yb][:, kc, sub * 128:(sub + 1) * 128], rhs=wbf[:, kc, :],
                                                                                 start=(kc == 0), stop=(kc == KC - 1)),
                          reads=[R_yt[yb], R_wbf], writes=[psr])
                em.op('dve', lambda e, ps=ps, xb=xb, csl=csl: e.tensor_tensor(out=ot[xb], in0=ps[:, :], in1=gt1r[:, csl], op=ALU.mult),
                      reads=[psr, R_gt1r], writes=[R_ot[xb]])
                em.op('pool', lambda e, xb=xb, csl=csl: e.tensor_tensor(out=xt[xb], in0=xt[xb], in1=gbr[:, csl], op=ALU.add),
                      reads=[R_xt[xb], R_gbr], writes=[R_xt[xb]])
                em.op('dve', lambda e, xb=xb: e.tensor_tensor(out=ot[xb], in0=ot[xb], in1=xt[xb], op=ALU.add),
                      reads=[R_ot[xb], R_xt[xb]], writes=[R_ot[xb]])
                em.dma('pool', lambda e, xb=xb, ct=ct, t0=t0: e.dma_start(out=acc[ct][t0:t0 + 128, :], in_=ot[xb]), reads=[R_ot[xb]], stream='co', depth=3)
    em.barrier()


def phase_moe(nc, em, L, L2):
    carve = L['carve']; PSB = L['PSB']; PSR = L['PSR']; dc = L['dc']; pc = L['pc']
    R_dc = L['R_dc']; R_modc = L['R_modc']; R_pc = L['R_pc']; R_tabs = L['R_tabs']; R_cf = L['R_cf']; MODX = L['MODX']
    identf = L['identf']; identb = L['identb']
    acc = L['acc']; hx2s = L['hx2s']; wr_d = L['wr_d']; wg = L['wg']; wu = L['wu']; wd = L['wd']; rows3_d = L['rows3_d']; out_d = L['out_d']
    dbg = L['dbg']
    KB = 256
    accv = acc.rearrange("c t j -> t c j")
    A2r = carve(0, [D]); R_A2r = Reg('A2r'); sh2r = carve(16 * KB, [D]); R_sh2r = Reg('sh2r')
    x1t = [carve((32 + 16 * i) * KB, [8, 512]) for i in range(2)]; R_x1t = [Reg(f'x1t{i}') for i in range(2)]
    junk = carve(64 * KB, [D], BF16); R_junk = Reg('junk')
    hx2b = [carve((72 + 8 * i) * KB, [D], BF16) for i in range(2)]; R_hx2b = [Reg(f'hx2b{i}') for i in range(2)]
    hx2T = carve(88 * KB, [32, 128], BF16); R_hx2T = Reg('hx2T')
    wrf = carve(96 * KB, [32, 16]); R_wrf = Reg('wrf'); wrb = carve(98 * KB, [32, 16], BF16); R_wrb = Reg('wrb')
    affT = carve(100 * KB, [T], parts=16); R_affT = Reg('affT')
    tmpd = [carve(116 * KB + i * 128, [128]) for i in range(2)]; R_tmpd = [Reg(f'td{i}') for i in range(2)]
    sm = carve(117 * KB, [64]); R_sm = Reg('sm')
    lg = carve(118 * KB, [16]); R_lg = Reg('lg'); aff = carve(118 * KB + 16, [16]); R_aff = Reg('aff')
    build_row(em, L, A2r, R_A2r, lambda kc: dc[:, 64 + kc:65 + kc], 32, tmpd, R_tmpd, [R_dc])
    build_row(em, L, sh2r, R_sh2r, lambda kc: MODX(96, kc), 32, tmpd, R_tmpd, [R_modc])
    em.dma('sp', lambda e: e.dma_start(out=wrf, in_=wr_d), writes=[R_wrf], stream='ml', depth=2)
    copy_op(em, 'dve', wrb, wrf, [R_wrf], [R_wrb])
    NT = int(os.environ.get('KTT', '32'))
    for tt in range(NT):
        xb = tt % 2; t0 = tt * 128
        em.dma('sp', lambda e, xb=xb, t0=t0: e.dma_start(out=x1t[xb], in_=accv[t0:t0 + 128]), writes=[R_x1t[xb]], stream='x1', depth=2)
        x1f = x1t[xb].rearrange("p a b -> p (a b)")
        em.op('act', lambda e, x1f=x1f: e.activation(out=junk, in_=x1f, func=AF.Square, accum_out=sm[:, 0:1]), reads=[R_x1t[xb]], writes=[R_junk, R_sm])
        em.op('act', lambda e: e.activation(out=sm[:, 1:2], in_=sm[:, 0:1], func=AF.Sqrt, scale=1.0 / D, bias=EPS), reads=[R_sm], writes=[R_sm])
        em.op('dve', lambda e: e.reciprocal(out=sm[:, 1:2], in_=sm[:, 1:2]), reads=[R_sm], writes=[R_sm])
        em.op('dve', lambda e, x1f=x1f: e.scalar_tensor_tensor(out=x1f, in0=x1f, scalar=sm[:, 1:2], in1=A2r, op0=ALU.mult, op1=ALU.mult),
              reads=[R_x1t[xb], R_sm, R_A2r], writes=[R_x1t[xb]])
        em.op('pool', lambda e, x1f=x1f, xb=xb: e.tensor_tensor(out=hx2b[xb], in0=x1f, in1=sh2r, op=ALU.add), reads=[R_x1t[xb], R_sh2r], writes=[R_hx2b[xb]])
        em.dma('pool', lambda e, xb=xb, t0=t0: e.dma_start(out=hx2s[t0:t0 + 128, :], in_=hx2b[xb]), reads=[R_hx2b[xb]], stream='h2', depth=2)
        for k8 in range(4):
            ps = PSB[k8 % 2]; psr = PSR[k8 % 2]
            psb = ps[:, :].bitcast(BF16)
            for j in range(8):
                kc = k8 * 8 + j
                em.op('pe', lambda e, kc=kc, j=j, xb=xb, psb=psb: e.transpose(out=psb[:, j * 128:(j + 1) * 128], in_=hx2b[xb][:, kc * 128:(kc + 1) * 128], identity=identb),
                      reads=[R_hx2b[xb], R_tabs], writes=[psr])
            copy_op(em, 'act' if k8 % 2 == 0 else 'dve', hx2T[:, k8 * 8:(k8 + 1) * 8, :], psb.rearrange("p (j t) -> p j t", j=8), [psr], [R_hx2T])
        ps = PSB[2]; psr = PSR[2]
        for kc in range(KC):
            em.op('pe', lambda e, kc=kc, ps=ps: e.matmul(ps[:, 0:16], lhsT=hx2T[:, kc, :], rhs=wrb[:, kc, :], start=(kc == 0), stop=(kc == KC - 1)),
                  reads=[R_hx2T, R_wrb], writes=[psr])
        copy_op(em, 'dve', lg, ps[:, 0:16], [psr], [R_lg])
        em.op('dve', lambda e: e.tensor_reduce(out=sm[:, 2:3], in_=lg, axis=AX.X, op=ALU.max, negate=True), reads=[R_lg], writes=[R_sm])
        em.op('act', lambda e: e.activation(out=aff, in_=lg, func=AF.Exp, bias=sm[:, 2:3], accum_out=sm[:, 3:4]), reads=[R_lg, R_sm], writes=[R_aff, R_sm])
        em.op('dve', lambda e: e.reciprocal(out=sm[:, 4:5], in_=sm[:, 3:4]), reads=[R_sm], writes=[R_sm])
        em.op('dve', lambda e: e.tensor_scalar(out=aff, in0=aff, scalar1=sm[:, 4:5], scalar2=None, op0=ALU.mult), reads=[R_aff, R_sm], writes=[R_aff])
        ps3 = PSB[3]; psr3 = PSR[3]
        em.op('pe', lambda e, ps3=ps3: e.transpose(out=ps3[0:16, 0:128], in_=aff, identity=identf), reads=[R_aff, R_cf], writes=[psr3])
        copy_op(em, 'act', affT[0:16, t0:t0 + 128], ps3[0:16, 0:128], [psr3], [R_affT])
    if STAGE < 99:
        em.dma('sp', lambda e: e.dma_start(out=dbg['d_a'][0:16, :], in_=affT), reads=[R_affT], stream='dbg', depth=2)
    em.barrier()
    if STAGE < 9:
        return
    work = carve(0, [T], parts=16); R_work = Reg('work')
    vals = carve(16 * KB, [CAP], parts=16); R_vals = Reg('vals')
    idxs = carve(18 * KB, [CAP], U32, parts=16); R_idxs = Reg('idxs')
    idxf = carve(20 * KB, [CAP], parts=16); R_idxf = Reg('idxf')
    IDT_OFF = 176 * KB
    idxT = carve(IDT_OFF, [64], U32); R_idxT = Reg('idxT')
    gateT = carve(IDT_OFF + 64, [64]); R_gateT = Reg('gateT')
    idxTf = carve(IDT_OFF + 128, [64]); R_idxTf = Reg('idxTf')
    copy_op(em, 'dve', work, affT, [R_affT], [R_work])
    for it in range(CAP // 8):
        sl = slice(it * 8, it * 8 + 8)
        em.op('dve', lambda e, sl=sl: e.max(out=vals[:, sl], in_=work), reads=[R_work], writes=[R_vals])
        em.op('dve', lambda e, sl=sl: e.max_index(out=idxs[:, sl], in_max=vals[:, sl], in_values=work), reads=[R_work, R_vals], writes=[R_idxs])
        em.op('dve', lambda e, sl=sl: e.match_replace(out=work, in_to_replace=vals[:, sl], in_values=work, imm_value=-1.0),
              reads=[R_work, R_vals], writes=[R_work])
    copy_op(em, 'dve', idxf, idxs, [R_idxs], [R_idxf])
    for st in range(4):
        ps = PSB[st % 2]; psr = PSR[st % 2]
        em.op('pe', lambda e, st=st, ps=ps: e.transpose(out=ps[:, 0:16], in_=idxf[0:16, st * 128:(st + 1) * 128], identity=identf[0:16, 0:16]),
              reads=[R_idxf, R_cf], writes=[psr])
        em.op('pe', lambda e, st=st, ps=ps: e.transpose(out=ps[:, 16:32], in_=vals[0:16, st * 128:(st + 1) * 128], identity=identf[0:16, 0:16]),
              reads=[R_vals, R_cf], writes=[psr])
        copy_op(em, 'dve', idxTf[:, st * 16:(st + 1) * 16], ps[:, 0:16], [psr], [R_idxTf])
        copy_op(em, 'dve', gateT[:, st * 16:(st + 1) * 16], ps[:, 16:32], [psr], [R_gateT])
    copy_op(em, 'dve', idxT, idxTf, [R_idxTf], [R_idxT])
    if STAGE < 99:
        em.dma('sp', lambda e: e.dma_start(out=dbg['d_b'][0:16, 0:512], in_=idxf), reads=[R_idxf], stream='dbg', depth=2)
        em.dma('sp', lambda e: e.dma_start(out=dbg['d_b'][16:32, 0:512], in_=vals), reads=[R_vals], stream='dbg', depth=2)
    em.barrier()
    if STAGE < 10:
        return
    XT = carve(0, [32, 512], BF16); R_XT = Reg('XT')
    actT = carve(32 * KB, [16, 512], BF16); R_actT = Reg('actT')
    wst = [carve((48 + 16 * i) * KB, [32, 128]) for i in range(2)]; R_wst = [Reg(f'mw{i}') for i in range(2)]
    wgb = [carve((80 + 8 * i) * KB, [32, 128], BF16) for i in range(2)]; R_wgb = [Reg(f'wgb{i}') for i in range(2)]
    wub = [carve((96 + 8 * i) * KB, [32, 128], BF16) for i in range(2)]; R_wub = [Reg(f'wub{i}') for i in range(2)]
    wdb = [carve((112 + 16 * i) * KB, [16, 512], BF16) for i in range(2)]; R_wdb = [Reg(f'wdb{i}') for i in range(2)]
    Xg = carve(112 * KB, [4, D], BF16)
    R_Xg = [R_wdb[0], R_wdb[0], R_wdb[1], R_wdb[1]]
    stmp = [carve((144 + 2 * i) * KB, [512]) for i in range(2)]; R_stmp = [Reg(f'st{i}') for i in range(2)]
    yo = [carve((148 + 2 * i) * KB, [512]) for i in range(4)]; R_yo = [Reg(f'yo{i}') for i in range(4)]
    gt2r = carve(156 * KB, [D]); R_gt2r = Reg('gt2r')
    tmpd2 = [carve(180 * KB + i * 128, [128]) for i in range(2)]; R_tmpd2 = [Reg(f'td2{i}') for i in range(2)]
    build_row(em, L, gt2r, R_gt2r, lambda kc: MODX(160, kc), 32, tmpd2, R_tmpd2, [R_modc])
    R_acc = [Reg(f'acc{i}') for i in range(8)]
    NEX = int(os.environ.get('KEXP', '16'))
    nyo = 0; nw = 0
    for ex in range(NEX):
        for st in range(4):
            em.dma('pool', lambda e, st=st, ex=ex: e.indirect_dma_start(out=Xg[:, st, :], out_offset=None, in_=hx2s,
                                                                       in_offset=bass.IndirectOffsetOnAxis(ap=idxT[:, st * 16 + ex:st * 16 + ex + 1], axis=0)),
                   reads=[R_idxT], writes=[R_Xg[st]], stream='gx', depth=2)
        for st in range(4):
            for k8 in range(4):
                ps = PSB[k8 % 2]; psr = PSR[k8 % 2]
                psb = ps[:, :].bitcast(BF16)
                for j in range(8):
                    kc = k8 * 8 + j
                    em.op('pe', lambda e, kc=kc, j=j, st=st, psb=psb: e.transpose(out=psb[:, j * 128:(j + 1) * 128], in_=Xg[:, st, kc * 128:(kc + 1) * 128], identity=identb),
                          reads=[R_Xg[st], R_tabs], writes=[psr])
                copy_op(em, 'act' if k8 % 2 == 0 else 'dve', XT[:, k8 * 8:(k8 + 1) * 8, st * 128:(st + 1) * 128], psb.rearrange("p (j t) -> p j t", j=8), [psr], [R_XT])
        for fc in range(16):
            wb = fc % 2
            em.dma('sp', lambda e, ex=ex, fc=fc: e.dma_start(out=wst[0], in_=wg[ex, fc]), writes=[R_wst[0]], stream='mw0', depth=1)
            copy_op(em, 'act', wgb[wb], wst[0], [R_wst[0]], [R_wgb[wb]])
            em.dma('sp', lambda e, ex=ex, fc=fc: e.dma_start(out=wst[1], in_=wu[ex, fc]), writes=[R_wst[1]], stream='mw1', depth=1)
            copy_op(em, 'dve', wub[wb], wst[1], [R_wst[1]], [R_wub[wb]])
            psg = PSB[2 + (fc % 2) * 2]; psrg = PSR[2 + (fc % 2) * 2]; psu = PSB[3 + (fc % 2) * 2]; psru = PSR[3 + (fc % 2) * 2]
            for kc in range(KC):
                em.op('pe', lambda e, kc=kc, psg=psg, wb=wb: e.matmul(psg[:, :], lhsT=wgb[wb][:, kc, :], rhs=XT[:, kc, :], start=(kc == 0), stop=(kc == KC - 1)),
                      reads=[R_wgb[wb], R_XT], writes=[psrg])
            for kc in range(KC):
                em.op('pe', lambda e, kc=kc, psu=psu, wb=wb: e.matmul(psu[:, :], lhsT=wub[wb][:, kc, :], rhs=XT[:, kc, :], start=(kc == 0), stop=(kc == KC - 1)),
                      reads=[R_wub[wb], R_XT], writes=[psru])
            sb = fc % 2
            em.op('act', lambda e, sb=sb, psg=psg: e.activation(out=stmp[sb], in_=psg[:, :], func=AF.Silu), reads=[psrg], writes=[R_stmp[sb]])
            em.op('dve', lambda e, sb=sb, psu=psu, fc=fc: e.tensor_tensor(out=actT[:, fc, :], in0=psu[:, :], in1=stmp[sb], op=ALU.mult),
                  reads=[psru, R_stmp[sb]], writes=[R_actT])
        for ct in range(8):
            db = ct % 2
            for hq in range(2):
                wv = wst[hq].rearrange("p (a b) c -> p a (b c)", a=8)
                em.dma('sp', lambda e, hq=hq, ex=ex, ct=ct, wv=wv: e.dma_start(out=wv, in_=wd[ex, ct][:, hq * 8:(hq + 1) * 8, :]),
                       writes=[R_wst[hq]], stream='mw%d' % hq, depth=1)
                copy_op(em, 'act' if hq == 0 else 'dve', wdb[db][:, hq * 8:(hq + 1) * 8, :], wv, [R_wst[hq]], [R_wdb[db]])
            for st in range(4):
                pi = (ct * 4 + st) % 2; ps = PSB[pi]; psr = PSR[pi]
                for fk in range(16):
                    em.op('pe', lambda e, fk=fk, st=st, ps=ps, db=db: e.matmul(ps[:, :], lhsT=actT[:, fk, st * 128:(st + 1) * 128], rhs=wdb[db][:, fk, :],
                                                                               start=(fk == 0), stop=(fk == 15)), reads=[R_actT, R_wdb[db]], writes=[psr])
                yb = nyo % 4; nyo += 1
                em.op('dve', lambda e, yb=yb, ps=ps, st=st, ex=ex, ct=ct: e.scalar_tensor_tensor(
                    out=yo[yb], in0=ps[:, :], scalar=gateT[:, st * 16 + ex:st * 16 + ex + 1], in1=gt2r[:, ct * 512:(ct + 1) * 512], op0=ALU.mult, op1=ALU.mult),
                    reads=[psr, R_gateT, R_gt2r], writes=[R_yo[yb]])
                em.dma('pool', lambda e, yb=yb, st=st, ex=ex, ct=ct: e.indirect_dma_start(
                    out=acc.rearrange("c t j -> (c t) j"), out_offset=bass.IndirectOffsetOnAxis(ap=idxT[:, st * 16 + ex:st * 16 + ex + 1], axis=0),
                    in_=yo[yb], in_offset=None, element_offset=ct * T * 512, compute_op=ALU.add),
                    reads=[R_yo[yb], R_idxT], writes=[R_acc[ct]], stream='sc', depth=4)
    em.barrier()
    if STAGE < 11:
        return
    gfr = carve(0, [D]); R_gfr = Reg('gfr')
    x2t = [carve((16 + 16 * i) * KB, [8, 512]) for i in range(2)]; R_x2t = [Reg(f'x2t{i}') for i in range(2)]
    junk2 = carve(48 * KB, [D], BF16); R_junk2 = Reg('junk2')
    ofin = [carve((56 + 16 * i) * KB, [D]) for i in range(2)]; R_ofin = [Reg(f'of{i}') for i in range(2)]
    sm2 = carve(90 * KB, [8]); R_sm2 = Reg('sm2')
    em.dma('sp', lambda e: e.dma_start(out=gfr, in_=rows3_d[1]), writes=[R_gfr], stream='ml', depth=2)
    for tt in range(32):
        xb = tt % 2; t0 = tt * 128
        em.dma('sp', lambda e, xb=xb, t0=t0: e.dma_start(out=x2t[xb], in_=accv[t0:t0 + 128]), reads=R_acc, writes=[R_x2t[xb]], stream='x2', depth=2)
        x2f = x2t[xb].rearrange("p a b -> p (a b)")
        em.op('act', lambda e, x2f=x2f: e.activation(out=junk2, in_=x2f, func=AF.Square, accum_out=sm2[:, 0:1]), reads=[R_x2t[xb]], writes=[R_junk2, R_sm2])
        em.op('act', lambda e: e.activation(out=sm2[:, 1:2], in_=sm2[:, 0:1], func=AF.Sqrt, scale=1.0 / D, bias=EPS), reads=[R_sm2], writes=[R_sm2])
        em.op('dve', lambda e: e.reciprocal(out=sm2[:, 1:2], in_=sm2[:, 1:2]), reads=[R_sm2], writes=[R_sm2])
        em.op('dve', lambda e, x2f=x2f, xb=xb: e.scalar_tensor_tensor(out=ofin[xb], in0=x2f, scalar=sm2[:, 1:2], in1=gfr, op0=ALU.mult, op1=ALU.mult),
              reads=[R_x2t[xb], R_sm2, R_gfr], writes=[R_ofin[xb]])
        em.dma('pool', lambda e, xb=xb, t0=t0: e.dma_start(out=out_d[t0:t0 + 128, :], in_=ofin[xb]), reads=[R_ofin[xb]], stream='fo', depth=2)
    em.barrier()


def kernel(**inputs):
    maps, poff, toff = prep_inputs(inputs)
    ntab = maps[0]['tabs'].shape[1]; npc = maps[0]['pcols'].shape[1]
    nc = build(poff, toff, ntab, npc)
    res = run_bass_kernel_spmd(nc, maps, core_ids=list(range(NCORES)))
    if STAGE < 99:
        return res
    return np.stack([r['out'] for r in res.results], 0)
```

```python
import os, math
import numpy as np
import ml_dtypes
from contextlib import ExitStack
import concourse.bass as bass
import concourse.mybir as mybir
from concourse.bass_utils import run_bass_kernel_spmd

F32 = mybir.dt.float32; BF16 = mybir.dt.bfloat16; U32 = mybir.dt.uint32; I32 = mybir.dt.int32
AF = mybir.ActivationFunctionType; ALU = mybir.AluOpType; AX = mybir.AxisListType
NPBF = ml_dtypes.bfloat16

D = 4096; T = 4096; NB = 4; KC = 32; NCTX = 256
DIN = 10240; NE = 16; FF = 2048; CAP = 512
EPS = 1e-6
MIN_DECAY = math.log(1e-2) / 1.5; MAX_DECAY = math.log(1e-2) / 0.3
MAGIC = 12582912.0
TWO_PI = 2.0 * math.pi
STAGE = int(os.environ.get("KSTAGE", "99"))
NCORES = int(os.environ.get('KCORES', '4'))

ENGS = {'pe': 'tensor', 'dve': 'vector', 'act': 'scalar', 'pool': 'gpsimd', 'sp': 'sync'}


class Reg:
    __slots__ = ('name', 'w', 'r')

    def __init__(self, name):
        self.name = name; self.w = None; self.r = {}


class Em:
    def __init__(self, nc, es):
        self.nc = nc; self.es = es
        self.q = {e: [] for e in ENGS}
        self.cnt = {e: 0 for e in ENGS}
        self.nsem = 0
        self.sem = {e: self._newsem("e_" + e) for e in ENGS}
        self.mine = {e: {id(self.sem[e])} for e in ENGS}
        self.seen = {e: {} for e in ENGS}
        self.streams = {}
        self.allsems = {}

    def _newsem(self, name):
        self.nsem += 1
        return self.es.enter_context(self.nc.semaphore(f"{name}_{self.nsem}"))

    def _wait(self, eng, tok):
        sem, val = tok
        if eng == 'pe' and id(sem) in self.mine['pe']:
            return
        if self.seen[eng].get(id(sem), 0) >= val:
            return
        self.seen[eng][id(sem)] = val
        self.q[eng].append(('w', sem, val))

    def _deps(self, eng, reads, writes):
        for r in reads:
            if r.w is not None:
                self._wait(eng, r.w)
        for w in writes:
            if w.w is not None:
                self._wait(eng, w.w)
            for t in w.r.values():
                self._wait(eng, t)

    def _mark(self, tok, reads, writes):
        k = id(tok[0])
        for r in reads:
            if k not in r.r or r.r[k][1] < tok[1]:
                r.r[k] = tok
        for w in writes:
            w.w = tok; w.r = {}
        self.allsems[k] = tok

    def op(self, eng, fn, reads=(), writes=()):
        self._deps(eng, reads, writes)
        if self.cnt[eng] >= 30000:
            self.sem[eng] = self._newsem("e_" + eng); self.mine[eng].add(id(self.sem[eng])); self.cnt[eng] = 0
        self.cnt[eng] += 1
        tok = (self.sem[eng], self.cnt[eng])
        self.q[eng].append(('o', fn, tok[0]))
        self._mark(tok, reads, writes)

    def dma(self, eng, fn, reads=(), writes=(), stream='d', depth=2):
        self._deps(eng, reads, writes)
        st = self.streams.setdefault(stream, {'n': 0, 'slots': []})
        i = st['n'] % depth; st['n'] += 1
        if i >= len(st['slots']):
            st['slots'].append([self._newsem("d_" + stream), 0])
        slot = st['slots'][i]
        if slot[1] > 0:
            self._wait(eng, (slot[0], slot[1]))
        if slot[1] >= 60000:
            raise RuntimeError("dma sem overflow " + stream)
        slot[1] += 16
        tok = (slot[0], slot[1])
        self.q[eng].append(('d', fn, tok[0]))
        self._mark(tok, reads, writes)

    def barrier(self):
        toks = list(self.allsems.values())
        for e in ENGS:
            for t in toks:
                self._wait(e, t)

    def run(self, block):
        def mk(name):
            items = self.q[name]

            def f(e):
                for it in items:
                    if it[0] == 'w':
                        e.wait_ge(it[1], it[2])
                    elif it[0] == 'o':
                        it[1](e).then_inc(it[2], 1)
                    else:
                        it[1](e).then_inc(it[2], 16)
            return f
        block.tensor(mk('pe')); block.vector(mk('dve')); block.scalar(mk('act'))
        block.gpsimd(mk('pool')); block.sync(mk('sp'))


def col_layout(v):
    v = np.asarray(v, np.float32).reshape(-1, 128)
    return np.ascontiguousarray(v.T)


class ColPack:
    def __init__(self):
        self.blocks = []; self.off = {}; self.n = 0

    def add(self, name, arr):
        arr = np.asarray(arr, np.float32)
        assert arr.shape[0] == 128
        arr = arr.reshape(128, -1)
        self.off[name] = self.n; self.blocks.append(arr); self.n += arr.shape[1]

    def build(self):
        return np.ascontiguousarray(np.concatenate(self.blocks, axis=1))


def pad128(a):
    out = np.zeros((128,) + a.shape[1:], a.dtype); out[:a.shape[0]] = a; return out


def dft_tables():
    f = np.arange(128)
    r = np.arange(64)
    th = 2 * np.pi * np.outer(r, f) / 128.0
    T1d = np.concatenate([np.cos(th), np.sin(th)], 1)
    T2ad = T1d.copy()
    T2bd = np.concatenate([-np.sin(th), np.cos(th)], 1)
    dk = np.arange(127) - 63
    thk = 2 * np.pi * np.outer(dk, f) / 128.0
    T1k = np.concatenate([np.cos(thk), np.sin(thk)], 1)
    T2ak = T1k.copy()
    T2bk = np.concatenate([-np.sin(thk), np.cos(thk)], 1)
    thi = 2 * np.pi * np.outer(f, r) / 128.0
    I2a = np.concatenate([np.cos(thi), np.sin(thi)], 1)
    I2b = np.concatenate([np.sin(thi), -np.cos(thi)], 1)
    I1a = np.cos(thi) / 16384.0
    I1b = -np.sin(thi) / 16384.0
    blocks = [pad128(T1d), pad128(T2ad), pad128(T2bd), pad128(T1k), pad128(T2ak), pad128(T2bk), I2a, I2b, I1a, I1b,
              np.eye(128), np.ones((128, 128))]
    names = ['T1d', 'T2ad', 'T2bd', 'T1k', 'T2ak', 'T2bk', 'I2a', 'I2b', 'I1a', 'I1b', 'identb', 'onesb']
    off = {}; n = 0
    for nm, b in zip(names, blocks):
        off[nm] = n; n += b.shape[1]
    tab = np.concatenate(blocks, 1).astype(np.float32).astype(NPBF)
    return tab, off


def filter_consts():
    n = T
    pos = np.arange(n, dtype=np.float32)
    t = np.linspace(0.0, 1.0, n, dtype=np.float32)
    bands = np.linspace(1e-4, 15, 16, dtype=np.float32)
    ang = (np.float32(2.0 * math.pi) * pos / np.float32(n))[:, None] * bands[None, :]
    z = np.concatenate([t[:, None], np.cos(ang), -np.sin(ang)], axis=-1).astype(np.float32)
    zT = pad128(np.ascontiguousarray(z.T))
    trow = np.ascontiguousarray(np.broadcast_to(t[None, :], (128, n))).astype(np.float32)
    delta = np.abs(np.linspace(MIN_DECAY, MAX_DECAY, 2048, dtype=np.float32))
    return zT, trow, delta


def prep_inputs(inp):
    g = lambda k: np.asarray(inp[k])
    sh = {}
    w_mod = g('w_mod')[0]
    sh['wmod'] = np.ascontiguousarray(w_mod.reshape(32, 128, 192, 128).transpose(2, 1, 0, 3))
    w_in = g('w_in')[0]
    sh['win'] = np.ascontiguousarray(w_in.reshape(32, 128, 80, 128).transpose(2, 1, 0, 3))
    w_out = g('w_out')[0]
    sh['wout'] = np.ascontiguousarray(w_out.reshape(32, 128, 8, 512).transpose(2, 1, 0, 3))
    sh['wg'] = np.ascontiguousarray(g('w_exp_gate')[0].reshape(16, 32, 128, 16, 128).transpose(0, 3, 2, 1, 4))
    sh['wu'] = np.ascontiguousarray(g('w_exp_up')[0].reshape(16, 32, 128, 16, 128).transpose(0, 3, 2, 1, 4))
    sh['wd'] = np.ascontiguousarray(g('w_exp_down')[0].reshape(16, 16, 128, 8, 512).transpose(0, 3, 2, 1, 4))
    sh['wr'] = np.ascontiguousarray(g('w_router')[0].reshape(32, 128, 16).transpose(1, 0, 2))
    wa = g('lru_wa')[0]; wx = g('lru_wx')[0]
    lw = np.stack([wa[0], wa[1], wx[0], wx[1]], 0)
    sh['lruw'] = np.ascontiguousarray(lw.transpose(1, 2, 0, 3))
    cp = ColPack()
    cp.add('b_mod', col_layout(g('b_mod')[0]))
    cp.add('g_mix', col_layout(g('g_mix')[0]))
    cp.add('g_ffn', col_layout(g('g_ffn')[0]))
    cp.add('b_in', col_layout(g('b_in')[0]))
    cp.add('hy_conv_b', col_layout(g('hy_conv_b')[0]))
    hcw = g('hy_conv_w')[0]
    cp.add('hy_conv_w', np.stack([col_layout(hcw[k]) for k in range(3)], -1))
    cp.add('hy_bias', col_layout(g('hy_bias')[0]))
    lcw = g('lru_conv_w')[0]
    cp.add('lru_conv_w', np.stack([col_layout(lcw[k]) for k in range(4)], -1))
    cp.add('lru_conv_b', col_layout(g('lru_conv_b')[0]))
    for nm in ['lru_ba', 'lru_bx', 'lru_lambda']:
        a = g(nm)[0]
        cp.add(nm, np.stack([col_layout(a[0]), col_layout(a[1])], 1))
    zT, trow, delta = filter_consts()
    cp.add('delta', col_layout(delta))
    for nm in ['hy_f_freq', 'hy_f_b1', 'hy_f_b2', 'hy_f_b3']:
        cp.add(nm, pad128(g(nm)[0].reshape(64, 1)))
    sh['pcols'] = cp.build()
    fw = np.zeros((128, 3, 64), np.float32)
    fw[:33, 0] = g('hy_f_w1')[0]; fw[:64, 1] = g('hy_f_w2')[0]; fw[:64, 2] = g('hy_f_w3')[0]
    sh['fw'] = fw
    sh['fwout'] = pad128(g('hy_f_wout')[0])
    sh['zT'] = zT; sh['trow'] = trow
    sh['rows3'] = np.ascontiguousarray(np.stack([
        np.broadcast_to(g('g_ffn')[0][None, :], (128, D)),
        np.broadcast_to(g('g_final')[None, :], (128, D)),
        np.broadcast_to(g('b_out')[0][None, :], (128, D))], 0)).astype(np.float32)
    tab, toff = dft_tables()
    sh['tabs'] = tab
    sh['cf32'] = np.ascontiguousarray(np.concatenate([np.eye(128), np.ones((128, 128))], 1)).astype(np.float32)
    x = g('x'); c = g('c'); ctx = g('ctx'); c_ctx = g('c_ctx')
    maps = []
    for b in range(NCORES):
        m = dict(sh)
        m['xT'] = np.ascontiguousarray(x[b].T)
        m['xtok'] = np.ascontiguousarray(x[b])
        m['ctxT'] = np.ascontiguousarray(ctx[b].T)
        m['ccol'] = np.ascontiguousarray(np.stack([col_layout(c[b]), col_layout(c_ctx)], -1))
        maps.append(m)
    return maps, cp.off, toff


def build(poff, toff, ntab, npc):
    nc = bass.Bass("TRN2", target_bir_lowering=False)
    dt_in = lambda name, shape, dt=F32: nc.dram_tensor(name, list(shape), dt, kind="ExternalInput").ap()
    xT = dt_in('xT', [D, T]); xtok = dt_in('xtok', [T, D]); ctxT = dt_in('ctxT', [D, NCTX])
    ccol_d = dt_in('ccol', [128, 32, 2])
    wmod = dt_in('wmod', [192, 128, 32, 128]); win = dt_in('win', [80, 128, 32, 128])
    wout = dt_in('wout', [8, 128, 32, 512])
    wg = dt_in('wg', [16, 16, 128, 32, 128]); wu = dt_in('wu', [16, 16, 128, 32, 128])
    wd = dt_in('wd', [16, 8, 128, 16, 512]); wr_d = dt_in('wr', [128, 32, 16])
    lruw_d = dt_in('lruw', [16, 128, 4, 128])
    pcols_d = dt_in('pcols', [128, npc]); fw_d = dt_in('fw', [128, 3, 64]); fwout_d = dt_in('fwout', [128, 4096])
    zT_d = dt_in('zT', [128, T]); trow_d = dt_in('trow', [128, T]); rows3_d = dt_in('rows3', [3, 128, D])
    tabs_d = dt_in('tabs', [128, ntab], BF16); cf32_d = dt_in('cf32', [128, 256])
    out_d = nc.dram_tensor('out', [T, D], F32, kind="ExternalOutput").ap()
    scr = lambda name, shape, dt: nc.dram_tensor(name, list(shape), dt, kind=("Internal" if STAGE >= 99 else "ExternalOutput")).ap()
    hxs = scr('hxs', [16, 128, 32 * 256], BF16)
    kfs = scr('kfs', [16, 128, 128 * 256], BF16)
    ysc = scr('ysc', [32, 128, T], BF16)
    acc = scr('acc', [8, T, 512], F32)
    hx2s = scr('hx2s', [T, D], BF16)
    dbg = {}
    if STAGE < 99:
        dbg['d_mod'] = nc.dram_tensor('d_mod', [128, 384], F32, kind="ExternalOutput").ap()
        dbg['d_a'] = nc.dram_tensor('d_a', [128, T], F32, kind="ExternalOutput").ap()
        dbg['d_b'] = nc.dram_tensor('d_b', [128, T], F32, kind="ExternalOutput").ap()
        dbg['d_c'] = nc.dram_tensor('d_c', [128, T], F32, kind="ExternalOutput").ap()
        dbg['d_d'] = nc.dram_tensor('d_d', [128, T], F32, kind="ExternalOutput").ap()
        dbg['d_kk'] = nc.dram_tensor('d_kk', [128, 8192], F32, kind="ExternalOutput").ap()
        dbg['d_dc'] = nc.dram_tensor('d_dc', [128, 512], F32, kind="ExternalOutput").ap()

    es = ExitStack()
    with es:
        E = es.enter_context
        em = Em(nc, es)
        AW = 50176
        arena = E(nc.sbuf_tensor("s_arena", [128, AW], F32))
        pc = E(nc.sbuf_tensor("s_pc", [128, npc], F32))
        dc = E(nc.sbuf_tensor("s_dc", [128, 512], F32))
        modc = E(nc.sbuf_tensor("s_modc", [128, 384], F32))
        tabs = E(nc.sbuf_tensor("s_tabs", [128, ntab], BF16))
        cf32 = E(nc.sbuf_tensor("s_cf32", [128, 256], F32))
        PSB = [E(nc.psum_tensor(f"ps{i}", [128, 512], F32)) for i in range(8)]
        PSR = [Reg(f"ps{i}") for i in range(8)]
        R_pc = Reg('pc'); R_dc = Reg('dc'); R_modc = Reg('modc'); R_tabs = Reg('tabs'); R_cf = Reg('cf32')
        identf = cf32[:, 0:128]; onesf = cf32[:, 128:256]
        TB = lambda nm, rows, c0, c1: tabs[0:rows, toff[nm] + c0: toff[nm] + c1]
        identb = TB('identb', 128, 0, 128); onesb = TB('onesb', 128, 0, 128)
        modc3 = modc[:, :].rearrange("p (j two) -> p j two", two=2)
        PC = lambda nm, i, n=1: pc[:, poff[nm] + i: poff[nm] + i + n]

        def carve(off, shape, dt=F32, parts=128):
            n = 1
            for s in shape:
                n *= s
            words = n if dt == F32 or dt == U32 or dt == I32 else (n + 1) // 2
            assert off + words <= AW, (off, words, AW)
            ap = arena[0:parts, off:off + words]
            if dt != F32:
                ap = ap.bitcast(dt)
            if len(shape) == 2:
                ap = ap.rearrange("p (a b) -> p a b", a=shape[0])
            elif len(shape) == 3:
                ap = ap.rearrange("p (a b c) -> p a b c", a=shape[0], b=shape[1])
            return ap

        em.dma('sp', lambda e: e.dma_start(out=pc[:, :], in_=pcols_d), writes=[R_pc], stream='su', depth=4)
        em.dma('sp', lambda e: e.dma_start(out=tabs[:, :], in_=tabs_d), writes=[R_tabs], stream='su', depth=4)
        em.dma('sp', lambda e: e.dma_start(out=cf32[:, :], in_=cf32_d), writes=[R_cf], stream='su', depth=4)

        scol = carve(0, [32, 2]); R_scol = Reg('scol')
        em.dma('sp', lambda e: e.dma_start(out=scol, in_=ccol_d), writes=[R_scol], stream='su', depth=4)
        em.op('act', lambda e: e.activation(out=scol, in_=scol, func=AF.Silu), reads=[R_scol], writes=[R_scol])
        wst = [carve(256 + i * 4096, [32, 128]) for i in range(3)]; R_wst = [Reg(f'wst{i}') for i in range(3)]
        for jc in range(192):
            b = jc % 3
            em.dma('sp' if jc % 2 == 0 else 'act', lambda e, b=b, jc=jc: e.dma_start(out=wst[b], in_=wmod[jc]),
                   writes=[R_wst[b]], stream='wm', depth=3)
            ps = PSB[jc % 2]
            for kc in range(KC):
                em.op('pe', lambda e, b=b, kc=kc, ps=ps: e.matmul(ps[:, 0:2], lhsT=wst[b][:, kc, :], rhs=scol[:, kc, :],
                                                                   start=(kc == 0), stop=(kc == KC - 1)),
                      reads=[R_wst[b], R_scol], writes=[PSR[jc % 2]])
            em.op('dve', lambda e, jc=jc, ps=ps: e.tensor_scalar(out=modc3[:, jc, :], in0=ps[:, 0:2], scalar1=PC('b_mod', jc),
                                                                  scalar2=None, op0=ALU.add),
                  reads=[PSR[jc % 2], R_pc], writes=[R_modc])
        MODX = lambda j0, kc: modc3[:, j0 + kc, 0:1]
        MODC = lambda j0, kc: modc3[:, j0 + kc, 1:2]
        em.op('dve', lambda e: e.scalar_tensor_tensor(out=dc[:, 0:32], in0=modc3[:, 32:64, 0], scalar=1.0, in1=PC('g_mix', 0, 32),
                                                      op0=ALU.add, op1=ALU.mult), reads=[R_modc, R_pc], writes=[R_dc])
        em.op('dve', lambda e: e.scalar_tensor_tensor(out=dc[:, 32:64], in0=modc3[:, 32:64, 1], scalar=1.0, in1=PC('g_mix', 0, 32),
                                                      op0=ALU.add, op1=ALU.mult), reads=[R_modc, R_pc], writes=[R_dc])
        em.op('dve', lambda e: e.scalar_tensor_tensor(out=dc[:, 64:96], in0=modc3[:, 128:160, 0], scalar=1.0, in1=PC('g_ffn', 0, 32),
                                                      op0=ALU.add, op1=ALU.mult), reads=[R_modc, R_pc], writes=[R_dc])
        em.op('act', lambda e: e.activation(out=dc[:, 96:128], in_=PC('lru_lambda', 0, 32), func=AF.Exp, scale=-1.0),
              reads=[R_pc], writes=[R_dc])
        em.op('act', lambda e: e.activation(out=dc[:, 96:128], in_=dc[:, 96:128], func=AF.Ln, bias=1.0),
              reads=[R_dc], writes=[R_dc])
        em.op('dve', lambda e: e.tensor_scalar(out=dc[:, 128:160], in0=dc[:, 96:128], scalar1=-8.0, scalar2=None, op0=ALU.mult),
              reads=[R_dc], writes=[R_dc])
        em.op('dve', lambda e: e.tensor_scalar(out=dc[:, 160:192], in0=dc[:, 96:128], scalar1=-16.0, scalar2=None, op0=ALU.mult),
              reads=[R_dc], writes=[R_dc])
        em.op('dve', lambda e: e.tensor_scalar(out=dc[:, 208:224], in0=PC('delta', 0, 16), scalar1=-1.0, scalar2=None, op0=ALU.mult),
              reads=[R_pc], writes=[R_dc])
        for i, nm in enumerate(['hy_f_b1', 'hy_f_b2', 'hy_f_b3']):
            em.op('dve', lambda e, i=i, nm=nm: e.tensor_tensor(out=dc[:, 224 + i:225 + i], in0=PC(nm, 0), in1=PC('hy_f_freq', 0),
                                                               op=ALU.mult), reads=[R_pc], writes=[R_dc])
        if STAGE < 99:
            em.dma('sp', lambda e: e.dma_start(out=dbg['d_mod'], in_=modc[:, :]), reads=[R_modc], stream='dbg', depth=2)
        em.barrier()
        if STAGE >= 2:
            phase2_onwards(nc, em, locals())
        em.barrier()
        block = E(nc.Block())
        em.run(block)
    return nc


def phase2_onwards(nc, em, L):
    globals().update({})
    carve = L['carve']; PSB = L['PSB']; PSR = L['PSR']; pc = L['pc']; dc = L['dc']; modc3 = L['modc3']
    R_pc = L['R_pc']; R_dc = L['R_dc']; R_modc = L['R_modc']; R_tabs = L['R_tabs']; R_cf = L['R_cf']
    PC = L['PC']; TB = L['TB']; identf = L['identf']; onesf = L['onesf']; identb = L['identb']; onesb = L['onesb']
    MODX = L['MODX']; MODC = L['MODC']; dbg = L['dbg']; poff = L['poff']
    xT = L['xT']; ctxT = L['ctxT']; hxs = L['hxs']; kfs = L['kfs']; ysc = L['ysc']; win = L['win']
    lruw_d = L['lruw_d']; fw_d = L['fw_d']; fwout_d = L['fwout_d']; zT_d = L['zT_d']; trow_d = L['trow_d']
    KB = 256

    HC_OFF = 180 * KB
    hc = carve(HC_OFF, [32, 256], BF16); R_hc = Reg('hc')
    xb = [carve(i * 32 * KB, [32, 256]) for i in range(2)]; R_xb = [Reg(f'xb{i}') for i in range(2)]
    sq = carve(64 * KB, [32, 256], BF16); R_sq = Reg('sq')
    hxo = [carve((80 + 16 * i) * KB, [32, 256], BF16) for i in range(2)]; R_hxo = [Reg(f'hxo{i}') for i in range(2)]
    rs = carve(112 * KB, [256]); R_rs = Reg('rs')
    xTv = xT.rearrange("(kc p) t -> p kc t", p=128)
    ctxTv = ctxT.rearrange("(kc p) t -> p kc t", p=128)
    for tt in range(17):
        b = tt % 2
        isctx = (tt == 16)
        src = ctxTv if isctx else xTv[:, :, tt * 256:(tt + 1) * 256]
        em.dma('sp', lambda e, b=b, src=src: e.dma_start(out=xb[b], in_=src), writes=[R_xb[b]], stream='xl', depth=2)
        em.op('act', lambda e, b=b: e.activation(out=sq, in_=xb[b], func=AF.Square), reads=[R_xb[b]], writes=[R_sq])
        ps = PSB[tt % 2]; psr = PSR[tt % 2]
        for kc in range(KC):
            em.op('pe', lambda e, kc=kc, ps=ps: e.matmul(ps[:, 0:256], lhsT=onesb, rhs=sq[:, kc, :], start=(kc == 0), stop=(kc == KC - 1)),
                  reads=[R_sq, R_tabs], writes=[psr])
        em.op('act', lambda e, ps=ps: e.activation(out=rs, in_=ps[:, 0:256], func=AF.Sqrt, scale=1.0 / D, bias=EPS),
              reads=[psr], writes=[R_rs])
        em.op('dve', lambda e: e.reciprocal(out=rs, in_=rs), reads=[R_rs], writes=[R_rs])
        em.op('dve', lambda e, b=b: e.tensor_tensor(out=xb[b], in0=xb[b], in1=rs[:, None, :].broadcast_to([128, 32, 256]), op=ALU.mult),
              reads=[R_xb[b], R_rs], writes=[R_xb[b]])
        dst = hc if isctx else hxo[b]; R_dst = R_hc if isctx else R_hxo[b]
        for kc in range(KC):
            sc_ap = dc[:, 32 + kc:33 + kc] if isctx else dc[:, kc:kc + 1]
            bi_ap = MODC(0, kc) if isctx else MODX(0, kc)
            em.op('act', lambda e, b=b, kc=kc, dst=dst, sc_ap=sc_ap, bi_ap=bi_ap: e.activation(
                out=dst[:, kc, :], in_=xb[b][:, kc, :], func=AF.Identity, scale=sc_ap, bias=bi_ap),
                reads=[R_xb[b], R_dc, R_modc], writes=[R_dst])
        if not isctx:
            em.dma('pool', lambda e, b=b, tt=tt: e.dma_start(out=hxs[tt].rearrange("p (a b) -> p a b", a=32), in_=hxo[b]),
                   reads=[R_hxo[b]], stream='hxst', depth=2)
    em.barrier()
    if STAGE < 3:
        return
    if not os.environ.get('KSKIPF'):
        phase_filter(nc, em, L, locals())
    if STAGE < 4:
        return
    phase_groups(nc, em, L, locals())
    if STAGE < 7:
        return
    phase_out(nc, em, L, locals())
    if STAGE < 8:
        return
    phase_moe(nc, em, L, locals())


def copy_op(em, eng, out, in_, reads, writes):
    if eng == 'act':
        em.op('act', lambda e: e.activation(out=out, in_=in_, func=AF.Copy), reads=reads, writes=writes)
    else:
        em.op(eng, lambda e: e.tensor_copy(out=out, in_=in_), reads=reads, writes=writes)


def sin_act(em, pre, R_pre, tmp, R_tmp, n):
    em.op('dve', lambda e: e.tensor_scalar(out=tmp, in0=pre, scalar1=1.0 / TWO_PI, scalar2=MAGIC, op0=ALU.mult, op1=ALU.add),
          reads=[R_pre], writes=[R_tmp])
    em.op('dve', lambda e: e.tensor_scalar(out=tmp, in0=tmp, scalar1=-MAGIC, scalar2=-TWO_PI, op0=ALU.add, op1=ALU.mult),
          reads=[R_tmp], writes=[R_tmp])
    em.op('dve', lambda e: e.tensor_tensor(out=pre, in0=pre, in1=tmp, op=ALU.add), reads=[R_pre, R_tmp], writes=[R_pre])
    em.op('dve', lambda e: e.tensor_scalar(out=pre, in0=pre, scalar1=-3.1415925, scalar2=3.1415925, op0=ALU.max, op1=ALU.min),
          reads=[R_pre], writes=[R_pre])
    em.op('act', lambda e: e.activation(out=pre, in_=pre, func=AF.Sin), reads=[R_pre], writes=[R_pre])


def phase_filter(nc, em, L, L2):
    carve = L['carve']; PSB = L['PSB']; PSR = L['PSR']; pc = L['pc']; dc = L['dc']
    R_pc = L['R_pc']; R_dc = L['R_dc']; R_tabs = L['R_tabs']; R_cf = L['R_cf']
    PC = L['PC']; TB = L['TB']; identf = L['identf']; dbg = L['dbg']
    kfs = L['kfs']; fw_d = L['fw_d']; fwout_d = L['fwout_d']; zT_d = L['zT_d']; trow_d = L['trow_d']
    KB = 256
    zT = carve(0, [T]); R_zT = Reg('zT')
    trow = carve(16 * KB, [T]); R_trow = Reg('trow')
    hA = carve(32 * KB, [T]); R_hA = Reg('hA')
    hB = carve(48 * KB, [T]); R_hB = Reg('hB')
    fwout = carve(64 * KB, [4096]); R_fwout = Reg('fwout')
    fw = carve(80 * KB, [3, 64]); R_fw = Reg('fw')
    kk = carve(82 * KB, [8192]); R_kk = Reg('kk')
    dec = [carve((114 + 2 * i) * KB, [512]) for i in range(2)]; R_dec = [Reg(f'dec{i}') for i in range(2)]
    Krm = carve(118 * KB, [128, 128], BF16); R_Krm = Reg('Krm')
    Zk = carve(150 * KB, [32, 256], BF16); R_Zk = Reg('Zk'); R_Zkc = [Reg(f'Zk{i}') for i in range(16)]
    Kfo = [carve((0 + 8 * i) * KB, [16, 256], BF16) for i in range(2)]; R_Kfo = [Reg(f'Kfo{i}') for i in range(2)]
    nrm = dc[:, 240:241]
    em.dma('sp', lambda e: e.dma_start(out=zT, in_=zT_d), writes=[R_zT], stream='fl', depth=4)
    em.dma('sp', lambda e: e.dma_start(out=trow, in_=trow_d), writes=[R_trow], stream='fl', depth=4)
    em.dma('sp', lambda e: e.dma_start(out=fwout, in_=fwout_d), writes=[R_fwout], stream='fl', depth=4)
    em.dma('sp', lambda e: e.dma_start(out=fw, in_=fw_d), writes=[R_fw], stream='fl', depth=4)
    srcs = [(zT, R_zT, 33), (hA, R_hA, 64), (hB, R_hB, 64)]
    dsts = [(hA, R_hA), (hB, R_hB), (hA, R_hA)]
    for layer in range(3):
        src, R_src, K = srcs[layer]; dst, R_d = dsts[layer]
        for tl in range(8):
            ps = PSB[tl % 2]; psr = PSR[tl % 2]
            em.op('pe', lambda e, src=src, K=K, tl=tl, ps=ps, layer=layer: e.matmul(
                ps[0:64, :], lhsT=fw[0:K, layer, :], rhs=src[0:K, tl * 512:(tl + 1) * 512], start=True, stop=True),
                reads=[R_fw, R_src], writes=[psr])
            em.op('act', lambda e, dst=dst, tl=tl, ps=ps, layer=layer: e.activation(
                out=dst[0:64, tl * 512:(tl + 1) * 512], in_=ps[0:64, :], func=AF.Identity,
                scale=PC('hy_f_freq', 0)[0:64], bias=dc[0:64, 224 + layer:225 + layer]),
                reads=[psr, R_pc, R_dc], writes=[R_d])
        tmp = carve(118 * KB, [T]); R_tmp = R_Krm
        sin_act(em, dst[0:64, :], R_d, tmp[0:64, :], R_tmp, T)
    h3 = hA; R_h3 = R_hA
    em.barrier()
    for cc in range(16):
        for tl in range(8):
            db = tl % 2
            em.op('act', lambda e, db=db, tl=tl, cc=cc: e.activation(out=dec[db], in_=trow[:, tl * 512:(tl + 1) * 512], func=AF.Exp,
                                                                    scale=dc[:, 208 + cc:209 + cc]),
                  reads=[R_trow, R_dc], writes=[R_dec[db]])
            for dr in range(2):
                pi = (tl * 2 + dr) % 4; ps = PSB[pi]; psr = PSR[pi]
                c0 = dr * 2048 + cc * 128
                em.op('pe', lambda e, c0=c0, tl=tl, ps=ps: e.matmul(ps[:, :], lhsT=fwout[0:64, c0:c0 + 128],
                                                                    rhs=h3[0:64, tl * 512:(tl + 1) * 512], start=True, stop=True),
                      reads=[R_fwout, R_h3], writes=[psr])
                if dr == 0:
                    em.op('dve', lambda e, tl=tl, ps=ps, db=db: e.tensor_tensor(out=kk[:, 4096 + tl * 512: 4096 + (tl + 1) * 512],
                                                                               in0=ps[:, :], in1=dec[db], op=ALU.mult),
                          reads=[psr, R_dec[db]], writes=[R_kk])
                else:
                    if tl == 0:
                        em.op('dve', lambda e, ps=ps, db=db: e.tensor_tensor(out=kk[:, 0:1], in0=ps[:, 0:1], in1=dec[db][:, 0:1], op=ALU.mult),
                              reads=[psr, R_dec[db]], writes=[R_kk])
                        em.op('dve', lambda e, ps=ps, db=db: e.tensor_tensor(out=kk[:, 3585:4096][:, ::-1], in0=ps[:, 1:512],
                                                                           in1=dec[db][:, 1:512], op=ALU.mult),
                              reads=[psr, R_dec[db]], writes=[R_kk])
                    else:
                        lo = 4096 - tl * 512 - 511
                        em.op('dve', lambda e, ps=ps, db=db, lo=lo: e.tensor_tensor(out=kk[:, lo:lo + 512][:, ::-1], in0=ps[:, :],
                                                                                  in1=dec[db], op=ALU.mult),
                              reads=[psr, R_dec[db]], writes=[R_kk])
        em.op('dve', lambda e: e.tensor_reduce(out=nrm, in_=kk, axis=AX.X, op=ALU.add, apply_absolute_value=True),
              reads=[R_kk], writes=[R_dc])
        em.op('dve', lambda e, cc=cc: e.reciprocal(out=dc[:, 192 + cc:193 + cc], in_=nrm), reads=[R_dc], writes=[R_dc])
        if STAGE < 99 and cc == 0:
            em.dma('sp', lambda e: e.dma_start(out=dbg['d_kk'], in_=kk), reads=[R_kk], stream='dbg', depth=2)
        for e4 in range(32):
            ps = PSB[4 + e4 % 2]; psr = PSR[4 + e4 % 2]
            ne = 4 if e4 < 31 else 3
            for j in range(ne):
                ee = e4 * 4 + j
                em.op('pe', lambda e, ee=ee, j=j, ps=ps: e.transpose(out=ps[0:127, j * 128:(j + 1) * 128],
                                                                     in_=kk[:, 1 + ee: 1 + ee + 64 * 126 + 1: 64], identity=identf),
                      reads=[R_kk, R_cf], writes=[psr])
            copy_op(em, 'act' if e4 % 2 == 0 else 'dve', Krm[0:127, :, e4 * 4:e4 * 4 + ne],
                    ps[0:127, 0:ne * 128].rearrange("p (j c) -> p c j", j=ne), [psr], [R_Krm])
        for cb in range(4):
            for c2 in range(16):
                ps = PSB[c2 % 2]; psr = PSR[c2 % 2]
                for j in range(2):
                    c = cb * 32 + c2 * 2 + j
                    em.op('pe', lambda e, c=c, j=j, ps=ps: e.matmul(ps[0:127, j * 256:(j + 1) * 256], lhsT=Krm[0:127, c, 0:127],
                                                                    rhs=TB('T1k', 127, 0, 256), start=True, stop=True),
                          reads=[R_Krm, R_tabs], writes=[psr])
                em.op('act', lambda e, c2=c2, ps=ps: e.activation(out=Zk[0:127, c2 * 2:c2 * 2 + 2, :],
                                                                  in_=ps[0:127, :].rearrange("p (j f) -> p j f", j=2), func=AF.Copy),
                      reads=[psr], writes=[R_Zkc[c2]])
            for c2 in range(16):
                ps = PSB[2 + c2 % 2]; psr = PSR[2 + c2 % 2]
                kb = c2 // 8
                for j in range(2):
                    cs = c2 * 2 + j
                    em.op('pe', lambda e, cs=cs, j=j, ps=ps: e.matmul(ps[:, j * 256:(j + 1) * 256], lhsT=Zk[0:127, cs, 0:128],
                                                                      rhs=TB('T2ak', 127, 0, 256), start=True, stop=False),
                          reads=[R_Zkc[c2], R_tabs], writes=[psr])
                    em.op('pe', lambda e, cs=cs, j=j, ps=ps: e.matmul(ps[:, j * 256:(j + 1) * 256], lhsT=Zk[0:127, cs, 128:256],
                                                                      rhs=TB('T2bk', 127, 0, 256), start=False, stop=True),
                          reads=[R_Zkc[c2], R_tabs], writes=[psr])
                em.op('dve', lambda e, c2=c2, kb=kb, ps=ps: e.tensor_copy(out=Kfo[kb][:, (c2 % 8) * 2:(c2 % 8) * 2 + 2, :],
                                                                          in_=ps[:, :].rearrange("p (j f) -> p j f", j=2)),
                      reads=[psr], writes=[R_Kfo[kb]])
                if c2 % 8 == 7:
                    c0 = cb * 32 + kb * 16
                    em.dma('sp', lambda e, kb=kb, cc=cc, c0=c0: e.dma_start(
                        out=kfs[cc][:, c0 * 256:(c0 + 16) * 256].rearrange("p (a b) -> p a b", a=16), in_=Kfo[kb]),
                        reads=[R_Kfo[kb]], stream='kfst', depth=2)
    em.barrier()


def phase_groups(nc, em, L, L2):
    carve = L['carve']; PSB = L['PSB']; PSR = L['PSR']; pc = L['pc']; dc = L['dc']
    R_pc = L['R_pc']; R_dc = L['R_dc']; R_modc = L['R_modc']; R_tabs = L['R_tabs']; R_cf = L['R_cf']
    PC = L['PC']; TB = L['TB']; identf = L['identf']; dbg = L['dbg']; poff = L['poff']
    hxs = L['hxs']; kfs = L['kfs']; ysc = L['ysc']; win = L['win']; lruw_d = L['lruw_d']
    hc = L2['hc']; R_hc = L2['R_hc']
    KB = 256
    NG = int(os.environ.get('KGROUPS', '16')); KSUB = int(os.environ.get('KSUB', '9'))
    U = carve(0, [T]); X0C = carve(16 * KB, [T]); XC = carve(32 * KB, [T]); GG = carve(48 * KB, [T])
    R_U = Reg('U'); R_X0C = Reg('X0C'); R_XC = Reg('XC'); R_GG = Reg('GG')
    wstg = [carve((64 + 16 * i) * KB, [32, 128]) for i in range(1)]; R_wstg = [Reg('wstg0')]
    wbf = carve(80 * KB, [5, 32, 128], BF16)
    wbf = wbf
    R_wbf = [Reg(f'wbf{i}') for i in range(5)]
    hxt = [carve((120 + 16 * i) * KB, [32, 256], BF16) for i in range(2)]; R_hxt = [Reg(f'hxt{i}') for i in range(2)]
    ptmp = [carve((152 + 5 * i) * KB, [5, 256]) for i in range(2)]; R_ptmp = [[Reg(f'pt{i}_{j}') for j in range(5)] for i in range(2)]
    lwst = carve(162 * KB, [4, 128]); R_lwst = Reg('lwst')
    lwb = carve(164 * KB, [4, 128], BF16); R_lwb = Reg('lwb')
    cst = 165 * KB
    ctmp = [carve(cst + i * 256, [256]) for i in range(10)]; R_ctmp = [Reg(f'ct{i}') for i in range(10)]
    cxb = carve(cst + 10 * 256, [256], BF16); R_cxb = Reg('cxb')
    ct2 = [carve(176 * KB + i * 256, [256]) for i in range(4)]; R_ct2 = [Reg(f'ct2_{i}') for i in range(4)]
    xcb = carve(64 * KB, [T], BF16); R_xcb = Reg('xcb')
    LA = carve(72 * KB, [T]); LB = carve(88 * KB, [T]); LT = carve(104 * KB, [T]); HF = carve(120 * KB, [T]); HB = carve(136 * KB, [T])
    R_LA = Reg('LA'); R_LB = Reg('LB'); R_LT = Reg('LT'); R_HF = Reg('HF'); R_HB = Reg('HB')
    ylru = carve(152 * KB, [T], BF16); R_ylru = Reg('ylru')
    Urm = carve(64 * KB, [128, 64], BF16, parts=64); R_Urm = Reg('Urm')
    Zs = [carve((80 + 8 * i) * KB, [16, 256], BF16, parts=64) for i in range(2)]; R_Zs = [Reg(f'Zs{i}') for i in range(2)]; R_Zsc = [[Reg(f'Zs{i}_{j}') for j in range(8)] for i in range(2)]
    Kfs = [carve((96 + 8 * i) * KB, [16, 256], BF16) for i in range(2)]; R_Kfs = [Reg(f'Kfs{i}') for i in range(2)]
    Yab = [carve(112 * KB + i * 256, [2, 2, 128], BF16) for i in range(4)]; R_Yab = [Reg(f'Yab{i}') for i in range(4)]
    prod = [carve(116 * KB + i * 1024, [2, 512]) for i in range(2)]; R_prod = [Reg(f'prod{i}') for i in range(2)]
    GH = carve(124 * KB, [2 * 64, 128], BF16); R_GH = Reg('GH')
    GH4 = GH.rearrange("p (g r) c -> p g r c", g=2)
    yhy = carve(156 * KB, [T], BF16); R_yhy = Reg('yhy')
    t1b = [carve((80 + 8 * i) * KB, [512]) for i in range(2)]; R_t1b = R_Zs
    h0 = lambda d: dc[:, 230 + d:231 + d]

    def lru_coeffs(n, xc_ap, xcb_ap, g, d, A, R_A, Bq, R_B, TMP, R_TMP, R_xc, R_xcbr, tile):
        nt = n // tile
        for which, dstb, R_dst in ((0, A, R_A), (1, TMP, R_TMP)):
            for tl in range(nt):
                ps = PSB[tl % 2]; psr = PSR[tl % 2]
                sl = slice(tl * tile, (tl + 1) * tile)
                em.op('pe', lambda e, ps=ps, sl=sl, which=which: e.matmul(ps[:, 0:tile], lhsT=lwb[:, which * 2 + d, :], rhs=xcb_ap[:, sl],
                                                                        start=True, stop=True), reads=[R_lwb, R_xcbr], writes=[psr])
                bcol = PC('lru_ba' if which == 0 else 'lru_bx', d * 16 + g)
                em.op('act', lambda e, ps=ps, sl=sl, dstb=dstb, bcol=bcol: e.activation(out=dstb[:, sl], in_=ps[:, 0:tile], func=AF.Sigmoid, bias=bcol),
                      reads=[psr, R_pc], writes=[R_dst])
        em.op('dve', lambda e: e.tensor_tensor(out=Bq, in0=TMP, in1=xc_ap, op=ALU.mult), reads=[R_TMP, R_xc], writes=[R_B])
        em.op('act', lambda e: e.activation(out=TMP, in_=A, func=AF.Exp, scale=dc[:, 160 + d * 16 + g:161 + d * 16 + g]),
              reads=[R_A, R_dc], writes=[R_TMP])
        em.op('act', lambda e: e.activation(out=TMP, in_=TMP, func=AF.Sqrt, scale=-1.0, bias=1.0), reads=[R_TMP], writes=[R_TMP])
        em.op('dve', lambda e: e.tensor_tensor(out=Bq, in0=Bq, in1=TMP, op=ALU.mult), reads=[R_TMP, R_B], writes=[R_B])
        em.op('act', lambda e: e.activation(out=A, in_=A, func=AF.Exp, scale=dc[:, 128 + d * 16 + g:129 + d * 16 + g]),
              reads=[R_A, R_dc], writes=[R_A])

    for g in range(NG):
        jl = [g, 16 + g, 32 + g, 48 + g, 64 + g]
        for i, jc in enumerate(jl):
            em.dma('sp', lambda e, jc=jc: e.dma_start(out=wstg[0], in_=win[jc]), writes=[R_wstg[0]], stream='wi', depth=2)
            copy_op(em, 'act' if i % 2 == 0 else 'pool', wbf[:, i], wstg[0], [R_wstg[0]], [R_wbf[i]])
        em.dma('sp', lambda e, g=g: e.dma_start(out=lwst, in_=lruw_d[g]), writes=[R_lwst], stream='wi', depth=2)
        copy_op(em, 'dve', lwb, lwst, [R_lwst], [R_lwb])
        ps = PSB[7]; psr = PSR[7]
        for kc in range(KC):
            em.op('pe', lambda e, kc=kc, ps=ps: e.matmul(ps[:, 0:256], lhsT=wbf[:, 4, kc, :], rhs=hc[:, kc, :], start=(kc == 0), stop=(kc == KC - 1)),
                  reads=[R_wbf[4], R_hc], writes=[psr])
        pcl, xcc, cA, cB, cT, cH = ctmp[0], ctmp[1], ctmp[2], ctmp[3], ctmp[4], ctmp[5]
        em.op('act', lambda e, ps=ps, g=g: e.activation(out=pcl, in_=ps[:, 0:256], func=AF.Identity, bias=PC('b_in', 64 + g)),
              reads=[psr, R_pc], writes=[R_ctmp[0]])
        LW = lambda k, g=g: PC('lru_conv_w', g * 4 + k)
        em.op('dve', lambda e, LW=LW, g=g: e.tensor_scalar(out=xcc, in0=pcl, scalar1=LW(2), scalar2=PC('lru_conv_b', g), op0=ALU.mult, op1=ALU.add),
              reads=[R_ctmp[0], R_pc], writes=[R_ctmp[1]])
        for k, (so, si) in ((0, (slice(2, 256), slice(0, 254))), (1, (slice(1, 256), slice(0, 255))), (3, (slice(0, 255), slice(1, 256)))):
            em.op('dve', lambda e, LW=LW, k=k, so=so, si=si: e.scalar_tensor_tensor(out=xcc[:, so], in0=pcl[:, si], scalar=LW(k), in1=xcc[:, so],
                                                                           op0=ALU.mult, op1=ALU.add),
                  reads=[R_ctmp[0], R_ctmp[1], R_pc], writes=[R_ctmp[1]])
        copy_op(em, 'act', cxb, xcc, [R_ctmp[1]], [R_cxb])
        for d in range(2):
            lru_coeffs(256, xcc, cxb, g, d, cA, R_ctmp[2], cB, R_ctmp[3], cT, R_ctmp[4], R_ctmp[1], R_cxb, 256)
            if d == 0:
                em.op('dve', lambda e: e.tensor_tensor_scan(out=cH, data0=cA, data1=cB, initial=0.0, op0=ALU.mult, op1=ALU.add),
                      reads=[R_ctmp[2], R_ctmp[3]], writes=[R_ctmp[5]])
                em.op('dve', lambda e: e.tensor_copy(out=h0(0), in_=cH[:, 255:256]), reads=[R_ctmp[5]], writes=[R_dc])
            else:
                em.op('dve', lambda e: e.tensor_tensor_scan(out=cH[:, ::-1], data0=cA[:, ::-1], data1=cB[:, ::-1], initial=0.0,
                                                            op0=ALU.mult, op1=ALU.add), reads=[R_ctmp[2], R_ctmp[3]], writes=[R_ctmp[5]])
                em.op('dve', lambda e: e.tensor_copy(out=h0(1), in_=cH[:, 0:1]), reads=[R_ctmp[5]], writes=[R_dc])
        HW = lambda ch, k: PC('hy_conv_w', ch * 3 + k)
        for tt in range(16):
            hb = tt % 2; pb = tt % 2
            em.dma('sp', lambda e, hb=hb, tt=tt: e.dma_start(out=hxt[hb], in_=hxs[tt].rearrange("p (a b) -> p a b", a=32)),
                   writes=[R_hxt[hb]], stream='hxl', depth=2)
            tsl = slice(tt * 256, (tt + 1) * 256)
            for i in range(5):
                pi = (tt * 5 + i) % 4; ps = PSB[pi]; psr = PSR[pi]
                for kc in range(KC):
                    em.op('pe', lambda e, i=i, kc=kc, hb=hb, ps=ps: e.matmul(ps[:, 0:256], lhsT=wbf[:, i, kc, :], rhs=hxt[hb][:, kc, :],
                                                                           start=(kc == 0), stop=(kc == KC - 1)),
                          reads=[R_wbf[i], R_hxt[hb]], writes=[psr])
                em.op('act', lambda e, i=i, pb=pb, ps=ps, jc=jl[i]: e.activation(out=ptmp[pb][:, i, :], in_=ps[:, 0:256], func=AF.Identity,
                                                                                 bias=PC('b_in', jc)),
                      reads=[psr, R_pc], writes=[R_ptmp[pb][i]])
            v3 = lambda ap: ap.rearrange("p (r q) -> p r q", q=64)
            x1c, vcv = ct2[0], ct2[1]
            for i, (dst, R_dst) in enumerate(((X0C[:, tsl], R_X0C), (x1c, R_ct2[0]), (vcv, R_ct2[1]))):
                ch = i * 16 + g
                src = ptmp[pb][:, i, :]
                em.op('dve', lambda e, dst=dst, src=src, ch=ch: e.tensor_scalar(out=dst, in0=src, scalar1=HW(ch, 1), scalar2=PC('hy_conv_b', ch),
                                                                                 op0=ALU.mult, op1=ALU.add),
                      reads=[R_ptmp[pb][i], R_pc], writes=[R_dst])
                em.op('dve', lambda e, dst=dst, src=src, ch=ch: e.scalar_tensor_tensor(out=v3(dst)[:, :, 1:64], in0=v3(src)[:, :, 0:63], scalar=HW(ch, 0),
                                                                                        in1=v3(dst)[:, :, 1:64], op0=ALU.mult, op1=ALU.add),
                      reads=[R_ptmp[pb][i], R_pc, R_dst], writes=[R_dst])
                em.op('dve', lambda e, dst=dst, src=src, ch=ch: e.scalar_tensor_tensor(out=v3(dst)[:, :, 0:63], in0=v3(src)[:, :, 1:64], scalar=HW(ch, 2),
                                                                                        in1=v3(dst)[:, :, 0:63], op0=ALU.mult, op1=ALU.add),
                      reads=[R_ptmp[pb][i], R_pc, R_dst], writes=[R_dst])
            em.op('pool', lambda e, tsl=tsl: e.tensor_tensor(out=U[:, tsl], in0=x1c, in1=vcv, op=ALU.mult), reads=[R_ct2[0], R_ct2[1]], writes=[R_U])
            src = ptmp[pb][:, 4, :]; dst = XC[:, tsl]
            em.op('dve', lambda e, LW=LW, dst=dst, src=src, g=g: e.tensor_scalar(out=dst, in0=src, scalar1=LW(2), scalar2=PC('lru_conv_b', g),
                                                                           op0=ALU.mult, op1=ALU.add), reads=[R_ptmp[pb][4], R_pc], writes=[R_XC])
            for k, (so, si) in ((0, (slice(2, 64), slice(0, 62))), (1, (slice(1, 64), slice(0, 63))), (3, (slice(0, 63), slice(1, 64)))):
                em.op('dve', lambda e, LW=LW, dst=dst, src=src, k=k, so=so, si=si: e.scalar_tensor_tensor(
                    out=v3(dst)[:, :, so], in0=v3(src)[:, :, si], scalar=LW(k), in1=v3(dst)[:, :, so], op0=ALU.mult, op1=ALU.add),
                    reads=[R_ptmp[pb][4], R_pc, R_XC], writes=[R_XC])
            pg = ptmp[pb][:, 3, :]; gt = ct2[2]
            em.op('pool', lambda e, pg=pg: e.tensor_tensor(out=gt, in0=pg, in1=pg, op=ALU.mult), reads=[R_ptmp[pb][3]], writes=[R_ct2[2]])
            em.op('pool', lambda e: e.tensor_scalar(out=gt, in0=gt, scalar1=0.044715, scalar2=1.0, op0=ALU.mult, op1=ALU.add),
                  reads=[R_ct2[2]], writes=[R_ct2[2]])
            em.op('pool', lambda e, pg=pg: e.tensor_tensor(out=gt, in0=gt, in1=pg, op=ALU.mult), reads=[R_ct2[2], R_ptmp[pb][3]], writes=[R_ct2[2]])
            em.op('act', lambda e: e.activation(out=gt, in_=gt, func=AF.Sigmoid, scale=1.5957691216057308), reads=[R_ct2[2]], writes=[R_ct2[2]])
            em.op('pool', lambda e, pg=pg, tsl=tsl: e.tensor_tensor(out=GG[:, tsl], in0=gt, in1=pg, op=ALU.mult),
                  reads=[R_ct2[2], R_ptmp[pb][3]], writes=[R_GG])
        if STAGE < 5 and g == 0:
            for nm, ap, R_ in (('d_a', U, R_U), ('d_b', X0C, R_X0C), ('d_c', XC, R_XC), ('d_d', GG, R_GG)):
                em.dma('sp', lambda e, nm=nm, ap=ap: e.dma_start(out=dbg[nm], in_=ap), reads=[R_], stream='dbg', depth=2)
        if STAGE < 5:
            continue
        mix_regs = [R_xcb, R_LA, R_LB, R_LT, R_HF, R_HB, R_ylru, R_Urm, R_GH, R_yhy] + R_Zs + R_Kfs + R_Yab + R_prod + R_t1b
        inp_regs = R_wstg + R_wbf + R_hxt + [r for rr in R_ptmp for r in rr] + [R_lwst]
        em.barrier()
        copy_op(em, 'act', xcb, XC, [R_XC], [R_xcb])
        for d in range(2):
            H = HF if d == 0 else HB; R_H = R_HF if d == 0 else R_HB
            lru_coeffs(T, XC, xcb, g, d, LA, R_LA, LB, R_LB, LT, R_LT, R_XC, R_xcb, 512)
            if d == 0:
                em.op('dve', lambda e, H=H: e.tensor_tensor_scan(out=H, data0=LA, data1=LB, initial=h0(0), op0=ALU.mult, op1=ALU.add),
                      reads=[R_LA, R_LB, R_dc], writes=[R_H])
            else:
                em.op('dve', lambda e, H=H: e.tensor_tensor_scan(out=H[:, ::-1], data0=LA[:, ::-1], data1=LB[:, ::-1], initial=h0(1),
                                                                 op0=ALU.mult, op1=ALU.add), reads=[R_LA, R_LB, R_dc], writes=[R_H])
        if STAGE == 5 and g == 0:
            for nm, ap, R_ in (('d_a', HF, R_HF), ('d_b', HB, R_HB), ('d_c', LA, R_LA), ('d_d', LB, R_LB)):
                em.dma('sp', lambda e, nm=nm, ap=ap: e.dma_start(out=dbg[nm], in_=ap), reads=[R_], stream='dbg', depth=2)
            em.dma('sp', lambda e: e.dma_start(out=dbg['d_dc'], in_=dc[:, :]), reads=[R_dc], stream='dbg', depth=2)
        em.op('pool', lambda e: e.tensor_tensor(out=HF, in0=HF, in1=HB, op=ALU.add), reads=[R_HF, R_HB], writes=[R_HF])
        em.op('pool', lambda e: e.tensor_tensor(out=ylru, in0=HF, in1=GG, op=ALU.mult), reads=[R_HF, R_GG], writes=[R_ylru])
        em.dma('sp', lambda e, g=g: e.dma_start(out=ysc[16 + g], in_=ylru), reads=[R_ylru], stream='yst', depth=2)
        em.barrier()
        if STAGE < 6:
            continue
        for q4 in range(16):
            ps = PSB[q4 % 2]; psr = PSR[q4 % 2]
            for j in range(4):
                q = q4 * 4 + j
                em.op('pe', lambda e, q=q, j=j, ps=ps: e.transpose(out=ps[0:64, j * 128:(j + 1) * 128], in_=U[:, q:T:64], identity=identf),
                      reads=[R_U, R_cf], writes=[psr])
            copy_op(em, 'act' if q4 % 2 == 0 else 'dve', Urm[0:64, :, q4 * 4:q4 * 4 + 4],
                    ps[0:64, :].rearrange("p (j c) -> p c j", j=4), [psr], [R_Urm])
        pending = None

        def emit_i2(cb, c2, yb):
            ps2 = PSB[4 + (c2 // 2) % 2]; psr2 = PSR[4 + (c2 // 2) % 2]
            for j in range(2):
                col = ((c2 % 2) * 2 + j) * 128
                em.op('pe', lambda e, j=j, yb=yb, ps2=ps2, col=col: e.matmul(ps2[:, col:col + 128], lhsT=Yab[yb][:, j, 0, :], rhs=TB('I2a', 128, 0, 128),
                                                                             start=True, stop=False), reads=[R_Yab[yb], R_tabs], writes=[psr2])
                em.op('pe', lambda e, j=j, yb=yb, ps2=ps2, col=col: e.matmul(ps2[:, col:col + 128], lhsT=Yab[yb][:, j, 1, :], rhs=TB('I2b', 128, 0, 128),
                                                                             start=False, stop=True), reads=[R_Yab[yb], R_tabs], writes=[psr2])
            if c2 % 2 == 1:
                c0 = cb * 16 + (c2 - 1) * 2
                copy_op(em, 'act', GH4[:, :, :, c0:c0 + 4].rearrange("p g r c -> p c g r"),
                        ps2[:, :].rearrange("p (c g r) -> p c g r", c=4, g=2), [psr2], [R_GH])

        for cb in range(8):
            zb = cb % 2
            em.dma('sp', lambda e, zb=zb, cb=cb, g=g: e.dma_start(out=Kfs[zb], in_=kfs[g][:, cb * 4096:(cb + 1) * 4096].rearrange("p (a b) -> p a b", a=16)),
                   writes=[R_Kfs[zb]], stream='kfl', depth=2)
            for c2 in range(8):
                ps = PSB[2 + c2 % 2]; psr = PSR[2 + c2 % 2]
                for j in range(2):
                    c = cb * 16 + c2 * 2 + j
                    em.op('pe', lambda e, c=c, j=j, ps=ps: e.matmul(ps[0:64, j * 256:(j + 1) * 256], lhsT=Urm[0:64, c, :], rhs=TB('T1d', 64, 0, 256),
                                                                    start=True, stop=True), reads=[R_Urm, R_tabs], writes=[psr])
                copy_op(em, 'act', Zs[zb][0:64, c2 * 2:c2 * 2 + 2, :], ps[0:64, :].rearrange("p (j f) -> p j f", j=2), [psr], [R_Zsc[zb][c2]])
            for c2 in range(8):
                ps = PSB[c2 % 2]; psr = PSR[c2 % 2]
                yb = c2 % 4; pb2 = c2 % 2
                for j in range(2):
                    cs = c2 * 2 + j
                    em.op('pe', lambda e, cs=cs, j=j, ps=ps, zb=zb: e.matmul(ps[:, j * 256:(j + 1) * 256], lhsT=Zs[zb][0:64, cs, 0:128],
                                                                             rhs=TB('T2ad', 64, 0, 256), start=True, stop=False),
                          reads=[R_Zsc[zb][c2], R_tabs], writes=[psr])
                    em.op('pe', lambda e, cs=cs, j=j, ps=ps, zb=zb: e.matmul(ps[:, j * 256:(j + 1) * 256], lhsT=Zs[zb][0:64, cs, 128:256],
                                                                             rhs=TB('T2bd', 64, 0, 256), start=False, stop=True),
                          reads=[R_Zsc[zb][c2], R_tabs], writes=[psr])
                psv = ps[:, :].rearrange("p (c h f) -> p c h f", c=2, h=2)
                kv = Kfs[zb][:, c2 * 2:c2 * 2 + 2, :].rearrange("p c (h f) -> p c h f", h=2)
                pr0 = prod[pb2][:, 0, :].rearrange("p (c h f) -> p c h f", c=2, h=2)
                pr1 = prod[pb2][:, 1, :].rearrange("p (c h f) -> p c h f", c=2, h=2)
                em.op('dve', lambda e, psv=psv, kv=kv, pr0=pr0: e.tensor_tensor(out=pr0, in0=psv, in1=kv, op=ALU.mult),
                      reads=[psr, R_Kfs[zb]], writes=[R_prod[pb2]])
                em.op('dve', lambda e, psv=psv, kv=kv, pr1=pr1: e.tensor_tensor(out=pr1, in0=psv, in1=kv[:, :, ::-1, :], op=ALU.mult),
                      reads=[psr, R_Kfs[zb]], writes=[R_prod[pb2]])
                em.op('pool', lambda e, pr0=pr0, yb=yb: e.tensor_tensor(out=Yab[yb][:, :, 0, :], in0=pr0[:, :, 0, :], in1=pr0[:, :, 1, :], op=ALU.subtract),
                      reads=[R_prod[pb2]], writes=[R_Yab[yb]])
                em.op('pool', lambda e, pr1=pr1, yb=yb: e.tensor_tensor(out=Yab[yb][:, :, 1, :], in0=pr1[:, :, 0, :], in1=pr1[:, :, 1, :], op=ALU.add),
                      reads=[R_prod[pb2]], writes=[R_Yab[yb]])
                if pending is not None:
                    emit_i2(*pending)
                pending = (cb, c2, yb)
        emit_i2(*pending)
        if KSUB < 6:
            em.barrier(); continue
        for half in range(2):
            for r in range(32):
                rr = half * 32 + r
                ps = PSB[4 + r // 8]; psr = PSR[4 + r // 8]
                col = (r % 8) * 64
                em.op('pe', lambda e, rr=rr, ps=ps, col=col: e.matmul(ps[:, col:col + 64], lhsT=GH4[:, 0, rr, :], rhs=TB('I1a', 128, 0, 64),
                                                                      start=True, stop=False), reads=[R_GH, R_tabs], writes=[psr])
                em.op('pe', lambda e, rr=rr, ps=ps, col=col: e.matmul(ps[:, col:col + 64], lhsT=GH4[:, 1, rr, :], rhs=TB('I1b', 128, 0, 64),
                                                                      start=False, stop=True), reads=[R_GH, R_tabs], writes=[psr])
            for bk in range(4):
                ps = PSB[4 + bk]; psr = PSR[4 + bk]
                seg = slice(half * 2048 + bk * 512, half * 2048 + (bk + 1) * 512)
                tb = bk % 2
                em.op('act', lambda e, seg=seg, tb=tb, g=g: e.activation(out=t1b[tb], in_=U[:, seg], func=AF.Identity, scale=PC('hy_bias', g)),
                      reads=[R_U, R_pc], writes=[R_t1b[tb]])
                em.op('dve', lambda e, ps=ps, tb=tb, g=g: e.scalar_tensor_tensor(out=t1b[tb], in0=ps[:, :], scalar=dc[:, 192 + g:193 + g], in1=t1b[tb],
                                                                                 op0=ALU.mult, op1=ALU.add), reads=[psr, R_dc, R_t1b[tb]], writes=[R_t1b[tb]])
                em.op('pool', lambda e, seg=seg, tb=tb: e.tensor_tensor(out=yhy[:, seg], in0=t1b[tb], in1=X0C[:, seg], op=ALU.mult),
                      reads=[R_t1b[tb], R_X0C], writes=[R_yhy])
        em.dma('sp', lambda e, g=g: e.dma_start(out=ysc[g], in_=yhy), reads=[R_yhy], stream='yst', depth=2)
        em.barrier()


def build_row(em, L, dst, R_dst, colfn, ncols, tmpd, R_tmpd, reads):
    PSB = L['PSB']; PSR = L['PSR']; identf = L['identf']; onesf = L['onesf']; R_cf = L['R_cf']
    for k4 in range(ncols // 4):
        ps = PSB[6 + k4 % 2]; psr = PSR[6 + k4 % 2]
        for j in range(4):
            kc = k4 * 4 + j; tb = kc % 2
            em.op('dve', lambda e, kc=kc, tb=tb: e.tensor_scalar(out=tmpd[tb], in0=identf, scalar1=colfn(kc), scalar2=None, op0=ALU.mult),
                  reads=[R_cf] + reads, writes=[R_tmpd[tb]])
            em.op('pe', lambda e, j=j, tb=tb, ps=ps: e.matmul(ps[:, j * 128:(j + 1) * 128], lhsT=onesf, rhs=tmpd[tb], start=True, stop=True),
                  reads=[R_cf, R_tmpd[tb]], writes=[psr])
        copy_op(em, 'act', dst[:, k4 * 512:(k4 + 1) * 512], ps[:, :], [psr], [R_dst])


def phase_out(nc, em, L, L2):
    carve = L['carve']; PSB = L['PSB']; PSR = L['PSR']; dc = L['dc']
    R_dc = L['R_dc']; R_modc = L['R_modc']; MODX = L['MODX']
    ysc = L['ysc']; wout = L['wout']; acc = L['acc']; xtok = L['xtok']; rows3_d = L['rows3_d']
    KB = 256
    wstg = [carve(16 * i * KB, [8, 512]) for i in range(2)]; R_wstg = [Reg(f'cw{i}') for i in range(2)]
    wbf = carve(32 * KB, [32, 512], BF16); R_wbf = Reg('cwbf')
    yt = [carve((64 + 32 * i) * KB, [32, 512], BF16) for i in range(2)]; R_yt = [Reg(f'yt{i}') for i in range(2)]
    gt1r = carve(128 * KB, [D]); R_gt1r = Reg('gt1r')
    gbr = carve(144 * KB, [D]); R_gbr = Reg('gbr')
    xt = [carve((160 + 2 * i) * KB, [512]) for i in range(3)]; R_xt = [Reg(f'xt{i}') for i in range(3)]
    ot = [carve((166 + 2 * i) * KB, [512]) for i in range(3)]; R_ot = [Reg(f'ot{i}') for i in range(3)]
    tmpd = [carve(172 * KB + i * 128, [128]) for i in range(2)]; R_tmpd = [Reg(f'td{i}') for i in range(2)]
    brow = carve(176 * KB, [D]); R_brow = Reg('brow')
    em.dma('sp', lambda e: e.dma_start(out=brow, in_=rows3_d[2]), writes=[R_brow], stream='cl', depth=2)
    build_row(em, L, gt1r, R_gt1r, lambda kc: MODX(64, kc), 32, tmpd, R_tmpd, [R_modc])
    em.op('dve', lambda e: e.tensor_tensor(out=gbr, in0=gt1r, in1=brow, op=ALU.mult), reads=[R_gt1r, R_brow], writes=[R_gbr])
    yv = ysc.rearrange("k p t -> p k t")
    n = 0
    for ct in range(8):
        for q in range(4):
            sb = q % 2
            em.dma('sp', lambda e, sb=sb, ct=ct, q=q: e.dma_start(out=wstg[sb], in_=wout[ct][:, q * 8:(q + 1) * 8, :]), writes=[R_wstg[sb]],
                   stream='cw', depth=2)
            copy_op(em, 'act' if q % 2 == 0 else 'pool', wbf[:, q * 8:(q + 1) * 8, :], wstg[sb], [R_wstg[sb]], [R_wbf])
        csl = slice(ct * 512, (ct + 1) * 512)
        for t8 in range(8):
            yb = (ct * 8 + t8) % 2
            em.dma('sp', lambda e, yb=yb, t8=t8: e.dma_start(out=yt[yb], in_=yv[:, :, t8 * 512:(t8 + 1) * 512]), writes=[R_yt[yb]], stream='cy', depth=2)
            for sub in range(4):
                t0 = t8 * 512 + sub * 128
                pi = n % 4; ps = PSB[pi]; psr = PSR[pi]
                xb = n % 3; n += 1
                em.dma('act', lambda e, xb=xb, t0=t0, csl=csl: e.dma_start(out=xt[xb], in_=xtok[t0:t0 + 128, csl]), writes=[R_xt[xb]], stream='cx', depth=3)
                for kc in range(KC):
                    em.op('pe', lambda e, kc=kc, yb=yb, sub=sub, ps=ps: e.matmul(ps[:, :], lhsT=yt[yb][:, kc, sub * 128:(sub + 1) * 128], rhs=wbf[:, kc, :],
                                                                                 start=(kc == 0), stop=(kc == KC - 1)),
                          reads=[R_yt[yb], R_wbf], writes=[psr])
                em.op('dve', lambda e, ps=ps, xb=xb, csl=csl: e.tensor_tensor(out=ot[xb], in0=ps[:, :], in1=gt1r[:, csl], op=ALU.mult),
                      reads=[psr, R_gt1r], writes=[R_ot[xb]])
                em.op('pool', lambda e, xb=xb, csl=csl: e.tensor_tensor(out=xt[xb], in0=xt[xb], in1=gbr[:, csl], op=ALU.add),
                      reads=[R_xt[xb], R_gbr], writes=[R_xt[xb]])
                em.op('dve', lambda e, xb=xb: e.tensor_tensor(out=ot[xb], in0=ot[xb], in1=xt[xb], op=ALU.add),
                      reads=[R_ot[xb], R_xt[xb]], writes=[R_ot[xb]])
                em.dma('pool', lambda e, xb=xb, ct=ct, t0=t0: e.dma_start(out=acc[ct][t0:t0 + 128, :], in_=ot[xb]), reads=[R_ot[xb]], stream='co', depth=3)
    em.barrier()


def phase_moe(nc, em, L, L2):
    carve = L['carve']; PSB = L['PSB']; PSR = L['PSR']; dc = L['dc']; pc = L['pc']
    R_dc = L['R_dc']; R_modc = L['R_modc']; R_pc = L['R_pc']; R_tabs = L['R_tabs']; R_cf = L['R_cf']; MODX = L['MODX']
    identf = L['identf']; identb = L['identb']
    acc = L['acc']; hx2s = L['hx2s']; wr_d = L['wr_d']; wg = L['wg']; wu = L['wu']; wd = L['wd']; rows3_d = L['rows3_d']; out_d = L['out_d']
    dbg = L['dbg']
    KB = 256
    accv = acc.rearrange("c t j -> t c j")
    A2r = carve(0, [D]); R_A2r = Reg('A2r'); sh2r = carve(16 * KB, [D]); R_sh2r = Reg('sh2r')
    x1t = [carve((32 + 16 * i) * KB, [8, 512]) for i in range(2)]; R_x1t = [Reg(f'x1t{i}') for i in range(2)]
    junk = carve(64 * KB, [D], BF16); R_junk = Reg('junk')
    hx2b = [carve((72 + 8 * i) * KB, [D], BF16) for i in range(2)]; R_hx2b = [Reg(f'hx2b{i}') for i in range(2)]
    hx2T = carve(88 * KB, [32, 128], BF16); R_hx2T = Reg('hx2T')
    wrf = carve(96 * KB, [32, 16]); R_wrf = Reg('wrf'); wrb = carve(98 * KB, [32, 16], BF16); R_wrb = Reg('wrb')
    affT = carve(100 * KB, [T], parts=16); R_affT = Reg('affT')
    tmpd = [carve(116 * KB + i * 128, [128]) for i in range(2)]; R_tmpd = [Reg(f'td{i}') for i in range(2)]
    sm = carve(117 * KB, [64]); R_sm = Reg('sm')
    lg = carve(118 * KB, [16]); R_lg = Reg('lg'); aff = carve(118 * KB + 16, [16]); R_aff = Reg('aff')
    build_row(em, L, A2r, R_A2r, lambda kc: dc[:, 64 + kc:65 + kc], 32, tmpd, R_tmpd, [R_dc])
    build_row(em, L, sh2r, R_sh2r, lambda kc: MODX(96, kc), 32, tmpd, R_tmpd, [R_modc])
    em.dma('sp', lambda e: e.dma_start(out=wrf, in_=wr_d), writes=[R_wrf], stream='ml', depth=2)
    copy_op(em, 'dve', wrb, wrf, [R_wrf], [R_wrb])
    NT = int(os.environ.get('KTT', '32'))
    for tt in range(NT):
        xb = tt % 2; t0 = tt * 128
        em.dma('sp', lambda e, xb=xb, t0=t0: e.dma_start(out=x1t[xb], in_=accv[t0:t0 + 128]), writes=[R_x1t[xb]], stream='x1', depth=2)
        x1f = x1t[xb].rearrange("p a b -> p (a b)")
        em.op('act', lambda e, x1f=x1f: e.activation(out=junk, in_=x1f, func=AF.Square, accum_out=sm[:, 0:1]), reads=[R_x1t[xb]], writes=[R_junk, R_sm])
        em.op('act', lambda e: e.activation(out=sm[:, 1:2], in_=sm[:, 0:1], func=AF.Sqrt, scale=1.0 / D, bias=EPS), reads=[R_sm], writes=[R_sm])
        em.op('dve', lambda e: e.reciprocal(out=sm[:, 1:2], in_=sm[:, 1:2]), reads=[R_sm], writes=[R_sm])
        em.op('dve', lambda e, x1f=x1f: e.scalar_tensor_tensor(out=x1f, in0=x1f, scalar=sm[:, 1:2], in1=A2r, op0=ALU.mult, op1=ALU.mult),
              reads=[R_x1t[xb], R_sm, R_A2r], writes=[R_x1t[xb]])
        em.op('pool', lambda e, x1f=x1f, xb=xb: e.tensor_tensor(out=hx2b[xb], in0=x1f, in1=sh2r, op=ALU.add), reads=[R_x1t[xb], R_sh2r], writes=[R_hx2b[xb]])
        em.dma('pool', lambda e, xb=xb, t0=t0: e.dma_start(out=hx2s[t0:t0 + 128, :], in_=hx2b[xb]), reads=[R_hx2b[xb]], stream='h2', depth=2)
        for k8 in range(4):
            ps = PSB[k8 % 2]; psr = PSR[k8 % 2]
            psb = ps[:, :].bitcast(BF16)
            for j in range(8):
                kc = k8 * 8 + j
                em.op('pe', lambda e, kc=kc, j=j, xb=xb, psb=psb: e.transpose(out=psb[:, j * 128:(j + 1) * 128], in_=hx2b[xb][:, kc * 128:(kc + 1) * 128], identity=identb),
                      reads=[R_hx2b[xb], R_tabs], writes=[psr])
            copy_op(em, 'act' if k8 % 2 == 0 else 'dve', hx2T[:, k8 * 8:(k8 + 1) * 8, :], psb.rearrange("p (j t) -> p j t", j=8), [psr], [R_hx2T])
        ps = PSB[2]; psr = PSR[2]
        for kc in range(KC):
            em.op('pe', lambda e, kc=kc, ps=ps: e.matmul(ps[:, 0:16], lhsT=hx2T[:, kc, :], rhs=wrb[:, kc, :], start=(kc == 0), stop=(kc == KC - 1)),
                  reads=[R_hx2T, R_wrb], writes=[psr])
        copy_op(em, 'dve', lg, ps[:, 0:16], [psr], [R_lg])
        em.op('dve', lambda e: e.tensor_reduce(out=sm[:, 2:3], in_=lg, axis=AX.X, op=ALU.max, negate=True), reads=[R_lg], writes=[R_sm])
        em.op('act', lambda e: e.activation(out=aff, in_=lg, func=AF.Exp, bias=sm[:, 2:3], accum_out=sm[:, 3:4]), reads=[R_lg, R_sm], writes=[R_aff, R_sm])
        em.op('dve', lambda e: e.reciprocal(out=sm[:, 4:5], in_=sm[:, 3:4]), reads=[R_sm], writes=[R_sm])
        em.op('dve', lambda e: e.tensor_scalar(out=aff, in0=aff, scalar1=sm[:, 4:5], scalar2=None, op0=ALU.mult), reads=[R_aff, R_sm], writes=[R_aff])
        ps3 = PSB[3]; psr3 = PSR[3]
        em.op('pe', lambda e, ps3=ps3: e.transpose(out=ps3[0:16, 0:128], in_=aff, identity=identf), reads=[R_aff, R_cf], writes=[psr3])
        copy_op(em, 'act', affT[0:16, t0:t0 + 128], ps3[0:16, 0:128], [psr3], [R_affT])
    if STAGE < 99:
        em.dma('sp', lambda e: e.dma_start(out=dbg['d_a'][0:16, :], in_=affT), reads=[R_affT], stream='dbg', depth=2)
    em.barrier()
    if STAGE < 9:
        return
    work = carve(0, [T], parts=16); R_work = Reg('work')
    vals = carve(16 * KB, [CAP], parts=16); R_vals = Reg('vals')
    idxs = carve(18 * KB, [CAP], U32, parts=16); R_idxs = Reg('idxs')
    idxf = carve(20 * KB, [CAP], parts=16); R_idxf = Reg('idxf')
    IDT_OFF = 176 * KB
    idxT = carve(IDT_OFF, [64], U32); R_idxT = Reg('idxT')
    gateT = carve(IDT_OFF + 64, [64]); R_gateT = Reg('gateT')
    idxTf = carve(IDT_OFF + 128, [64]); R_idxTf = Reg('idxTf')
    copy_op(em, 'dve', work, affT, [R_affT], [R_work])
    for it in range(CAP // 8):
        sl = slice(it * 8, it * 8 + 8)
        em.op('dve', lambda e, sl=sl: e.max(out=vals[:, sl], in_=work), reads=[R_work], writes=[R_vals])
        em.op('dve', lambda e, sl=sl: e.max_index(out=idxs[:, sl], in_max=vals[:, sl], in_values=work), reads=[R_work, R_vals], writes=[R_idxs])
        em.op('dve', lambda e, sl=sl: e.match_replace(out=work, in_to_replace=vals[:, sl], in_values=work, imm_value=-1.0),
              reads=[R_work, R_vals], writes=[R_work])
    copy_op(em, 'dve', idxf, idxs, [R_idxs], [R_idxf])
    for st in range(4):
        ps = PSB[st % 2]; psr = PSR[st % 2]
        em.op('pe', lambda e, st=st, ps=ps: e.transpose(out=ps[:, 0:16], in_=idxf[0:16, st * 128:(st + 1) * 128], identity=identf[0:16, 0:16]),
              reads=[R_idxf, R_cf], writes=[psr])
        em.op('pe', lambda e, st=st, ps=ps: e.transpose(out=ps[:, 16:32], in_=vals[0:16, st * 128:(st + 1) * 128], identity=identf[0:16, 0:16]),
              reads=[R_vals, R_cf], writes=[psr])
        copy_op(em, 'dve', idxTf[:, st * 16:(st + 1) * 16], ps[:, 0:16], [psr], [R_idxTf])
        copy_op(em, 'dve', gateT[:, st * 16:(st + 1) * 16], ps[:, 16:32], [psr], [R_gateT])
    copy_op(em, 'dve', idxT, idxTf, [R_idxTf], [R_idxT])
    if STAGE < 99:
        em.dma('sp', lambda e: e.dma_start(out=dbg['d_b'][0:16, 0:512], in_=idxf), reads=[R_idxf], stream='dbg', depth=2)
        em.dma('sp', lambda e: e.dma_start(out=dbg['d_b'][16:32, 0:512], in_=vals), reads=[R_vals], stream='dbg', depth=2)
    em.barrier()
    if STAGE < 10:
        return
    XT = carve(0, [32, 512], BF16); R_XT = Reg('XT')
    actT = carve(32 * KB, [16, 512], BF16); R_actT = Reg('actT')
    wst = [carve((48 + 16 * i) * KB, [32, 128]) for i in range(2)]; R_wst = [Reg(f'mw{i}') for i in range(2)]
    wgb = [carve((80 + 8 * i) * KB, [32, 128], BF16) for i in range(2)]; R_wgb = [Reg(f'wgb{i}') for i in range(2)]
    wub = [carve((96 + 8 * i) * KB, [32, 128], BF16) for i in range(2)]; R_wub = [Reg(f'wub{i}') for i in range(2)]
    wdb = [carve((112 + 16 * i) * KB, [16, 512], BF16) for i in range(2)]; R_wdb = [Reg(f'wdb{i}') for i in range(2)]
    Xg = carve(112 * KB, [4, D], BF16)
    R_Xg = [R_wdb[0], R_wdb[0], R_wdb[1], R_wdb[1]]
    stmp = [carve((144 + 2 * i) * KB, [512]) for i in range(2)]; R_stmp = [Reg(f'st{i}') for i in range(2)]
    yo = [carve((148 + 2 * i) * KB, [512]) for i in range(4)]; R_yo = [Reg(f'yo{i}') for i in range(4)]
    gt2r = carve(156 * KB, [D]); R_gt2r = Reg('gt2r')
    tmpd2 = [carve(180 * KB + i * 128, [128]) for i in range(2)]; R_tmpd2 = [Reg(f'td2{i}') for i in range(2)]
    build_row(em, L, gt2r, R_gt2r, lambda kc: MODX(160, kc), 32, tmpd2, R_tmpd2, [R_modc])
    R_acc = [Reg(f'acc{i}') for i in range(8)]
    R_wsth = [[Reg(f'mwh{i}{j}') for j in range(2)] for i in range(2)]
    NEX = int(os.environ.get('KEXP', '16'))
    nyo = 0; nw = 0
    for ex in range(NEX):
        for st in range(4):
            em.dma('pool', lambda e, st=st, ex=ex: e.indirect_dma_start(out=Xg[:, st, :], out_offset=None, in_=hx2s,
                                                                       in_offset=bass.IndirectOffsetOnAxis(ap=idxT[:, st * 16 + ex:st * 16 + ex + 1], axis=0)),
                   reads=[R_idxT], writes=[R_Xg[st]], stream='gx', depth=2)
        for st in range(4):
            for k8 in range(4):
                ps = PSB[k8 % 2]; psr = PSR[k8 % 2]
                psb = ps[:, :].bitcast(BF16)
                for j in range(8):
                    kc = k8 * 8 + j
                    em.op('pe', lambda e, kc=kc, j=j, st=st, psb=psb: e.transpose(out=psb[:, j * 128:(j + 1) * 128], in_=Xg[:, st, kc * 128:(kc + 1) * 128], identity=identb),
                          reads=[R_Xg[st], R_tabs], writes=[psr])
                copy_op(em, 'act' if k8 % 2 == 0 else 'dve', XT[:, k8 * 8:(k8 + 1) * 8, st * 128:(st + 1) * 128], psb.rearrange("p (j t) -> p j t", j=8), [psr], [R_XT])
        for fc in range(16):
            wb = fc % 2
            for hh in range(2):
                hs = slice(hh * 16, (hh + 1) * 16)
                em.dma('sp', lambda e, ex=ex, fc=fc, hs=hs: e.dma_start(out=wst[0][:, hs, :], in_=wg[ex, fc][:, hs, :]), writes=[R_wsth[0][hh]],
                       stream='mwg%d' % hh, depth=1)
                copy_op(em, 'act', wgb[wb][:, hs, :], wst[0][:, hs, :], [R_wsth[0][hh]], [R_wgb[wb]])
                em.dma('sp', lambda e, ex=ex, fc=fc, hs=hs: e.dma_start(out=wst[1][:, hs, :], in_=wu[ex, fc][:, hs, :]), writes=[R_wsth[1][hh]],
                       stream='mwu%d' % hh, depth=1)
                copy_op(em, 'dve', wub[wb][:, hs, :], wst[1][:, hs, :], [R_wsth[1][hh]], [R_wub[wb]])
            psg = PSB[2 + (fc % 2) * 2]; psrg = PSR[2 + (fc % 2) * 2]; psu = PSB[3 + (fc % 2) * 2]; psru = PSR[3 + (fc % 2) * 2]
            for kc in range(KC):
                em.op('pe', lambda e, kc=kc, psg=psg, wb=wb: e.matmul(psg[:, :], lhsT=wgb[wb][:, kc, :], rhs=XT[:, kc, :], start=(kc == 0), stop=(kc == KC - 1)),
                      reads=[R_wgb[wb], R_XT], writes=[psrg])
            for kc in range(KC):
                em.op('pe', lambda e, kc=kc, psu=psu, wb=wb: e.matmul(psu[:, :], lhsT=wub[wb][:, kc, :], rhs=XT[:, kc, :], start=(kc == 0), stop=(kc == KC - 1)),
                      reads=[R_wub[wb], R_XT], writes=[psru])
            sb = fc % 2
            em.op('act', lambda e, sb=sb, psg=psg: e.activation(out=stmp[sb], in_=psg[:, :], func=AF.Silu), reads=[psrg], writes=[R_stmp[sb]])
            em.op('dve', lambda e, sb=sb, psu=psu, fc=fc: e.tensor_tensor(out=actT[:, fc, :], in0=psu[:, :], in1=stmp[sb], op=ALU.mult),
                  reads=[psru, R_stmp[sb]], writes=[R_actT])
        for ct in range(8):
            db = ct % 2
            for hq in range(2):
                wv = wst[hq].rearrange("p (a b) c -> p a (b c)", a=8)
                em.dma('sp', lambda e, hq=hq, ex=ex, ct=ct, wv=wv: e.dma_start(out=wv, in_=wd[ex, ct][:, hq * 8:(hq + 1) * 8, :]),
                       writes=[R_wst[hq]] + R_wsth[hq], stream='mw%d' % hq, depth=1)
                copy_op(em, 'act' if hq == 0 else 'dve', wdb[db][:, hq * 8:(hq + 1) * 8, :], wv, [R_wst[hq]] + R_wsth[hq], [R_wdb[db]])
            for st in range(4):
                pi = (ct * 4 + st) % 2; ps = PSB[pi]; psr = PSR[pi]
                for fk in range(16):
                    em.op('pe', lambda e, fk=fk, st=st, ps=ps, db=db: e.matmul(ps[:, :], lhsT=actT[:, fk, st * 128:(st + 1) * 128], rhs=wdb[db][:, fk, :],
                                                                               start=(fk == 0), stop=(fk == 15)), reads=[R_actT, R_wdb[db]], writes=[psr])
                yb = nyo % 4; nyo += 1
                em.op('dve', lambda e, yb=yb, ps=ps, st=st, ex=ex, ct=ct: e.scalar_tensor_tensor(
                    out=yo[yb], in0=ps[:, :], scalar=gateT[:, st * 16 + ex:st * 16 + ex + 1], in1=gt2r[:, ct * 512:(ct + 1) * 512], op0=ALU.mult, op1=ALU.mult),
                    reads=[psr, R_gateT, R_gt2r], writes=[R_yo[yb]])
                em.dma('pool', lambda e, yb=yb, st=st, ex=ex, ct=ct: e.indirect_dma_start(
                    out=acc.rearrange("c t j -> (c t) j"), out_offset=bass.IndirectOffsetOnAxis(ap=idxT[:, st * 16 + ex:st * 16 + ex + 1], axis=0),
                    in_=yo[yb], in_offset=None, element_offset=ct * T * 512, compute_op=ALU.add),
                    reads=[R_yo[yb], R_idxT], writes=[R_acc[ct]], stream='sc', depth=4)
    em.barrier()
    if STAGE < 11:
        return
    gfr = carve(0, [D]); R_gfr = Reg('gfr')
    x2t = [carve((16 + 16 * i) * KB, [8, 512]) for i in range(2)]; R_x2t = [Reg(f'x2t{i}') for i in range(2)]
    junk2 = carve(48 * KB, [D], BF16); R_junk2 = Reg('junk2')
    ofin = [carve((56 + 16 * i) * KB, [D]) for i in range(2)]; R_ofin = [Reg(f'of{i}') for i in range(2)]
    sm2 = carve(90 * KB, [8]); R_sm2 = Reg('sm2')
    em.dma('sp', lambda e: e.dma_start(out=gfr, in_=rows3_d[1]), writes=[R_gfr], stream='ml', depth=2)
    for tt in range(32):
        xb = tt % 2; t0 = tt * 128
        em.dma('sp', lambda e, xb=xb, t0=t0: e.dma_start(out=x2t[xb], in_=accv[t0:t0 + 128]), reads=R_acc, writes=[R_x2t[xb]], stream='x2', depth=2)
        x2f = x2t[xb].rearrange("p a b -> p (a b)")
        em.op('act', lambda e, x2f=x2f: e.activation(out=junk2, in_=x2f, func=AF.Square, accum_out=sm2[:, 0:1]), reads=[R_x2t[xb]], writes=[R_junk2, R_sm2])
        em.op('act', lambda e: e.activation(out=sm2[:, 1:2], in_=sm2[:, 0:1], func=AF.Sqrt, scale=1.0 / D, bias=EPS), reads=[R_sm2], writes=[R_sm2])
        em.op('dve', lambda e: e.reciprocal(out=sm2[:, 1:2], in_=sm2[:, 1:2]), reads=[R_sm2], writes=[R_sm2])
        em.op('dve', lambda e, x2f=x2f, xb=xb: e.scalar_tensor_tensor(out=ofin[xb], in0=x2f, scalar=sm2[:, 1:2], in1=gfr, op0=ALU.mult, op1=ALU.mult),
              reads=[R_x2t[xb], R_sm2, R_gfr], writes=[R_ofin[xb]])
        em.dma('pool', lambda e, xb=xb, t0=t0: e.dma_start(out=out_d[t0:t0 + 128, :], in_=ofin[xb]), reads=[R_ofin[xb]], stream='fo', depth=2)
    em.barrier()


def kernel(**inputs):
    maps, poff, toff = prep_inputs(inputs)
    ntab = maps[0]['tabs'].shape[1]; npc = maps[0]['pcols'].shape[1]
    nc = build(poff, toff, ntab, npc)
    res = run_bass_kernel_spmd(nc, maps, core_ids=list(range(NCORES)))
    if STAGE < 99:
        return res
    return np.stack([r['out'] for r in res.results], 0)
```

```python
import os, math
import numpy as np
import ml_dtypes
from contextlib import ExitStack
import concourse.bass as bass
import concourse.mybir as mybir
from concourse.bass_utils import run_bass_kernel_spmd

F32 = mybir.dt.float32; BF16 = mybir.dt.bfloat16; U32 = mybir.dt.uint32; I32 = mybir.dt.int32
AF = mybir.ActivationFunctionType; ALU = mybir.AluOpType; AX = mybir.AxisListType
NPBF = ml_dtypes.bfloat16

D = 4096; T = 4096; NB = 4; KC = 32; NCTX = 256
DIN = 10240; NE = 16; FF = 2048; CAP = 512
EPS = 1e-6
MIN_DECAY = math.log(1e-2) / 1.5; MAX_DECAY = math.log(1e-2) / 0.3
MAGIC = 12582912.0
TWO_PI = 2.0 * math.pi
STAGE = int(os.environ.get("KSTAGE", "99"))
NCORES = int(os.environ.get('KCORES', '4'))

ENGS = {'pe': 'tensor', 'dve': 'vector', 'act': 'scalar', 'pool': 'gpsimd', 'sp': 'sync'}


class Reg:
    __slots__ = ('name', 'w', 'r')

    def __init__(self, name):
        self.name = name; self.w = None; self.r = {}


class Em:
    def __init__(self, nc, es):
        self.nc = nc; self.es = es
        self.q = {e: [] for e in ENGS}
        self.cnt = {e: 0 for e in ENGS}
        self.nsem = 0
        self.sem = {e: self._newsem("e_" + e) for e in ENGS}
        self.mine = {e: {id(self.sem[e])} for e in ENGS}
        self.seen = {e: {} for e in ENGS}
        self.streams = {}
        self.allsems = {}

    def _newsem(self, name):
        self.nsem += 1
        return self.es.enter_context(self.nc.semaphore(f"{name}_{self.nsem}"))

    def _wait(self, eng, tok):
        sem, val = tok
        if eng == 'pe' and id(sem) in self.mine['pe']:
            return
        if self.seen[eng].get(id(sem), 0) >= val:
            return
        self.seen[eng][id(sem)] = val
        self.q[eng].append(('w', sem, val))

    def _deps(self, eng, reads, writes):
        for r in reads:
            if r.w is not None:
                self._wait(eng, r.w)
        for w in writes:
            if w.w is not None:
                self._wait(eng, w.w)
            for t in w.r.values():
                self._wait(eng, t)

    def _mark(self, tok, reads, writes):
        k = id(tok[0])
        for r in reads:
            if k not in r.r or r.r[k][1] < tok[1]:
                r.r[k] = tok
        for w in writes:
            w.w = tok; w.r = {}
        self.allsems[k] = tok

    def op(self, eng, fn, reads=(), writes=()):
        self._deps(eng, reads, writes)
        if self.cnt[eng] >= 30000:
            self.sem[eng] = self._newsem("e_" + eng); self.mine[eng].add(id(self.sem[eng])); self.cnt[eng] = 0
        self.cnt[eng] += 1
        tok = (self.sem[eng], self.cnt[eng])
        self.q[eng].append(('o', fn, tok[0]))
        self._mark(tok, reads, writes)

    def dma(self, eng, fn, reads=(), writes=(), stream='d', depth=2):
        self._deps(eng, reads, writes)
        st = self.streams.setdefault(stream, {'n': 0, 'slots': []})
        i = st['n'] % depth; st['n'] += 1
        if i >= len(st['slots']):
            st['slots'].append([self._newsem("d_" + stream), 0])
        slot = st['slots'][i]
        if slot[1] > 0:
            self._wait(eng, (slot[0], slot[1]))
        if slot[1] >= 60000:
            raise RuntimeError("dma sem overflow " + stream)
        slot[1] += 16
        tok = (slot[0], slot[1])
        self.q[eng].append(('d', fn, tok[0]))
        self._mark(tok, reads, writes)

    def barrier(self):
        toks = list(self.allsems.values())
        for e in ENGS:
            for t in toks:
                self._wait(e, t)

    def run(self, block):
        def mk(name):
            items = self.q[name]

            def f(e):
                for it in items:
                    if it[0] == 'w':
                        e.wait_ge(it[1], it[2])
                    elif it[0] == 'o':
                        it[1](e).then_inc(it[2], 1)
                    else:
                        it[1](e).then_inc(it[2], 16)
            return f
        block.tensor(mk('pe')); block.vector(mk('dve')); block.scalar(mk('act'))
        block.gpsimd(mk('pool')); block.sync(mk('sp'))


def col_layout(v):
    v = np.asarray(v, np.float32).reshape(-1, 128)
    return np.ascontiguousarray(v.T)


class ColPack:
    def __init__(self):
        self.blocks = []; self.off = {}; self.n = 0

    def add(self, name, arr):
        arr = np.asarray(arr, np.float32)
        assert arr.shape[0] == 128
        arr = arr.reshape(128, -1)
        self.off[name] = self.n; self.blocks.append(arr); self.n += arr.shape[1]

    def build(self):
        return np.ascontiguousarray(np.concatenate(self.blocks, axis=1))


def pad128(a):
    out = np.zeros((128,) + a.shape[1:], a.dtype); out[:a.shape[0]] = a; return out


def dft_tables():
    f = np.arange(128)
    r = np.arange(64)
    th = 2 * np.pi * np.outer(r, f) / 128.0
    T1d = np.concatenate([np.cos(th), np.sin(th)], 1)
    T2ad = T1d.copy()
    T2bd = np.concatenate([-np.sin(th), np.cos(th)], 1)
    dk = np.arange(127) - 63
    thk = 2 * np.pi * np.outer(dk, f) / 128.0
    T1k = np.concatenate([np.cos(thk), np.sin(thk)], 1)
    T2ak = T1k.copy()
    T2bk = np.concatenate([-np.sin(thk), np.cos(thk)], 1)
    thi = 2 * np.pi * np.outer(f, r) / 128.0
    I2a = np.concatenate([np.cos(thi), np.sin(thi)], 1)
    I2b = np.concatenate([np.sin(thi), -np.cos(thi)], 1)
    I1a = np.cos(thi) / 16384.0
    I1b = -np.sin(thi) / 16384.0
    blocks = [pad128(T1d), pad128(T2ad), pad128(T2bd), pad128(T1k), pad128(T2ak), pad128(T2bk), I2a, I2b, I1a, I1b,
              np.eye(128), np.ones((128, 128))]
    names = ['T1d', 'T2ad', 'T2bd', 'T1k', 'T2ak', 'T2bk', 'I2a', 'I2b', 'I1a', 'I1b', 'identb', 'onesb']
    off = {}; n = 0
    for nm, b in zip(names, blocks):
        off[nm] = n; n += b.shape[1]
    tab = np.concatenate(blocks, 1).astype(np.float32).astype(NPBF)
    return tab, off


def filter_consts():
    n = T
    pos = np.arange(n, dtype=np.float32)
    t = np.linspace(0.0, 1.0, n, dtype=np.float32)
    bands = np.linspace(1e-4, 15, 16, dtype=np.float32)
    ang = (np.float32(2.0 * math.pi) * pos / np.float32(n))[:, None] * bands[None, :]
    z = np.concatenate([t[:, None], np.cos(ang), -np.sin(ang)], axis=-1).astype(np.float32)
    zT = pad128(np.ascontiguousarray(z.T))
    trow = np.ascontiguousarray(np.broadcast_to(t[None, :], (128, n))).astype(np.float32)
    delta = np.abs(np.linspace(MIN_DECAY, MAX_DECAY, 2048, dtype=np.float32))
    return zT, trow, delta


def prep_inputs(inp):
    g = lambda k: np.asarray(inp[k])
    sh = {}
    w_mod = g('w_mod')[0]
    sh['wmod'] = np.ascontiguousarray(w_mod.reshape(32, 128, 48, 512).transpose(2, 1, 0, 3))
    w_in = g('w_in')[0]
    sh['win'] = np.ascontiguousarray(w_in.reshape(32, 128, 80, 128).transpose(2, 1, 0, 3))
    w_out = g('w_out')[0]
    sh['wout'] = np.ascontiguousarray(w_out.reshape(32, 128, 8, 512).transpose(2, 1, 0, 3))
    sh['wg'] = np.ascontiguousarray(g('w_exp_gate')[0].reshape(16, 32, 128, 16, 128).transpose(0, 3, 2, 1, 4))
    sh['wu'] = np.ascontiguousarray(g('w_exp_up')[0].reshape(16, 32, 128, 16, 128).transpose(0, 3, 2, 1, 4))
    sh['wd'] = np.ascontiguousarray(g('w_exp_down')[0].reshape(16, 16, 128, 8, 512).transpose(0, 3, 2, 1, 4))
    sh['wr'] = np.ascontiguousarray(g('w_router')[0].reshape(32, 128, 16).transpose(1, 0, 2))
    wa = g('lru_wa')[0]; wx = g('lru_wx')[0]
    lw = np.stack([wa[0], wa[1], wx[0], wx[1]], 0)
    sh['lruw'] = np.ascontiguousarray(lw.transpose(1, 2, 0, 3))
    cp = ColPack()
    cp.add('b_mod', col_layout(g('b_mod')[0]))
    cp.add('g_mix', col_layout(g('g_mix')[0]))
    cp.add('g_ffn', col_layout(g('g_ffn')[0]))
    cp.add('b_in', col_layout(g('b_in')[0]))
    cp.add('hy_conv_b', col_layout(g('hy_conv_b')[0]))
    hcw = g('hy_conv_w')[0]
    cp.add('hy_conv_w', np.stack([col_layout(hcw[k]) for k in range(3)], -1))
    cp.add('hy_bias', col_layout(g('hy_bias')[0]))
    lcw = g('lru_conv_w')[0]
    cp.add('lru_conv_w', np.stack([col_layout(lcw[k]) for k in range(4)], -1))
    cp.add('lru_conv_b', col_layout(g('lru_conv_b')[0]))
    for nm in ['lru_ba', 'lru_bx', 'lru_lambda']:
        a = g(nm)[0]
        cp.add(nm, np.stack([col_layout(a[0]), col_layout(a[1])], 1))
    zT, trow, delta = filter_consts()
    cp.add('delta', col_layout(delta))
    for nm in ['hy_f_freq', 'hy_f_b1', 'hy_f_b2', 'hy_f_b3']:
        cp.add(nm, pad128(g(nm)[0].reshape(64, 1)))
    sh['pcols'] = cp.build()
    fw = np.zeros((128, 3, 64), np.float32)
    fw[:33, 0] = g('hy_f_w1')[0]; fw[:64, 1] = g('hy_f_w2')[0]; fw[:64, 2] = g('hy_f_w3')[0]
    sh['fw'] = fw
    sh['fwout'] = pad128(g('hy_f_wout')[0])
    sh['zT'] = zT; sh['trow'] = trow
    sh['rows3'] = np.ascontiguousarray(np.stack([
        np.broadcast_to(g('g_ffn')[0][None, :], (128, D)),
        np.broadcast_to(g('g_final')[None, :], (128, D)),
        np.broadcast_to(g('b_out')[0][None, :], (128, D))], 0)).astype(np.float32)
    tab, toff = dft_tables()
    sh['tabs'] = tab
    sh['cf32'] = np.ascontiguousarray(np.concatenate([np.eye(128), np.ones((128, 128))], 1)).astype(np.float32)
    x = g('x'); c = g('c'); ctx = g('ctx'); c_ctx = g('c_ctx')
    maps = []
    for b in range(NCORES):
        m = dict(sh)
        m['xT'] = np.ascontiguousarray(x[b].T)
        m['xtok'] = np.ascontiguousarray(x[b])
        m['ctxT'] = np.ascontiguousarray(ctx[b].T)
        m['ccol'] = np.ascontiguousarray(np.stack([col_layout(c[b]), col_layout(c_ctx)], -1))
        maps.append(m)
    return maps, cp.off, toff


def build(poff, toff, ntab, npc):
    nc = bass.Bass("TRN2", target_bir_lowering=False)
    dt_in = lambda name, shape, dt=F32: nc.dram_tensor(name, list(shape), dt, kind="ExternalInput").ap()
    xT = dt_in('xT', [D, T]); xtok = dt_in('xtok', [T, D]); ctxT = dt_in('ctxT', [D, NCTX])
    ccol_d = dt_in('ccol', [128, 32, 2])
    wmod = dt_in('wmod', [48, 128, 32, 512]); win = dt_in('win', [80, 128, 32, 128])
    wout = dt_in('wout', [8, 128, 32, 512])
    wg = dt_in('wg', [16, 16, 128, 32, 128]); wu = dt_in('wu', [16, 16, 128, 32, 128])
    wd = dt_in('wd', [16, 8, 128, 16, 512]); wr_d = dt_in('wr', [128, 32, 16])
    lruw_d = dt_in('lruw', [16, 128, 4, 128])
    pcols_d = dt_in('pcols', [128, npc]); fw_d = dt_in('fw', [128, 3, 64]); fwout_d = dt_in('fwout', [128, 4096])
    zT_d = dt_in('zT', [128, T]); trow_d = dt_in('trow', [128, T]); rows3_d = dt_in('rows3', [3, 128, D])
    tabs_d = dt_in('tabs', [128, ntab], BF16); cf32_d = dt_in('cf32', [128, 256])
    out_d = nc.dram_tensor('out', [T, D], F32, kind="ExternalOutput").ap()
    scr = lambda name, shape, dt: nc.dram_tensor(name, list(shape), dt, kind=("Internal" if STAGE >= 99 else "ExternalOutput")).ap()
    hxs = scr('hxs', [16, 128, 32 * 256], BF16)
    kfs = scr('kfs', [16, 128, 128 * 256], BF16)
    ysc = scr('ysc', [32, 128, T], BF16)
    acc = scr('acc', [8, T, 512], F32)
    hx2s = scr('hx2s', [T, D], BF16)
    dbg = {}
    if STAGE < 99:
        dbg['d_mod'] = nc.dram_tensor('d_mod', [128, 384], F32, kind="ExternalOutput").ap()
        dbg['d_a'] = nc.dram_tensor('d_a', [128, T], F32, kind="ExternalOutput").ap()
        dbg['d_b'] = nc.dram_tensor('d_b', [128, T], F32, kind="ExternalOutput").ap()
        dbg['d_c'] = nc.dram_tensor('d_c', [128, T], F32, kind="ExternalOutput").ap()
        dbg['d_d'] = nc.dram_tensor('d_d', [128, T], F32, kind="ExternalOutput").ap()
        dbg['d_kk'] = nc.dram_tensor('d_kk', [128, 8192], F32, kind="ExternalOutput").ap()
        dbg['d_dc'] = nc.dram_tensor('d_dc', [128, 512], F32, kind="ExternalOutput").ap()

    es = ExitStack()
    with es:
        E = es.enter_context
        em = Em(nc, es)
        AW = 50176
        arena = E(nc.sbuf_tensor("s_arena", [128, AW], F32))
        pc = E(nc.sbuf_tensor("s_pc", [128, npc], F32))
        dc = E(nc.sbuf_tensor("s_dc", [128, 512], F32))
        modc = E(nc.sbuf_tensor("s_modc", [128, 384], F32))
        tabs = E(nc.sbuf_tensor("s_tabs", [128, ntab], BF16))
        cf32 = E(nc.sbuf_tensor("s_cf32", [128, 256], F32))
        PSB = [E(nc.psum_tensor(f"ps{i}", [128, 512], F32)) for i in range(8)]
        PSR = [Reg(f"ps{i}") for i in range(8)]
        R_pc = Reg('pc'); R_dc = Reg('dc'); R_modc = Reg('modc'); R_tabs = Reg('tabs'); R_cf = Reg('cf32')
        identf = cf32[:, 0:128]; onesf = cf32[:, 128:256]
        TB = lambda nm, rows, c0, c1: tabs[0:rows, toff[nm] + c0: toff[nm] + c1]
        identb = TB('identb', 128, 0, 128); onesb = TB('onesb', 128, 0, 128)
        modc3 = modc[:, :].rearrange("p (j two) -> p j two", two=2)
        PC = lambda nm, i, n=1: pc[:, poff[nm] + i: poff[nm] + i + n]

        def carve(off, shape, dt=F32, parts=128):
            n = 1
            for s in shape:
                n *= s
            words = n if dt == F32 or dt == U32 or dt == I32 else (n + 1) // 2
            assert off + words <= AW, (off, words, AW)
            ap = arena[0:parts, off:off + words]
            if dt != F32:
                ap = ap.bitcast(dt)
            if len(shape) == 2:
                ap = ap.rearrange("p (a b) -> p a b", a=shape[0])
            elif len(shape) == 3:
                ap = ap.rearrange("p (a b c) -> p a b c", a=shape[0], b=shape[1])
            return ap

        em.dma('sp', lambda e: e.dma_start(out=pc[:, :], in_=pcols_d), writes=[R_pc], stream='su', depth=4)
        em.dma('sp', lambda e: e.dma_start(out=tabs[:, :], in_=tabs_d), writes=[R_tabs], stream='su', depth=4)
        em.dma('sp', lambda e: e.dma_start(out=cf32[:, :], in_=cf32_d), writes=[R_cf], stream='su', depth=4)

        scol = carve(0, [32, 2]); R_scol = Reg('scol')
        em.dma('sp', lambda e: e.dma_start(out=scol, in_=ccol_d), writes=[R_scol], stream='su', depth=4)
        em.op('act', lambda e: e.activation(out=scol, in_=scol, func=AF.Silu), reads=[R_scol], writes=[R_scol])
        wst = [carve(256 + i * 4096, [8, 512]) for i in range(3)]; R_wst = [Reg(f'wst{i}') for i in range(3)]
        modrow = carve(256 + 3 * 4096, [24576], parts=2); R_modrow = Reg('modrow')
        nld = 0
        for ct in range(48):
            ps = PSB[ct % 2]
            for q in range(4):
                b = nld % 3; nld += 1
                em.dma('sp' if nld % 2 == 0 else 'act', lambda e, b=b, ct=ct, q=q: e.dma_start(out=wst[b], in_=wmod[ct][:, q * 8:(q + 1) * 8, :]),
                       writes=[R_wst[b]], stream='wm', depth=3)
                for k in range(8):
                    kc = q * 8 + k
                    em.op('pe', lambda e, b=b, k=k, kc=kc, ps=ps: e.matmul(ps[0:2, :], lhsT=scol[:, kc, :], rhs=wst[b][:, k, :],
                                                                           start=(kc == 0), stop=(kc == KC - 1)),
                          reads=[R_wst[b], R_scol], writes=[PSR[ct % 2]])
            copy_op(em, 'act', modrow[0:2, ct * 512:(ct + 1) * 512], ps[0:2, :], [PSR[ct % 2]], [R_modrow])
        for jc in range(192):
            ps = PSB[2 + jc % 2]; psr = PSR[2 + jc % 2]
            em.op('pe', lambda e, jc=jc, ps=ps: e.transpose(out=ps[:, 0:2], in_=modrow[0:2, jc * 128:(jc + 1) * 128], identity=identf[0:2, 0:2]),
                  reads=[R_modrow, R_cf], writes=[psr])
            em.op('dve', lambda e, jc=jc, ps=ps: e.tensor_scalar(out=modc3[:, jc, :], in0=ps[:, 0:2], scalar1=PC('b_mod', jc),
                                                                  scalar2=None, op0=ALU.add),
                  reads=[psr, R_pc], writes=[R_modc])
        MODX = lambda j0, kc: modc3[:, j0 + kc, 0:1]
        MODC = lambda j0, kc: modc3[:, j0 + kc, 1:2]
        em.op('dve', lambda e: e.scalar_tensor_tensor(out=dc[:, 0:32], in0=modc3[:, 32:64, 0], scalar=1.0, in1=PC('g_mix', 0, 32),
                                                      op0=ALU.add, op1=ALU.mult), reads=[R_modc, R_pc], writes=[R_dc])
        em.op('dve', lambda e: e.scalar_tensor_tensor(out=dc[:, 32:64], in0=modc3[:, 32:64, 1], scalar=1.0, in1=PC('g_mix', 0, 32),
                                                      op0=ALU.add, op1=ALU.mult), reads=[R_modc, R_pc], writes=[R_dc])
        em.op('dve', lambda e: e.scalar_tensor_tensor(out=dc[:, 64:96], in0=modc3[:, 128:160, 0], scalar=1.0, in1=PC('g_ffn', 0, 32),
                                                      op0=ALU.add, op1=ALU.mult), reads=[R_modc, R_pc], writes=[R_dc])
        em.op('act', lambda e: e.activation(out=dc[:, 96:128], in_=PC('lru_lambda', 0, 32), func=AF.Exp, scale=-1.0),
              reads=[R_pc], writes=[R_dc])
        em.op('act', lambda e: e.activation(out=dc[:, 96:128], in_=dc[:, 96:128], func=AF.Ln, bias=1.0),
              reads=[R_dc], writes=[R_dc])
        em.op('dve', lambda e: e.tensor_scalar(out=dc[:, 128:160], in0=dc[:, 96:128], scalar1=-8.0, scalar2=None, op0=ALU.mult),
              reads=[R_dc], writes=[R_dc])
        em.op('dve', lambda e: e.tensor_scalar(out=dc[:, 160:192], in0=dc[:, 96:128], scalar1=-16.0, scalar2=None, op0=ALU.mult),
              reads=[R_dc], writes=[R_dc])
        em.op('dve', lambda e: e.tensor_scalar(out=dc[:, 208:224], in0=PC('delta', 0, 16), scalar1=-1.0, scalar2=None, op0=ALU.mult),
              reads=[R_pc], writes=[R_dc])
        for i, nm in enumerate(['hy_f_b1', 'hy_f_b2', 'hy_f_b3']):
            em.op('dve', lambda e, i=i, nm=nm: e.tensor_tensor(out=dc[:, 224 + i:225 + i], in0=PC(nm, 0), in1=PC('hy_f_freq', 0),
                                                               op=ALU.mult), reads=[R_pc], writes=[R_dc])
        if STAGE < 99:
            em.dma('sp', lambda e: e.dma_start(out=dbg['d_mod'], in_=modc[:, :]), reads=[R_modc], stream='dbg', depth=2)
        em.barrier()
        if STAGE >= 2:
            phase2_onwards(nc, em, locals())
        em.barrier()
        block = E(nc.Block())
        em.run(block)
    return nc


def phase2_onwards(nc, em, L):
    globals().update({})
    carve = L['carve']; PSB = L['PSB']; PSR = L['PSR']; pc = L['pc']; dc = L['dc']; modc3 = L['modc3']
    R_pc = L['R_pc']; R_dc = L['R_dc']; R_modc = L['R_modc']; R_tabs = L['R_tabs']; R_cf = L['R_cf']
    PC = L['PC']; TB = L['TB']; identf = L['identf']; onesf = L['onesf']; identb = L['identb']; onesb = L['onesb']
    MODX = L['MODX']; MODC = L['MODC']; dbg = L['dbg']; poff = L['poff']
    xT = L['xT']; ctxT = L['ctxT']; hxs = L['hxs']; kfs = L['kfs']; ysc = L['ysc']; win = L['win']
    lruw_d = L['lruw_d']; fw_d = L['fw_d']; fwout_d = L['fwout_d']; zT_d = L['zT_d']; trow_d = L['trow_d']
    KB = 256

    HC_OFF = 180 * KB
    hc = carve(HC_OFF, [32, 256], BF16); R_hc = Reg('hc')
    xb = [carve(i * 32 * KB, [32, 256]) for i in range(2)]; R_xb = [Reg(f'xb{i}') for i in range(2)]
    sq = carve(64 * KB, [32, 256], BF16); R_sq = Reg('sq')
    hxo = [carve((80 + 16 * i) * KB, [32, 256], BF16) for i in range(2)]; R_hxo = [Reg(f'hxo{i}') for i in range(2)]
    rs = carve(112 * KB, [256]); R_rs = Reg('rs')
    xTv = xT.rearrange("(kc p) t -> p kc t", p=128)
    ctxTv = ctxT.rearrange("(kc p) t -> p kc t", p=128)
    for tt in range(17):
        b = tt % 2
        isctx = (tt == 16)
        src = ctxTv if isctx else xTv[:, :, tt * 256:(tt + 1) * 256]
        em.dma('sp', lambda e, b=b, src=src: e.dma_start(out=xb[b], in_=src), writes=[R_xb[b]], stream='xl', depth=2)
        em.op('act', lambda e, b=b: e.activation(out=sq, in_=xb[b], func=AF.Square), reads=[R_xb[b]], writes=[R_sq])
        ps = PSB[tt % 2]; psr = PSR[tt % 2]
        for kc in range(KC):
            em.op('pe', lambda e, kc=kc, ps=ps: e.matmul(ps[:, 0:256], lhsT=onesb, rhs=sq[:, kc, :], start=(kc == 0), stop=(kc == KC - 1)),
                  reads=[R_sq, R_tabs], writes=[psr])
        em.op('act', lambda e, ps=ps: e.activation(out=rs, in_=ps[:, 0:256], func=AF.Sqrt, scale=1.0 / D, bias=EPS),
              reads=[psr], writes=[R_rs])
        em.op('dve', lambda e: e.reciprocal(out=rs, in_=rs), reads=[R_rs], writes=[R_rs])
        em.op('dve', lambda e, b=b: e.tensor_tensor(out=xb[b], in0=xb[b], in1=rs[:, None, :].broadcast_to([128, 32, 256]), op=ALU.mult),
              reads=[R_xb[b], R_rs], writes=[R_xb[b]])
        dst = hc if isctx else hxo[b]; R_dst = R_hc if isctx else R_hxo[b]
        for kc in range(KC):
            sc_ap = dc[:, 32 + kc:33 + kc] if isctx else dc[:, kc:kc + 1]
            bi_ap = MODC(0, kc) if isctx else MODX(0, kc)
            em.op('act', lambda e, b=b, kc=kc, dst=dst, sc_ap=sc_ap, bi_ap=bi_ap: e.activation(
                out=dst[:, kc, :], in_=xb[b][:, kc, :], func=AF.Identity, scale=sc_ap, bias=bi_ap),
                reads=[R_xb[b], R_dc, R_modc], writes=[R_dst])
        if not isctx:
            em.dma('pool', lambda e, b=b, tt=tt: e.dma_start(out=hxs[tt].rearrange("p (a b) -> p a b", a=32), in_=hxo[b]),
                   reads=[R_hxo[b]], stream='hxst', depth=2)
    em.barrier()
    if STAGE < 3:
        return
    if not os.environ.get('KSKIPF'):
        phase_filter(nc, em, L, locals())
    if STAGE < 4:
        return
    phase_groups(nc, em, L, locals())
    if STAGE < 7:
        return
    phase_out(nc, em, L, locals())
    if STAGE < 8:
        return
    phase_moe(nc, em, L, locals())


def copy_op(em, eng, out, in_, reads, writes):
    if eng == 'act':
        em.op('act', lambda e: e.activation(out=out, in_=in_, func=AF.Copy), reads=reads, writes=writes)
    else:
        em.op(eng, lambda e: e.tensor_copy(out=out, in_=in_), reads=reads, writes=writes)


def sin_act(em, pre, R_pre, tmp, R_tmp, n):
    em.op('dve', lambda e: e.tensor_scalar(out=tmp, in0=pre, scalar1=1.0 / TWO_PI, scalar2=MAGIC, op0=ALU.mult, op1=ALU.add),
          reads=[R_pre], writes=[R_tmp])
    em.op('dve', lambda e: e.tensor_scalar(out=tmp, in0=tmp, scalar1=-MAGIC, scalar2=-TWO_PI, op0=ALU.add, op1=ALU.mult),
          reads=[R_tmp], writes=[R_tmp])
    em.op('dve', lambda e: e.tensor_tensor(out=pre, in0=pre, in1=tmp, op=ALU.add), reads=[R_pre, R_tmp], writes=[R_pre])
    em.op('dve', lambda e: e.tensor_scalar(out=pre, in0=pre, scalar1=-3.1415925, scalar2=3.1415925, op0=ALU.max, op1=ALU.min),
          reads=[R_pre], writes=[R_pre])
    em.op('act', lambda e: e.activation(out=pre, in_=pre, func=AF.Sin), reads=[R_pre], writes=[R_pre])


def phase_filter(nc, em, L, L2):
    carve = L['carve']; PSB = L['PSB']; PSR = L['PSR']; pc = L['pc']; dc = L['dc']
    R_pc = L['R_pc']; R_dc = L['R_dc']; R_tabs = L['R_tabs']; R_cf = L['R_cf']
    PC = L['PC']; TB = L['TB']; identf = L['identf']; dbg = L['dbg']
    kfs = L['kfs']; fw_d = L['fw_d']; fwout_d = L['fwout_d']; zT_d = L['zT_d']; trow_d = L['trow_d']
    KB = 256
    zT = carve(0, [T]); R_zT = Reg('zT')
    trow = carve(16 * KB, [T]); R_trow = Reg('trow')
    hA = carve(32 * KB, [T]); R_hA = Reg('hA')
    hB = carve(48 * KB, [T]); R_hB = Reg('hB')
    fwout = carve(64 * KB, [4096]); R_fwout = Reg('fwout')
    fw = carve(80 * KB, [3, 64]); R_fw = Reg('fw')
    kk = carve(82 * KB, [8192]); R_kk = Reg('kk')
    dec = [carve((114 + 2 * i) * KB, [512]) for i in range(2)]; R_dec = [Reg(f'dec{i}') for i in range(2)]
    Krm = carve(118 * KB, [128, 128], BF16); R_Krm = Reg('Krm')
    Zk = carve(150 * KB, [32, 256], BF16); R_Zk = Reg('Zk'); R_Zkc = [Reg(f'Zk{i}') for i in range(16)]
    Kfo = [carve((0 + 8 * i) * KB, [16, 256], BF16) for i in range(2)]; R_Kfo = [Reg(f'Kfo{i}') for i in range(2)]
    nrm = dc[:, 240:241]
    em.dma('sp', lambda e: e.dma_start(out=zT, in_=zT_d), writes=[R_zT], stream='fl', depth=4)
    em.dma('sp', lambda e: e.dma_start(out=trow, in_=trow_d), writes=[R_trow], stream='fl', depth=4)
    em.dma('sp', lambda e: e.dma_start(out=fwout, in_=fwout_d), writes=[R_fwout], stream='fl', depth=4)
    em.dma('sp', lambda e: e.dma_start(out=fw, in_=fw_d), writes=[R_fw], stream='fl', depth=4)
    srcs = [(zT, R_zT, 33), (hA, R_hA, 64), (hB, R_hB, 64)]
    dsts = [(hA, R_hA), (hB, R_hB), (hA, R_hA)]
    for layer in range(3):
        src, R_src, K = srcs[layer]; dst, R_d = dsts[layer]
        for tl in range(8):
            ps = PSB[tl % 2]; psr = PSR[tl % 2]
            em.op('pe', lambda e, src=src, K=K, tl=tl, ps=ps, layer=layer: e.matmul(
                ps[0:64, :], lhsT=fw[0:K, layer, :], rhs=src[0:K, tl * 512:(tl + 1) * 512], start=True, stop=True),
                reads=[R_fw, R_src], writes=[psr])
            em.op('act', lambda e, dst=dst, tl=tl, ps=ps, layer=layer: e.activation(
                out=dst[0:64, tl * 512:(tl + 1) * 512], in_=ps[0:64, :], func=AF.Identity,
                scale=PC('hy_f_freq', 0)[0:64], bias=dc[0:64, 224 + layer:225 + layer]),
                reads=[psr, R_pc, R_dc], writes=[R_d])
        tmp = carve(118 * KB, [T]); R_tmp = R_Krm
        sin_act(em, dst[0:64, :], R_d, tmp[0:64, :], R_tmp, T)
    h3 = hA; R_h3 = R_hA
    em.barrier()
    for cc in range(16):
        for tl in range(8):
            db = tl % 2
            em.op('act', lambda e, db=db, tl=tl, cc=cc: e.activation(out=dec[db], in_=trow[:, tl * 512:(tl + 1) * 512], func=AF.Exp,
                                                                    scale=dc[:, 208 + cc:209 + cc]),
                  reads=[R_trow, R_dc], writes=[R_dec[db]])
            for dr in range(2):
                pi = (tl * 2 + dr) % 4; ps = PSB[pi]; psr = PSR[pi]
                c0 = dr * 2048 + cc * 128
                em.op('pe', lambda e, c0=c0, tl=tl, ps=ps: e.matmul(ps[:, :], lhsT=fwout[0:64, c0:c0 + 128],
                                                                    rhs=h3[0:64, tl * 512:(tl + 1) * 512], start=True, stop=True),
                      reads=[R_fwout, R_h3], writes=[psr])
                if dr == 0:
                    em.op('dve', lambda e, tl=tl, ps=ps, db=db: e.tensor_tensor(out=kk[:, 4096 + tl * 512: 4096 + (tl + 1) * 512],
                                                                               in0=ps[:, :], in1=dec[db], op=ALU.mult),
                          reads=[psr, R_dec[db]], writes=[R_kk])
                else:
                    if tl == 0:
                        em.op('dve', lambda e, ps=ps, db=db: e.tensor_tensor(out=kk[:, 0:1], in0=ps[:, 0:1], in1=dec[db][:, 0:1], op=ALU.mult),
                              reads=[psr, R_dec[db]], writes=[R_kk])
                        em.op('dve', lambda e, ps=ps, db=db: e.tensor_tensor(out=kk[:, 3585:4096][:, ::-1], in0=ps[:, 1:512],
                                                                           in1=dec[db][:, 1:512], op=ALU.mult),
                              reads=[psr, R_dec[db]], writes=[R_kk])
                    else:
                        lo = 4096 - tl * 512 - 511
                        em.op('dve', lambda e, ps=ps, db=db, lo=lo: e.tensor_tensor(out=kk[:, lo:lo + 512][:, ::-1], in0=ps[:, :],
                                                                                  in1=dec[db], op=ALU.mult),
                              reads=[psr, R_dec[db]], writes=[R_kk])
        em.op('dve', lambda e: e.tensor_reduce(out=nrm, in_=kk, axis=AX.X, op=ALU.add, apply_absolute_value=True),
              reads=[R_kk], writes=[R_dc])
        em.op('dve', lambda e, cc=cc: e.reciprocal(out=dc[:, 192 + cc:193 + cc], in_=nrm), reads=[R_dc], writes=[R_dc])
        if STAGE < 99 and cc == 0:
            em.dma('sp', lambda e: e.dma_start(out=dbg['d_kk'], in_=kk), reads=[R_kk], stream='dbg', depth=2)
        for e4 in range(32):
            ps = PSB[4 + e4 % 2]; psr = PSR[4 + e4 % 2]
            ne = 4 if e4 < 31 else 3
            for j in range(ne):
                ee = e4 * 4 + j
                em.op('pe', lambda e, ee=ee, j=j, ps=ps: e.transpose(out=ps[0:127, j * 128:(j + 1) * 128],
                                                                     in_=kk[:, 1 + ee: 1 + ee + 64 * 126 + 1: 64], identity=identf),
                      reads=[R_kk, R_cf], writes=[psr])
            copy_op(em, 'act' if e4 % 2 == 0 else 'dve', Krm[0:127, :, e4 * 4:e4 * 4 + ne],
                    ps[0:127, 0:ne * 128].rearrange("p (j c) -> p c j", j=ne), [psr], [R_Krm])
        for cb in range(4):
            for c2 in range(16):
                ps = PSB[c2 % 2]; psr = PSR[c2 % 2]
                for j in range(2):
                    c = cb * 32 + c2 * 2 + j
                    em.op('pe', lambda e, c=c, j=j, ps=ps: e.matmul(ps[0:127, j * 256:(j + 1) * 256], lhsT=Krm[0:127, c, 0:127],
                                                                    rhs=TB('T1k', 127, 0, 256), start=True, stop=True),
                          reads=[R_Krm, R_tabs], writes=[psr])
                em.op('act', lambda e, c2=c2, ps=ps: e.activation(out=Zk[0:127, c2 * 2:c2 * 2 + 2, :],
                                                                  in_=ps[0:127, :].rearrange("p (j f) -> p j f", j=2), func=AF.Copy),
                      reads=[psr], writes=[R_Zkc[c2]])
            for c2 in range(16):
                ps = PSB[2 + c2 % 2]; psr = PSR[2 + c2 % 2]
                kb = c2 // 8
                for j in range(2):
                    cs = c2 * 2 + j
                    em.op('pe', lambda e, cs=cs, j=j, ps=ps: e.matmul(ps[:, j * 256:(j + 1) * 256], lhsT=Zk[0:127, cs, 0:128],
                                                                      rhs=TB('T2ak', 127, 0, 256), start=True, stop=False),
                          reads=[R_Zkc[c2], R_tabs], writes=[psr])
                    em.op('pe', lambda e, cs=cs, j=j, ps=ps: e.matmul(ps[:, j * 256:(j + 1) * 256], lhsT=Zk[0:127, cs, 128:256],
                                                                      rhs=TB('T2bk', 127, 0, 256), start=False, stop=True),
                          reads=[R_Zkc[c2], R_tabs], writes=[psr])
                em.op('dve', lambda e, c2=c2, kb=kb, ps=ps: e.tensor_copy(out=Kfo[kb][:, (c2 % 8) * 2:(c2 % 8) * 2 + 2, :],
                                                                          in_=ps[:, :].rearrange("p (j f) -> p j f", j=2)),
                      reads=[psr], writes=[R_Kfo[kb]])
                if c2 % 8 == 7:
                    c0 = cb * 32 + kb * 16
                    em.dma('sp', lambda e, kb=kb, cc=cc, c0=c0: e.dma_start(
                        out=kfs[cc][:, c0 * 256:(c0 + 16) * 256].rearrange("p (a b) -> p a b", a=16), in_=Kfo[kb]),
                        reads=[R_Kfo[kb]], stream='kfst', depth=2)
    em.barrier()


def phase_groups(nc, em, L, L2):
    carve = L['carve']; PSB = L['PSB']; PSR = L['PSR']; pc = L['pc']; dc = L['dc']
    R_pc = L['R_pc']; R_dc = L['R_dc']; R_modc = L['R_modc']; R_tabs = L['R_tabs']; R_cf = L['R_cf']
    PC = L['PC']; TB = L['TB']; identf = L['identf']; dbg = L['dbg']; poff = L['poff']
    hxs = L['hxs']; kfs = L['kfs']; ysc = L['ysc']; win = L['win']; lruw_d = L['lruw_d']
    hc = L2['hc']; R_hc = L2['R_hc']
    KB = 256
    NG = int(os.environ.get('KGROUPS', '16')); KSUB = int(os.environ.get('KSUB', '9'))
    U = carve(0, [T]); X0C = carve(16 * KB, [T]); XC = carve(32 * KB, [T]); GG = carve(48 * KB, [T])
    R_U = Reg('U'); R_X0C = Reg('X0C'); R_XC = Reg('XC'); R_GG = Reg('GG')
    wstg = [carve((64 + 16 * i) * KB, [32, 128]) for i in range(1)]; R_wstg = [Reg('wstg0')]
    wbf = carve(80 * KB, [5, 32, 128], BF16)
    wbf = wbf
    R_wbf = [Reg(f'wbf{i}') for i in range(5)]
    hxt = [carve((120 + 16 * i) * KB, [32, 256], BF16) for i in range(2)]; R_hxt = [Reg(f'hxt{i}') for i in range(2)]
    hxf = [carve((120 + 16 * i) * KB, [32, 128]) for i in range(2)]
    ptmp = [carve((152 + 5 * i) * KB, [5, 256]) for i in range(2)]; R_ptmp = [[Reg(f'pt{i}_{j}') for j in range(5)] for i in range(2)]
    lwst = carve(162 * KB, [4, 128]); R_lwst = Reg('lwst')
    lwb = carve(164 * KB, [4, 128], BF16); R_lwb = Reg('lwb')
    cst = 165 * KB
    ctmp = [carve(cst + i * 256, [256]) for i in range(10)]; R_ctmp = [Reg(f'ct{i}') for i in range(10)]
    cxb = carve(cst + 10 * 256, [256], BF16); R_cxb = Reg('cxb')
    ct2 = [carve(176 * KB + i * 256, [256]) for i in range(4)]; R_ct2 = [Reg(f'ct2_{i}') for i in range(4)]
    xcb = carve(64 * KB, [T], BF16); R_xcb = Reg('xcb')
    LA = carve(72 * KB, [T]); LB = carve(88 * KB, [T]); LT = carve(104 * KB, [T]); HF = carve(120 * KB, [T]); HB = carve(136 * KB, [T])
    R_LA = Reg('LA'); R_LB = Reg('LB'); R_LT = Reg('LT'); R_HF = Reg('HF'); R_HB = Reg('HB')
    ylru = carve(152 * KB, [T], BF16); R_ylru = Reg('ylru')
    Urm = carve(64 * KB, [128, 64], BF16, parts=64); R_Urm = Reg('Urm')
    Zs = [carve((80 + 8 * i) * KB, [16, 256], BF16, parts=64) for i in range(2)]; R_Zs = [Reg(f'Zs{i}') for i in range(2)]; R_Zsc = [[Reg(f'Zs{i}_{j}') for j in range(8)] for i in range(2)]
    Kfs = [carve((96 + 8 * i) * KB, [16, 256], BF16) for i in range(2)]; R_Kfs = [Reg(f'Kfs{i}') for i in range(2)]
    Yab = [carve(112 * KB + i * 256, [2, 2, 128], BF16) for i in range(4)]; R_Yab = [Reg(f'Yab{i}') for i in range(4)]
    prod = [carve(116 * KB + i * 1024, [2, 512]) for i in range(2)]; R_prod = [Reg(f'prod{i}') for i in range(2)]
    GH = carve(124 * KB, [2 * 64, 128], BF16); R_GH = Reg('GH')
    GH4 = GH.rearrange("p (g r) c -> p g r c", g=2)
    yhy = carve(156 * KB, [T], BF16); R_yhy = Reg('yhy')
    t1b = [carve((80 + 8 * i) * KB, [512]) for i in range(2)]; R_t1b = R_Zs
    h0 = lambda d: dc[:, 230 + d:231 + d]

    def lru_coeffs(n, xc_ap, xcb_ap, g, d, A, R_A, Bq, R_B, TMP, R_TMP, R_xc, R_xcbr, tile):
        nt = n // tile
        for which, dstb, R_dst in ((0, A, R_A), (1, TMP, R_TMP)):
            for tl in range(nt):
                ps = PSB[tl % 2]; psr = PSR[tl % 2]
                sl = slice(tl * tile, (tl + 1) * tile)
                em.op('pe', lambda e, ps=ps, sl=sl, which=which: e.matmul(ps[:, 0:tile], lhsT=lwb[:, which * 2 + d, :], rhs=xcb_ap[:, sl],
                                                                        start=True, stop=True), reads=[R_lwb, R_xcbr], writes=[psr])
                bcol = PC('lru_ba' if which == 0 else 'lru_bx', d * 16 + g)
                em.op('act', lambda e, ps=ps, sl=sl, dstb=dstb, bcol=bcol: e.activation(out=dstb[:, sl], in_=ps[:, 0:tile], func=AF.Sigmoid, bias=bcol),
                      reads=[psr, R_pc], writes=[R_dst])
        em.op('dve', lambda e: e.tensor_tensor(out=Bq, in0=TMP, in1=xc_ap, op=ALU.mult), reads=[R_TMP, R_xc], writes=[R_B])
        em.op('act', lambda e: e.activation(out=TMP, in_=A, func=AF.Exp, scale=dc[:, 160 + d * 16 + g:161 + d * 16 + g]),
              reads=[R_A, R_dc], writes=[R_TMP])
        em.op('act', lambda e: e.activation(out=TMP, in_=TMP, func=AF.Sqrt, scale=-1.0, bias=1.0), reads=[R_TMP], writes=[R_TMP])
        em.op('dve', lambda e: e.tensor_tensor(out=Bq, in0=Bq, in1=TMP, op=ALU.mult), reads=[R_TMP, R_B], writes=[R_B])
        em.op('act', lambda e: e.activation(out=A, in_=A, func=AF.Exp, scale=dc[:, 128 + d * 16 + g:129 + d * 16 + g]),
              reads=[R_A, R_dc], writes=[R_A])

    for g in range(NG):
        jl = [g, 16 + g, 32 + g, 48 + g, 64 + g]
        stg = [(wstg[0], R_wstg[0]), (hxf[0], R_hxt[0]), (hxf[1], R_hxt[1])]
        for i, jc in enumerate(jl):
            sbuf_, R_sb = stg[i % 3]
            em.dma('sp', lambda e, jc=jc, sbuf_=sbuf_: e.dma_start(out=sbuf_, in_=win[jc]), writes=[R_sb], stream='wi', depth=3)
            copy_op(em, 'act' if i % 2 == 0 else 'dve', wbf[:, i], sbuf_, [R_sb], [R_wbf[i]])
        em.dma('sp', lambda e, g=g: e.dma_start(out=lwst, in_=lruw_d[g]), writes=[R_lwst], stream='wi', depth=2)
        copy_op(em, 'dve', lwb, lwst, [R_lwst], [R_lwb])
        ps = PSB[7]; psr = PSR[7]
        for kc in range(KC):
            em.op('pe', lambda e, kc=kc, ps=ps: e.matmul(ps[:, 0:256], lhsT=wbf[:, 4, kc, :], rhs=hc[:, kc, :], start=(kc == 0), stop=(kc == KC - 1)),
                  reads=[R_wbf[4], R_hc], writes=[psr])
        pcl, xcc, cA, cB, cT, cH = ctmp[0], ctmp[1], ctmp[2], ctmp[3], ctmp[4], ctmp[5]
        em.op('act', lambda e, ps=ps, g=g: e.activation(out=pcl, in_=ps[:, 0:256], func=AF.Identity, bias=PC('b_in', 64 + g)),
              reads=[psr, R_pc], writes=[R_ctmp[0]])
        LW = lambda k, g=g: PC('lru_conv_w', g * 4 + k)
        em.op('dve', lambda e, LW=LW, g=g: e.tensor_scalar(out=xcc, in0=pcl, scalar1=LW(2), scalar2=PC('lru_conv_b', g), op0=ALU.mult, op1=ALU.add),
              reads=[R_ctmp[0], R_pc], writes=[R_ctmp[1]])
        for k, (so, si) in ((0, (slice(2, 256), slice(0, 254))), (1, (slice(1, 256), slice(0, 255))), (3, (slice(0, 255), slice(1, 256)))):
            em.op('dve', lambda e, LW=LW, k=k, so=so, si=si: e.scalar_tensor_tensor(out=xcc[:, so], in0=pcl[:, si], scalar=LW(k), in1=xcc[:, so],
                                                                           op0=ALU.mult, op1=ALU.add),
                  reads=[R_ctmp[0], R_ctmp[1], R_pc], writes=[R_ctmp[1]])
        copy_op(em, 'act', cxb, xcc, [R_ctmp[1]], [R_cxb])
        for d in range(2):
            lru_coeffs(256, xcc, cxb, g, d, cA, R_ctmp[2], cB, R_ctmp[3], cT, R_ctmp[4], R_ctmp[1], R_cxb, 256)
            if d == 0:
                em.op('dve', lambda e: e.tensor_tensor_scan(out=cH, data0=cA, data1=cB, initial=0.0, op0=ALU.mult, op1=ALU.add),
                      reads=[R_ctmp[2], R_ctmp[3]], writes=[R_ctmp[5]])
                em.op('dve', lambda e: e.tensor_copy(out=h0(0), in_=cH[:, 255:256]), reads=[R_ctmp[5]], writes=[R_dc])
            else:
                em.op('dve', lambda e: e.tensor_tensor_scan(out=cH[:, ::-1], data0=cA[:, ::-1], data1=cB[:, ::-1], initial=0.0,
                                                            op0=ALU.mult, op1=ALU.add), reads=[R_ctmp[2], R_ctmp[3]], writes=[R_ctmp[5]])
                em.op('dve', lambda e: e.tensor_copy(out=h0(1), in_=cH[:, 0:1]), reads=[R_ctmp[5]], writes=[R_dc])
        HW = lambda ch, k: PC('hy_conv_w', ch * 3 + k)
        for tt in range(16):
            hb = tt % 2; pb = tt % 2
            em.dma('sp', lambda e, hb=hb, tt=tt: e.dma_start(out=hxt[hb], in_=hxs[tt].rearrange("p (a b) -> p a b", a=32)),
                   writes=[R_hxt[hb]], stream='hxl', depth=2)
            tsl = slice(tt * 256, (tt + 1) * 256)
            for i in range(5):
                pi = (tt * 5 + i) % 4; ps = PSB[pi]; psr = PSR[pi]
                for kc in range(KC):
                    em.op('pe', lambda e, i=i, kc=kc, hb=hb, ps=ps: e.matmul(ps[:, 0:256], lhsT=wbf[:, i, kc, :], rhs=hxt[hb][:, kc, :],
                                                                           start=(kc == 0), stop=(kc == KC - 1)),
                          reads=[R_wbf[i], R_hxt[hb]], writes=[psr])
                em.op('act', lambda e, i=i, pb=pb, ps=ps, jc=jl[i]: e.activation(out=ptmp[pb][:, i, :], in_=ps[:, 0:256], func=AF.Identity,
                                                                                 bias=PC('b_in', jc)),
                      reads=[psr, R_pc], writes=[R_ptmp[pb][i]])
            v3 = lambda ap: ap.rearrange("p (r q) -> p r q", q=64)
            x1c, vcv = ct2[0], ct2[1]
            for i, (dst, R_dst) in enumerate(((X0C[:, tsl], R_X0C), (x1c, R_ct2[0]), (vcv, R_ct2[1]))):
                ch = i * 16 + g
                src = ptmp[pb][:, i, :]
                em.op('dve', lambda e, dst=dst, src=src, ch=ch: e.tensor_scalar(out=dst, in0=src, scalar1=HW(ch, 1), scalar2=PC('hy_conv_b', ch),
                                                                                 op0=ALU.mult, op1=ALU.add),
                      reads=[R_ptmp[pb][i], R_pc], writes=[R_dst])
                em.op('dve', lambda e, dst=dst, src=src, ch=ch: e.scalar_tensor_tensor(out=v3(dst)[:, :, 1:64], in0=v3(src)[:, :, 0:63], scalar=HW(ch, 0),
                                                                                        in1=v3(dst)[:, :, 1:64], op0=ALU.mult, op1=ALU.add),
                      reads=[R_ptmp[pb][i], R_pc, R_dst], writes=[R_dst])
                em.op('dve', lambda e, dst=dst, src=src, ch=ch: e.scalar_tensor_tensor(out=v3(dst)[:, :, 0:63], in0=v3(src)[:, :, 1:64], scalar=HW(ch, 2),
                                                                                        in1=v3(dst)[:, :, 0:63], op0=ALU.mult, op1=ALU.add),
                      reads=[R_ptmp[pb][i], R_pc, R_dst], writes=[R_dst])
            em.op('pool', lambda e, tsl=tsl: e.tensor_tensor(out=U[:, tsl], in0=x1c, in1=vcv, op=ALU.mult), reads=[R_ct2[0], R_ct2[1]], writes=[R_U])
            src = ptmp[pb][:, 4, :]; dst = XC[:, tsl]
            em.op('dve', lambda e, LW=LW, dst=dst, src=src, g=g: e.tensor_scalar(out=dst, in0=src, scalar1=LW(2), scalar2=PC('lru_conv_b', g),
                                                                           op0=ALU.mult, op1=ALU.add), reads=[R_ptmp[pb][4], R_pc], writes=[R_XC])
            for k, (so, si) in ((0, (slice(2, 64), slice(0, 62))), (1, (slice(1, 64), slice(0, 63))), (3, (slice(0, 63), slice(1, 64)))):
                em.op('dve', lambda e, LW=LW, dst=dst, src=src, k=k, so=so, si=si: e.scalar_tensor_tensor(
                    out=v3(dst)[:, :, so], in0=v3(src)[:, :, si], scalar=LW(k), in1=v3(dst)[:, :, so], op0=ALU.mult, op1=ALU.add),
                    reads=[R_ptmp[pb][4], R_pc, R_XC], writes=[R_XC])
            pg = ptmp[pb][:, 3, :]; gt = ct2[2]
            em.op('pool', lambda e, pg=pg: e.tensor_tensor(out=gt, in0=pg, in1=pg, op=ALU.mult), reads=[R_ptmp[pb][3]], writes=[R_ct2[2]])
            em.op('pool', lambda e: e.tensor_scalar(out=gt, in0=gt, scalar1=0.044715, scalar2=1.0, op0=ALU.mult, op1=ALU.add),
                  reads=[R_ct2[2]], writes=[R_ct2[2]])
            em.op('pool', lambda e, pg=pg: e.tensor_tensor(out=gt, in0=gt, in1=pg, op=ALU.mult), reads=[R_ct2[2], R_ptmp[pb][3]], writes=[R_ct2[2]])
            em.op('act', lambda e: e.activation(out=gt, in_=gt, func=AF.Sigmoid, scale=1.5957691216057308), reads=[R_ct2[2]], writes=[R_ct2[2]])
            em.op('pool', lambda e, pg=pg, tsl=tsl: e.tensor_tensor(out=GG[:, tsl], in0=gt, in1=pg, op=ALU.mult),
                  reads=[R_ct2[2], R_ptmp[pb][3]], writes=[R_GG])
        if STAGE < 5 and g == 0:
            for nm, ap, R_ in (('d_a', U, R_U), ('d_b', X0C, R_X0C), ('d_c', XC, R_XC), ('d_d', GG, R_GG)):
                em.dma('sp', lambda e, nm=nm, ap=ap: e.dma_start(out=dbg[nm], in_=ap), reads=[R_], stream='dbg', depth=2)
        if STAGE < 5:
            continue
        mix_regs = [R_xcb, R_LA, R_LB, R_LT, R_HF, R_HB, R_ylru, R_Urm, R_GH, R_yhy] + R_Zs + R_Kfs + R_Yab + R_prod + R_t1b
        inp_regs = R_wstg + R_wbf + R_hxt + [r for rr in R_ptmp for r in rr] + [R_lwst]
        em.barrier()
        copy_op(em, 'act', xcb, XC, [R_XC], [R_xcb])
        for d in range(2):
            H = HF if d == 0 else HB; R_H = R_HF if d == 0 else R_HB
            lru_coeffs(T, XC, xcb, g, d, LA, R_LA, LB, R_LB, LT, R_LT, R_XC, R_xcb, 512)
            if d == 0:
                em.op('dve', lambda e, H=H: e.tensor_tensor_scan(out=H, data0=LA, data1=LB, initial=h0(0), op0=ALU.mult, op1=ALU.add),
                      reads=[R_LA, R_LB, R_dc], writes=[R_H])
            else:
                em.op('dve', lambda e, H=H: e.tensor_tensor_scan(out=H[:, ::-1], data0=LA[:, ::-1], data1=LB[:, ::-1], initial=h0(1),
                                                                 op0=ALU.mult, op1=ALU.add), reads=[R_LA, R_LB, R_dc], writes=[R_H])
        if STAGE == 5 and g == 0:
            for nm, ap, R_ in (('d_a', HF, R_HF), ('d_b', HB, R_HB), ('d_c', LA, R_LA), ('d_d', LB, R_LB)):
                em.dma('sp', lambda e, nm=nm, ap=ap: e.dma_start(out=dbg[nm], in_=ap), reads=[R_], stream='dbg', depth=2)
            em.dma('sp', lambda e: e.dma_start(out=dbg['d_dc'], in_=dc[:, :]), reads=[R_dc], stream='dbg', depth=2)
        em.op('pool', lambda e: e.tensor_tensor(out=HF, in0=HF, in1=HB, op=ALU.add), reads=[R_HF, R_HB], writes=[R_HF])
        em.op('pool', lambda e: e.tensor_tensor(out=ylru, in0=HF, in1=GG, op=ALU.mult), reads=[R_HF, R_GG], writes=[R_ylru])
        em.dma('sp', lambda e, g=g: e.dma_start(out=ysc[16 + g], in_=ylru), reads=[R_ylru], stream='yst', depth=2)
        em.barrier()
        if STAGE < 6:
            continue
        for q4 in range(16):
            ps = PSB[q4 % 2]; psr = PSR[q4 % 2]
            for j in range(4):
                q = q4 * 4 + j
                em.op('pe', lambda e, q=q, j=j, ps=ps: e.transpose(out=ps[0:64, j * 128:(j + 1) * 128], in_=U[:, q:T:64], identity=identf),
                      reads=[R_U, R_cf], writes=[psr])
            copy_op(em, 'act' if q4 % 2 == 0 else 'dve', Urm[0:64, :, q4 * 4:q4 * 4 + 4],
                    ps[0:64, :].rearrange("p (j c) -> p c j", j=4), [psr], [R_Urm])
        pending = None

        def emit_i2(cb, c2, yb):
            ps2 = PSB[4 + (c2 // 2) % 2]; psr2 = PSR[4 + (c2 // 2) % 2]
            for j in range(2):
                col = ((c2 % 2) * 2 + j) * 128
                em.op('pe', lambda e, j=j, yb=yb, ps2=ps2, col=col: e.matmul(ps2[:, col:col + 128], lhsT=Yab[yb][:, j, 0, :], rhs=TB('I2a', 128, 0, 128),
                                                                             start=True, stop=False), reads=[R_Yab[yb], R_tabs], writes=[psr2])
                em.op('pe', lambda e, j=j, yb=yb, ps2=ps2, col=col: e.matmul(ps2[:, col:col + 128], lhsT=Yab[yb][:, j, 1, :], rhs=TB('I2b', 128, 0, 128),
                                                                             start=False, stop=True), reads=[R_Yab[yb], R_tabs], writes=[psr2])
            if c2 % 2 == 1:
                c0 = cb * 16 + (c2 - 1) * 2
                copy_op(em, 'act', GH4[:, :, :, c0:c0 + 4].rearrange("p g r c -> p c g r"),
                        ps2[:, :].rearrange("p (c g r) -> p c g r", c=4, g=2), [psr2], [R_GH])

        for cb in range(8):
            zb = cb % 2
            em.dma('sp', lambda e, zb=zb, cb=cb, g=g: e.dma_start(out=Kfs[zb], in_=kfs[g][:, cb * 4096:(cb + 1) * 4096].rearrange("p (a b) -> p a b", a=16)),
                   writes=[R_Kfs[zb]], stream='kfl', depth=2)
            for c2 in range(8):
                ps = PSB[2 + c2 % 2]; psr = PSR[2 + c2 % 2]
                for j in range(2):
                    c = cb * 16 + c2 * 2 + j
                    em.op('pe', lambda e, c=c, j=j, ps=ps: e.matmul(ps[0:64, j * 256:(j + 1) * 256], lhsT=Urm[0:64, c, :], rhs=TB('T1d', 64, 0, 256),
                                                                    start=True, stop=True), reads=[R_Urm, R_tabs], writes=[psr])
                copy_op(em, 'act', Zs[zb][0:64, c2 * 2:c2 * 2 + 2, :], ps[0:64, :].rearrange("p (j f) -> p j f", j=2), [psr], [R_Zsc[zb][c2]])
            for c2 in range(8):
                ps = PSB[c2 % 2]; psr = PSR[c2 % 2]
                yb = c2 % 4; pb2 = c2 % 2
                for j in range(2):
                    cs = c2 * 2 + j
                    em.op('pe', lambda e, cs=cs, j=j, ps=ps, zb=zb: e.matmul(ps[:, j * 256:(j + 1) * 256], lhsT=Zs[zb][0:64, cs, 0:128],
                                                                             rhs=TB('T2ad', 64, 0, 256), start=True, stop=False),
                          reads=[R_Zsc[zb][c2], R_tabs], writes=[psr])
                    em.op('pe', lambda e, cs=cs, j=j, ps=ps, zb=zb: e.matmul(ps[:, j * 256:(j + 1) * 256], lhsT=Zs[zb][0:64, cs, 128:256],
                                                                             rhs=TB('T2bd', 64, 0, 256), start=False, stop=True),
                          reads=[R_Zsc[zb][c2], R_tabs], writes=[psr])
                psv = ps[:, :].rearrange("p (c h f) -> p c h f", c=2, h=2)
                kv = Kfs[zb][:, c2 * 2:c2 * 2 + 2, :].rearrange("p c (h f) -> p c h f", h=2)
                pr0 = prod[pb2][:, 0, :].rearrange("p (c h f) -> p c h f", c=2, h=2)
                pr1 = prod[pb2][:, 1, :].rearrange("p (c h f) -> p c h f", c=2, h=2)
                em.op('dve', lambda e, psv=psv, kv=kv, pr0=pr0: e.tensor_tensor(out=pr0, in0=psv, in1=kv, op=ALU.mult),
                      reads=[psr, R_Kfs[zb]], writes=[R_prod[pb2]])
                em.op('dve', lambda e, psv=psv, kv=kv, pr1=pr1: e.tensor_tensor(out=pr1, in0=psv, in1=kv[:, :, ::-1, :], op=ALU.mult),
                      reads=[psr, R_Kfs[zb]], writes=[R_prod[pb2]])
                em.op('pool', lambda e, pr0=pr0, yb=yb: e.tensor_tensor(out=Yab[yb][:, :, 0, :], in0=pr0[:, :, 0, :], in1=pr0[:, :, 1, :], op=ALU.subtract),
                      reads=[R_prod[pb2]], writes=[R_Yab[yb]])
                em.op('pool', lambda e, pr1=pr1, yb=yb: e.tensor_tensor(out=Yab[yb][:, :, 1, :], in0=pr1[:, :, 0, :], in1=pr1[:, :, 1, :], op=ALU.add),
                      reads=[R_prod[pb2]], writes=[R_Yab[yb]])
                if pending is not None:
                    emit_i2(*pending)
                pending = (cb, c2, yb)
        emit_i2(*pending)
        if KSUB < 6:
            em.barrier(); continue
        for half in range(2):
            for r in range(32):
                rr = half * 32 + r
                ps = PSB[4 + r // 8]; psr = PSR[4 + r // 8]
                col = (r % 8) * 64
                em.op('pe', lambda e, rr=rr, ps=ps, col=col: e.matmul(ps[:, col:col + 64], lhsT=GH4[:, 0, rr, :], rhs=TB('I1a', 128, 0, 64),
                                                                      start=True, stop=False), reads=[R_GH, R_tabs], writes=[psr])
                em.op('pe', lambda e, rr=rr, ps=ps, col=col: e.matmul(ps[:, col:col + 64], lhsT=GH4[:, 1, rr, :], rhs=TB('I1b', 128, 0, 64),
                                                                      start=False, stop=True), reads=[R_GH, R_tabs], writes=[psr])
            for bk in range(4):
                ps = PSB[4 + bk]; psr = PSR[4 + bk]
                seg = slice(half * 2048 + bk * 512, half * 2048 + (bk + 1) * 512)
                tb = bk % 2
                em.op('act', lambda e, seg=seg, tb=tb, g=g: e.activation(out=t1b[tb], in_=U[:, seg], func=AF.Identity, scale=PC('hy_bias', g)),
                      reads=[R_U, R_pc], writes=[R_t1b[tb]])
                em.op('dve', lambda e, ps=ps, tb=tb, g=g: e.scalar_tensor_tensor(out=t1b[tb], in0=ps[:, :], scalar=dc[:, 192 + g:193 + g], in1=t1b[tb],
                                                                                 op0=ALU.mult, op1=ALU.add), reads=[psr, R_dc, R_t1b[tb]], writes=[R_t1b[tb]])
                em.op('pool', lambda e, seg=seg, tb=tb: e.tensor_tensor(out=yhy[:, seg], in0=t1b[tb], in1=X0C[:, seg], op=ALU.mult),
                      reads=[R_t1b[tb], R_X0C], writes=[R_yhy])
        em.dma('sp', lambda e, g=g: e.dma_start(out=ysc[g], in_=yhy), reads=[R_yhy], stream='yst', depth=2)
        em.barrier()


def build_row(em, L, dst, R_dst, colfn, ncols, tmpd, R_tmpd, reads):
    PSB = L['PSB']; PSR = L['PSR']; identf = L['identf']; onesf = L['onesf']; R_cf = L['R_cf']
    for k4 in range(ncols // 4):
        ps = PSB[6 + k4 % 2]; psr = PSR[6 + k4 % 2]
        for j in range(4):
            kc = k4 * 4 + j; tb = kc % 2
            em.op('dve', lambda e, kc=kc, tb=tb: e.tensor_scalar(out=tmpd[tb], in0=identf, scalar1=colfn(kc), scalar2=None, op0=ALU.mult),
                  reads=[R_cf] + reads, writes=[R_tmpd[tb]])
            em.op('pe', lambda e, j=j, tb=tb, ps=ps: e.matmul(ps[:, j * 128:(j + 1) * 128], lhsT=onesf, rhs=tmpd[tb], start=True, stop=True),
                  reads=[R_cf, R_tmpd[tb]], writes=[psr])
        copy_op(em, 'act', dst[:, k4 * 512:(k4 + 1) * 512], ps[:, :], [psr], [R_dst])


def phase_out(nc, em, L, L2):
    carve = L['carve']; PSB = L['PSB']; PSR = L['PSR']; dc = L['dc']
    R_dc = L['R_dc']; R_modc = L['R_modc']; MODX = L['MODX']
    ysc = L['ysc']; wout = L['wout']; acc = L['acc']; xtok = L['xtok']; rows3_d = L['rows3_d']
    KB = 256
    wstg = [carve(16 * i * KB, [8, 512]) for i in range(2)]; R_wstg = [Reg(f'cw{i}') for i in range(2)]
    wbf = carve(32 * KB, [32, 512], BF16); R_wbf = Reg('cwbf')
    yt = [carve((64 + 32 * i) * KB, [32, 512], BF16) for i in range(2)]; R_yt = [Reg(f'yt{i}') for i in range(2)]
    gt1r = carve(128 * KB, [D]); R_gt1r = Reg('gt1r')
    gbr = carve(144 * KB, [D]); R_gbr = Reg('gbr')
    xt = [carve((160 + 2 * i) * KB, [512]) for i in range(3)]; R_xt = [Reg(f'xt{i}') for i in range(3)]
    ot = [carve((166 + 2 * i) * KB, [512]) for i in range(3)]; R_ot = [Reg(f'ot{i}') for i in range(3)]
    tmpd = [carve(172 * KB + i * 128, [128]) for i in range(2)]; R_tmpd = [Reg(f'td{i}') for i in range(2)]
    brow = carve(176 * KB, [D]); R_brow = Reg('brow')
    em.dma('sp', lambda e: e.dma_start(out=brow, in_=rows3_d[2]), writes=[R_brow], stream='cl', depth=2)
    build_row(em, L, gt1r, R_gt1r, lambda kc: MODX(64, kc), 32, tmpd, R_tmpd, [R_modc])
    em.op('dve', lambda e: e.tensor_tensor(out=gbr, in0=gt1r, in1=brow, op=ALU.mult), reads=[R_gt1r, R_brow], writes=[R_gbr])
    yv = ysc.rearrange("k p t -> p k t")
    n = 0
    for ct in range(8):
        for q in range(4):
            sb = q % 2
            em.dma('sp', lambda e, sb=sb, ct=ct, q=q: e.dma_start(out=wstg[sb], in_=wout[ct][:, q * 8:(q + 1) * 8, :]), writes=[R_wstg[sb]],
                   stream='cw', depth=2)
            copy_op(em, 'act' if q % 2 == 0 else 'dve', wbf[:, q * 8:(q + 1) * 8, :], wstg[sb], [R_wstg[sb]], [R_wbf])
        csl = slice(ct * 512, (ct + 1) * 512)
        for t8 in range(8):
            yb = (ct * 8 + t8) % 2
            em.dma('sp', lambda e, yb=yb, t8=t8: e.dma_start(out=yt[yb], in_=yv[:, :, t8 * 512:(t8 + 1) * 512]), writes=[R_yt[yb]], stream='cy', depth=2)
            for sub in range(4):
                t0 = t8 * 512 + sub * 128
                pi = n % 4; ps = PSB[pi]; psr = PSR[pi]
                xb = n % 3; n += 1
                em.dma('act', lambda e, xb=xb, t0=t0, csl=csl: e.dma_start(out=xt[xb], in_=xtok[t0:t0 + 128, csl]), writes=[R_xt[xb]], stream='cx', depth=3)
                for kc in range(KC):
                    em.op('pe', lambda e, kc=kc, yb=yb, sub=sub, ps=ps: e.matmul(ps[:, :], lhsT=yt[yb][:, kc, sub * 128:(sub + 1) * 128], rhs=wbf[:, kc, :],
                                                                                 start=(kc == 0), stop=(kc == KC - 1)),
                          reads=[R_yt[yb], R_wbf], writes=[psr])
                em.op('dve', lambda e, ps=ps, xb=xb, csl=csl: e.tensor_tensor(out=ot[xb], in0=ps[:, :], in1=gt1r[:, csl], op=ALU.mult),
                      reads=[psr, R_gt1r], writes=[R_ot[xb]])
                em.op('pool', lambda e, xb=xb, csl=csl: e.tensor_tensor(out=xt[xb], in0=xt[xb], in1=gbr[:, csl], op=ALU.add),
                      reads=[R_xt[xb], R_gbr], writes=[R_xt[xb]])
                em.op('dve', lambda e, xb=xb: e.tensor_tensor(out=ot[xb], in0=ot[xb], in1=xt[xb], op=ALU.add),
                      reads=[R_ot[xb], R_xt[xb]], writes=[R_ot[xb]])
                em.dma('pool', lambda e, xb=xb, ct=ct, t0=t0: e.dma_start(out=acc[ct][t0:t0 + 128, :], in_=ot[xb]), reads=[R_ot[xb]], stream='co', depth=3)
    em.barrier()


def phase_moe(nc, em, L, L2):
    carve = L['carve']; PSB = L['PSB']; PSR = L['PSR']; dc = L['dc']; pc = L['pc']
    R_dc = L['R_dc']; R_modc = L['R_modc']; R_pc = L['R_pc']; R_tabs = L['R_tabs']; R_cf = L['R_cf']; MODX = L['MODX']
    identf = L['identf']; identb = L['identb']
    acc = L['acc']; hx2s = L['hx2s']; wr_d = L['wr_d']; wg = L['wg']; wu = L['wu']; wd = L['wd']; rows3_d = L['rows3_d']; out_d = L['out_d']
    dbg = L['dbg']
    KB = 256
    accv = acc.rearrange("c t j -> t c j")
    A2r = carve(0, [D]); R_A2r = Reg('A2r'); sh2r = carve(16 * KB, [D]); R_sh2r = Reg('sh2r')
    x1t = [carve((32 + 16 * i) * KB, [8, 512]) for i in range(2)]; R_x1t = [Reg(f'x1t{i}') for i in range(2)]
    junk = carve(64 * KB, [D], BF16); R_junk = Reg('junk')
    hx2b = [carve((72 + 8 * i) * KB, [D], BF16) for i in range(2)]; R_hx2b = [Reg(f'hx2b{i}') for i in range(2)]
    hx2T = carve(88 * KB, [32, 128], BF16); R_hx2T = Reg('hx2T')
    wrf = carve(96 * KB, [32, 16]); R_wrf = Reg('wrf'); wrb = carve(98 * KB, [32, 16], BF16); R_wrb = Reg('wrb')
    affT = carve(100 * KB, [T], parts=16); R_affT = Reg('affT')
    tmpd = [carve(116 * KB + i * 128, [128]) for i in range(2)]; R_tmpd = [Reg(f'td{i}') for i in range(2)]
    sm = carve(117 * KB, [64]); R_sm = Reg('sm')
    lg = carve(118 * KB, [16]); R_lg = Reg('lg'); aff = carve(118 * KB + 16, [16]); R_aff = Reg('aff')
    build_row(em, L, A2r, R_A2r, lambda kc: dc[:, 64 + kc:65 + kc], 32, tmpd, R_tmpd, [R_dc])
    build_row(em, L, sh2r, R_sh2r, lambda kc: MODX(96, kc), 32, tmpd, R_tmpd, [R_modc])
    em.dma('sp', lambda e: e.dma_start(out=wrf, in_=wr_d), writes=[R_wrf], stream='ml', depth=2)
    copy_op(em, 'dve', wrb, wrf, [R_wrf], [R_wrb])
    NT = int(os.environ.get('KTT', '32'))
    for tt in range(NT):
        xb = tt % 2; t0 = tt * 128
        em.dma('sp', lambda e, xb=xb, t0=t0: e.dma_start(out=x1t[xb], in_=accv[t0:t0 + 128]), writes=[R_x1t[xb]], stream='x1', depth=2)
        x1f = x1t[xb].rearrange("p a b -> p (a b)")
        em.op('act', lambda e, x1f=x1f: e.activation(out=junk, in_=x1f, func=AF.Square, accum_out=sm[:, 0:1]), reads=[R_x1t[xb]], writes=[R_junk, R_sm])
        em.op('act', lambda e: e.activation(out=sm[:, 1:2], in_=sm[:, 0:1], func=AF.Sqrt, scale=1.0 / D, bias=EPS), reads=[R_sm], writes=[R_sm])
        em.op('dve', lambda e: e.reciprocal(out=sm[:, 1:2], in_=sm[:, 1:2]), reads=[R_sm], writes=[R_sm])
        em.op('dve', lambda e, x1f=x1f: e.scalar_tensor_tensor(out=x1f, in0=x1f, scalar=sm[:, 1:2], in1=A2r, op0=ALU.mult, op1=ALU.mult),
              reads=[R_x1t[xb], R_sm, R_A2r], writes=[R_x1t[xb]])
        em.op('pool', lambda e, x1f=x1f, xb=xb: e.tensor_tensor(out=hx2b[xb], in0=x1f, in1=sh2r, op=ALU.add), reads=[R_x1t[xb], R_sh2r], writes=[R_hx2b[xb]])
        em.dma('pool', lambda e, xb=xb, t0=t0: e.dma_start(out=hx2s[t0:t0 + 128, :], in_=hx2b[xb]), reads=[R_hx2b[xb]], stream='h2', depth=2)
        for k8 in range(4):
            ps = PSB[k8 % 2]; psr = PSR[k8 % 2]
            psb = ps[:, :].bitcast(BF16)
            for j in range(8):
                kc = k8 * 8 + j
                em.op('pe', lambda e, kc=kc, j=j, xb=xb, psb=psb: e.transpose(out=psb[:, j * 128:(j + 1) * 128], in_=hx2b[xb][:, kc * 128:(kc + 1) * 128], identity=identb),
                      reads=[R_hx2b[xb], R_tabs], writes=[psr])
            copy_op(em, 'act' if k8 % 2 == 0 else 'dve', hx2T[:, k8 * 8:(k8 + 1) * 8, :], psb.rearrange("p (j t) -> p j t", j=8), [psr], [R_hx2T])
        ps = PSB[2]; psr = PSR[2]
        for kc in range(KC):
            em.op('pe', lambda e, kc=kc, ps=ps: e.matmul(ps[:, 0:16], lhsT=hx2T[:, kc, :], rhs=wrb[:, kc, :], start=(kc == 0), stop=(kc == KC - 1)),
                  reads=[R_hx2T, R_wrb], writes=[psr])
        copy_op(em, 'dve', lg, ps[:, 0:16], [psr], [R_lg])
        em.op('dve', lambda e: e.tensor_reduce(out=sm[:, 2:3], in_=lg, axis=AX.X, op=ALU.max, negate=True), reads=[R_lg], writes=[R_sm])
        em.op('act', lambda e: e.activation(out=aff, in_=lg, func=AF.Exp, bias=sm[:, 2:3], accum_out=sm[:, 3:4]), reads=[R_lg, R_sm], writes=[R_aff, R_sm])
        em.op('dve', lambda e: e.reciprocal(out=sm[:, 4:5], in_=sm[:, 3:4]), reads=[R_sm], writes=[R_sm])
        em.op('dve', lambda e: e.tensor_scalar(out=aff, in0=aff, scalar1=sm[:, 4:5], scalar2=None, op0=ALU.mult), reads=[R_aff, R_sm], writes=[R_aff])
        ps3 = PSB[3]; psr3 = PSR[3]
        em.op('pe', lambda e, ps3=ps3: e.transpose(out=ps3[0:16, 0:128], in_=aff, identity=identf), reads=[R_aff, R_cf], writes=[psr3])
        copy_op(em, 'act', affT[0:16, t0:t0 + 128], ps3[0:16, 0:128], [psr3], [R_affT])
    if STAGE < 99:
        em.dma('sp', lambda e: e.dma_start(out=dbg['d_a'][0:16, :], in_=affT), reads=[R_affT], stream='dbg', depth=2)
    em.barrier()
    if STAGE < 9:
        return
    work = carve(0, [T], parts=16); R_work = Reg('work')
    vals = carve(16 * KB, [CAP], parts=16); R_vals = Reg('vals')
    idxs = carve(18 * KB, [CAP], U32, parts=16); R_idxs = Reg('idxs')
    idxf = carve(20 * KB, [CAP], parts=16); R_idxf = Reg('idxf')
    IDT_OFF = 176 * KB
    idxT = carve(IDT_OFF, [64], U32); R_idxT = Reg('idxT')
    gateT = carve(IDT_OFF + 64, [64]); R_gateT = Reg('gateT')
    idxTf = carve(IDT_OFF + 128, [64]); R_idxTf = Reg('idxTf')
    copy_op(em, 'dve', work, affT, [R_affT], [R_work])
    for it in range(CAP // 8):
        sl = slice(it * 8, it * 8 + 8)
        em.op('dve', lambda e, sl=sl: e.max(out=vals[:, sl], in_=work), reads=[R_work], writes=[R_vals])
        em.op('dve', lambda e, sl=sl: e.max_index(out=idxs[:, sl], in_max=vals[:, sl], in_values=work), reads=[R_work, R_vals], writes=[R_idxs])
        em.op('dve', lambda e, sl=sl: e.match_replace(out=work, in_to_replace=vals[:, sl], in_values=work, imm_value=-1.0),
              reads=[R_work, R_vals], writes=[R_work])
    copy_op(em, 'dve', idxf, idxs, [R_idxs], [R_idxf])
    for st in range(4):
        ps = PSB[st % 2]; psr = PSR[st % 2]
        em.op('pe', lambda e, st=st, ps=ps: e.transpose(out=ps[:, 0:16], in_=idxf[0:16, st * 128:(st + 1) * 128], identity=identf[0:16, 0:16]),
              reads=[R_idxf, R_cf], writes=[psr])
        em.op('pe', lambda e, st=st, ps=ps: e.transpose(out=ps[:, 16:32], in_=vals[0:16, st * 128:(st + 1) * 128], identity=identf[0:16, 0:16]),
              reads=[R_vals, R_cf], writes=[psr])
        copy_op(em, 'dve', idxTf[:, st * 16:(st + 1) * 16], ps[:, 0:16], [psr], [R_idxTf])
        copy_op(em, 'dve', gateT[:, st * 16:(st + 1) * 16], ps[:, 16:32], [psr], [R_gateT])
    copy_op(em, 'dve', idxT, idxTf, [R_idxTf], [R_idxT])
    if STAGE < 99:
        em.dma('sp', lambda e: e.dma_start(out=dbg['d_b'][0:16, 0:512], in_=idxf), reads=[R_idxf], stream='dbg', depth=2)
        em.dma('sp', lambda e: e.dma_start(out=dbg['d_b'][16:32, 0:512], in_=vals), reads=[R_vals], stream='dbg', depth=2)
    em.barrier()
    if STAGE < 10:
        return
    XT = carve(0, [32, 512], BF16); R_XT = Reg('XT')
    actT = carve(32 * KB, [16, 512], BF16); R_actT = Reg('actT')
    wst = [carve((48 + 16 * i) * KB, [32, 128]) for i in range(2)]; R_wst = [Reg(f'mw{i}') for i in range(2)]
    wgb = [carve((80 + 8 * i) * KB, [32, 128], BF16) for i in range(2)]; R_wgb = [Reg(f'wgb{i}') for i in range(2)]
    wub = [carve((96 + 8 * i) * KB, [32, 128], BF16) for i in range(2)]; R_wub = [Reg(f'wub{i}') for i in range(2)]
    wdb = [carve((112 + 16 * i) * KB, [16, 512], BF16) for i in range(2)]; R_wdb = [Reg(f'wdb{i}') for i in range(2)]
    Xg = carve(112 * KB, [4, D], BF16)
    R_Xg = [R_wdb[0], R_wdb[0], R_wdb[1], R_wdb[1]]
    stmp = [carve((144 + 2 * i) * KB, [512]) for i in range(2)]; R_stmp = [Reg(f'st{i}') for i in range(2)]
    yo = [carve((148 + 2 * i) * KB, [512]) for i in range(4)]; R_yo = [Reg(f'yo{i}') for i in range(4)]
    gt2r = carve(156 * KB, [D]); R_gt2r = Reg('gt2r')
    tmpd2 = [carve(180 * KB + i * 128, [128]) for i in range(2)]; R_tmpd2 = [Reg(f'td2{i}') for i in range(2)]
    build_row(em, L, gt2r, R_gt2r, lambda kc: MODX(160, kc), 32, tmpd2, R_tmpd2, [R_modc])
    R_acc = [Reg(f'acc{i}') for i in range(8)]
    R_wsth = [[Reg(f'mwh{i}{j}') for j in range(2)] for i in range(2)]
    NEX = int(os.environ.get('KEXP', '16'))
    nyo = 0; nw = 0
    for ex in range(NEX):
        for st in range(4):
            em.dma('pool', lambda e, st=st, ex=ex: e.indirect_dma_start(out=Xg[:, st, :], out_offset=None, in_=hx2s,
                                                                       in_offset=bass.IndirectOffsetOnAxis(ap=idxT[:, st * 16 + ex:st * 16 + ex + 1], axis=0)),
                   reads=[R_idxT], writes=[R_Xg[st]], stream='gx', depth=2)
        for st in range(4):
            for k8 in range(4):
                ps = PSB[k8 % 2]; psr = PSR[k8 % 2]
                psb = ps[:, :].bitcast(BF16)
                for j in range(8):
                    kc = k8 * 8 + j
                    em.op('pe', lambda e, kc=kc, j=j, st=st, psb=psb: e.transpose(out=psb[:, j * 128:(j + 1) * 128], in_=Xg[:, st, kc * 128:(kc + 1) * 128], identity=identb),
                          reads=[R_Xg[st], R_tabs], writes=[psr])
                copy_op(em, 'act' if k8 % 2 == 0 else 'dve', XT[:, k8 * 8:(k8 + 1) * 8, st * 128:(st + 1) * 128], psb.rearrange("p (j t) -> p j t", j=8), [psr], [R_XT])
        for fc in range(16):
            wb = fc % 2
            em.dma('sp', lambda e, ex=ex, fc=fc: e.dma_start(out=wst[0], in_=wg[ex, fc]), writes=[R_wst[0]], stream='mw0', depth=1)
            copy_op(em, 'act', wgb[wb], wst[0], [R_wst[0]], [R_wgb[wb]])
            em.dma('sp', lambda e, ex=ex, fc=fc: e.dma_start(out=wst[1], in_=wu[ex, fc]), writes=[R_wst[1]], stream='mw1', depth=1)
            copy_op(em, 'dve', wub[wb], wst[1], [R_wst[1]], [R_wub[wb]])
            psg = PSB[2 + (fc % 2) * 2]; psrg = PSR[2 + (fc % 2) * 2]; psu = PSB[3 + (fc % 2) * 2]; psru = PSR[3 + (fc % 2) * 2]
            for kc in range(KC):
                em.op('pe', lambda e, kc=kc, psg=psg, wb=wb: e.matmul(psg[:, :], lhsT=wgb[wb][:, kc, :], rhs=XT[:, kc, :], start=(kc == 0), stop=(kc == KC - 1)),
                      reads=[R_wgb[wb], R_XT], writes=[psrg])
            for kc in range(KC):
                em.op('pe', lambda e, kc=kc, psu=psu, wb=wb: e.matmul(psu[:, :], lhsT=wub[wb][:, kc, :], rhs=XT[:, kc, :], start=(kc == 0), stop=(kc == KC - 1)),
                      reads=[R_wub[wb], R_XT], writes=[psru])
            sb = fc % 2
            em.op('act', lambda e, sb=sb, psg=psg: e.activation(out=stmp[sb], in_=psg[:, :], func=AF.Silu), reads=[psrg], writes=[R_stmp[sb]])
            em.op('dve', lambda e, sb=sb, psu=psu, fc=fc: e.tensor_tensor(out=actT[:, fc, :], in0=psu[:, :], in1=stmp[sb], op=ALU.mult),
                  reads=[psru, R_stmp[sb]], writes=[R_actT])
        for ct in range(8):
            db = ct % 2
            for hq in range(2):
                wv = wst[hq].rearrange("p (a b) c -> p a (b c)", a=8)
                em.dma('sp', lambda e, hq=hq, ex=ex, ct=ct, wv=wv: e.dma_start(out=wv, in_=wd[ex, ct][:, hq * 8:(hq + 1) * 8, :]),
                       writes=[R_wst[hq]], stream='mw%d' % hq, depth=1)
                copy_op(em, 'act' if hq == 0 else 'dve', wdb[db][:, hq * 8:(hq + 1) * 8, :], wv, [R_wst[hq]], [R_wdb[db]])
            for st in range(4):
                pi = (ct * 4 + st) % 2; ps = PSB[pi]; psr = PSR[pi]
                for fk in range(16):
                    em.op('pe', lambda e, fk=fk, st=st, ps=ps, db=db: e.matmul(ps[:, :], lhsT=actT[:, fk, st * 128:(st + 1) * 128], rhs=wdb[db][:, fk, :],
                                                                               start=(fk == 0), stop=(fk == 15)), reads=[R_actT, R_wdb[db]], writes=[psr])
                yb = nyo % 4; nyo += 1
                em.op('dve', lambda e, yb=yb, ps=ps, st=st, ex=ex, ct=ct: e.scalar_tensor_tensor(
                    out=yo[yb], in0=ps[:, :], scalar=gateT[:, st * 16 + ex:st * 16 + ex + 1], in1=gt2r[:, ct * 512:(ct + 1) * 512], op0=ALU.mult, op1=ALU.mult),
                    reads=[psr, R_gateT, R_gt2r], writes=[R_yo[yb]])
                em.dma('pool', lambda e, yb=yb, st=st, ex=ex, ct=ct: e.indirect_dma_start(
                    out=acc.rearrange("c t j -> (c t) j"), out_offset=bass.IndirectOffsetOnAxis(ap=idxT[:, st * 16 + ex:st * 16 + ex + 1], axis=0),
                    in_=yo[yb], in_offset=None, element_offset=ct * T * 512, compute_op=ALU.add),
                    reads=[R_yo[yb], R_idxT], writes=[R_acc[ct]], stream='sc', depth=4)
    em.barrier()
    if STAGE < 11:
        return
    gfr = carve(0, [D]); R_gfr = Reg('gfr')
    x2t = [carve((16 + 16 * i) * KB, [8, 512]) for i in range(2)]; R_x2t = [Reg(f'x2t{i}') for i in range(2)]
    junk2 = carve(48 * KB, [D], BF16); R_junk2 = Reg('junk2')
    ofin = [carve((56 + 16 * i) * KB, [D]) for i in range(2)]; R_ofin = [Reg(f'of{i}') for i in range(2)]
    sm2 = carve(90 * KB, [8]); R_sm2 = Reg('sm2')
    em.dma('sp', lambda e: e.dma_start(out=gfr, in_=rows3_d[1]), writes=[R_gfr], stream='ml', depth=2)
    for tt in range(32):
        xb = tt % 2; t0 = tt * 128
        em.dma('sp', lambda e, xb=xb, t0=t0: e.dma_start(out=x2t[xb], in_=accv[t0:t0 + 128]), reads=R_acc, writes=[R_x2t[xb]], stream='x2', depth=2)
        x2f = x2t[xb].rearrange("p a b -> p (a b)")
        em.op('act', lambda e, x2f=x2f: e.activation(out=junk2, in_=x2f, func=AF.Square, accum_out=sm2[:, 0:1]), reads=[R_x2t[xb]], writes=[R_junk2, R_sm2])
        em.op('act', lambda e: e.activation(out=sm2[:, 1:2], in_=sm2[:, 0:1], func=AF.Sqrt, scale=1.0 / D, bias=EPS), reads=[R_sm2], writes=[R_sm2])
        em.op('dve', lambda e: e.reciprocal(out=sm2[:, 1:2], in_=sm2[:, 1:2]), reads=[R_sm2], writes=[R_sm2])
        em.op('dve', lambda e, x2f=x2f, xb=xb: e.scalar_tensor_tensor(out=ofin[xb], in0=x2f, scalar=sm2[:, 1:2], in1=gfr, op0=ALU.mult, op1=ALU.mult),
              reads=[R_x2t[xb], R_sm2, R_gfr], writes=[R_ofin[xb]])
        em.dma('pool', lambda e, xb=xb, t0=t0: e.dma_start(out=out_d[t0:t0 + 128, :], in_=ofin[xb]), reads=[R_ofin[xb]], stream='fo', depth=2)
    em.barrier()


def kernel(**inputs):
    maps, poff, toff = prep_inputs(inputs)
    ntab = maps[0]['tabs'].shape[1]; npc = maps[0]['pcols'].shape[1]
    nc = build(poff, toff, ntab, npc)
    res = run_bass_kernel_spmd(nc, maps, core_ids=list(range(NCORES)))
    if STAGE < 99:
        return res
    return np.stack([r['out'] for r in res.results], 0)
```
